# Optimizing a Trainium2 kernel written in Bass

```python
import jax, jax.numpy as jnp
from jax import lax
import numpy as np

D_MODEL = 1024
BATCH = 32
SEQ = 2048
DEPTH = 1

CHUNK = 64
N_HEADS = 8
HEAD_DIM = 64
ATTN_WIDTH = N_HEADS * HEAD_DIM
N_IDX_HEADS = 4
IDX_DIM = 64
TOPK_MAX = 256
Q_BLOCK = 128
POOL_WINDOWS = (2, 4, 8, 16)
N_POOL_GROUPS = 4
POOL_WIDTH = 512
POOL_GROUP = POOL_WIDTH // N_POOL_GROUPS
N_BRANCHES = 2
PEER_HEADS = 8
PEER_QDIM = 128
PEER_HALF = PEER_QDIM // 2
N_KEYS = 128
N_EXPERTS = N_KEYS * N_KEYS
PEER_TOPK = 16
TOKEN_BLOCK = 128
EPS = 1e-6
IN_SIZES = (ATTN_WIDTH, HEAD_DIM, HEAD_DIM, N_IDX_HEADS * IDX_DIM, IDX_DIM, N_IDX_HEADS, POOL_WIDTH, N_BRANCHES * D_MODEL)
IN_WIDTH = sum(IN_SIZES)

kernel_name = 'chunk_causal_dsa_pool_peer_hybrid'


def rms_norm(x, g):
    x32 = x.astype(jnp.float32)
    y = x32 * lax.rsqrt(jnp.mean(x32 * x32, axis=-1, keepdims=True) + EPS)
    return (y * g.astype(jnp.float32)).astype(x.dtype)


def dsa_attention(q, k, v, iq, ik, iw):
    B, S = q.shape[0], q.shape[1]
    n_sel = min(TOPK_MAX, S // 4)
    n_blk = S // Q_BLOCK
    key_chunk = jnp.arange(S) // CHUNK
    idx_scale = IDX_DIM ** -0.5
    w_scale = N_IDX_HEADS ** -0.5
    attn_scale = HEAD_DIM ** -0.5

    def to_blocks(a):
        return jnp.swapaxes(a.reshape((B, n_blk, Q_BLOCK) + a.shape[2:]), 0, 1)

    def gather_rows(table, idx):
        return jax.vmap(lambda tb, ib: tb[ib])(table, idx)

    def one_block(args):
        qb, iqb, iwb, blk = args
        q_chunk = (blk * Q_BLOCK + jnp.arange(Q_BLOCK)) // CHUNK
        admissible = key_chunk[None, :] <= q_chunk[:, None]
        rel = jax.nn.relu(jnp.einsum('bqhd,bsd->bqhs', iqb, ik) * idx_scale)
        score = jnp.einsum('bqhs,bqh->bqs', rel, iwb * w_scale).astype(jnp.float32)
        score = jnp.where(admissible[None], score, -jnp.inf)
        _, sel = lax.top_k(score, n_sel)
        k_sel = gather_rows(k, sel)
        v_sel = gather_rows(v, sel)
        ok = key_chunk[sel] <= q_chunk[None, :, None]
        logits = jnp.einsum('bqhd,bqkd->bqhk', qb, k_sel).astype(jnp.float32) * attn_scale
        logits = jnp.where(ok[:, :, None, :], logits, -jnp.inf)
        p = jax.nn.softmax(logits, axis=-1).astype(v.dtype)
        return jnp.einsum('bqhk,bqkd->bqhd', p, v_sel)

    out = lax.map(one_block, (to_blocks(q), to_blocks(iq), to_blocks(iw), jnp.arange(n_blk)))
    return jnp.swapaxes(out, 0, 1).reshape(B, S, ATTN_WIDTH)


def pool_mixer(p, w_grp, s_pool):
    B, S = p.shape[0], p.shape[1]
    pg = p.reshape(B, S, N_POOL_GROUPS, POOL_GROUP).astype(jnp.float32)
    cs = jnp.pad(jnp.cumsum(pg, axis=1), ((0, 0), (1, 0), (0, 0), (0, 0)))
    t = jnp.arange(S)
    outs = []
    for g, w in enumerate(POOL_WINDOWS):
        cs_g = cs[:, :, g, :]
        lo = jnp.maximum(t + 1 - w, 0)
        cnt = (t + 1 - lo).astype(jnp.float32)
        mean = (cs_g[:, t + 1] - cs_g[:, lo]) / cnt[None, :, None]
        outs.append(mean - pg[:, :, g, :])
    mixed = jnp.stack(outs, axis=2).astype(p.dtype)
    mixed = jnp.einsum('bsgc,gcd->bsgd', mixed, w_grp) * s_pool
    return mixed.reshape(B, S, POOL_WIDTH)


def peer_ffn(h, w_q, sub_keys, u, v):
    B, S, D = h.shape
    n_blk = (B * S) // TOKEN_BLOCK
    hb = h.reshape(n_blk, TOKEN_BLOCK, D)

    def one_block(hx):
        q = (hx @ w_q).reshape(TOKEN_BLOCK, PEER_HEADS, 2, PEER_HALF)
        s = jnp.einsum('nhpd,hpkd->nhpk', q, sub_keys).astype(jnp.float32)
        top_s, top_i = lax.top_k(s, PEER_TOPK)
        cand = top_s[:, :, 0, :, None] + top_s[:, :, 1, None, :]
        best_s, best_p = lax.top_k(cand.reshape(TOKEN_BLOCK, PEER_HEADS, PEER_TOPK * PEER_TOPK), PEER_TOPK)
        i1 = jnp.take_along_axis(top_i[:, :, 0], best_p // PEER_TOPK, axis=-1)
        i2 = jnp.take_along_axis(top_i[:, :, 1], best_p % PEER_TOPK, axis=-1)
        expert = (i1 * N_KEYS + i2).reshape(TOKEN_BLOCK, PEER_HEADS * PEER_TOPK)
        gate = jax.nn.softmax(best_s, axis=-1).reshape(TOKEN_BLOCK, PEER_HEADS * PEER_TOPK).astype(hx.dtype)
        u_sel = u[expert]
        v_sel = v[expert]
        act = jax.nn.gelu(jnp.einsum('nd,ned->ne', hx, u_sel), approximate=False)
        return jnp.einsum('ne,ned->nd', gate * act, v_sel)

    return lax.map(one_block, hb).reshape(B, S, D)


def setup_inputs(seed: int = 0) -> dict:
    key = jax.random.key(seed)
    ks = jax.random.split(key, 20)
    f32 = jnp.float32
    D = D_MODEL

    def nrm(k, shape, scale):
        return jax.random.normal(k, shape, f32) * scale

    return {
        'x': nrm(ks[0], (BATCH, SEQ, D), 1.0),
        'c': nrm(ks[1], (BATCH, D), 1.0),
        'w_ada': nrm(ks[2], (DEPTH, D, 6 * D), 0.5 * D ** -0.5),
        'b_ada': nrm(ks[3], (DEPTH, 6 * D), 0.02),
        'g_norm1': 1.0 + nrm(ks[4], (DEPTH, D), 0.02),
        'w_in': nrm(ks[5], (DEPTH, D, IN_WIDTH), D ** -0.5),
        'g_q': 1.0 + nrm(ks[6], (DEPTH, HEAD_DIM), 0.02),
        'g_k': 1.0 + nrm(ks[7], (DEPTH, HEAD_DIM), 0.02),
        'g_ik': 1.0 + nrm(ks[8], (DEPTH, IDX_DIM), 0.02),
        'w_pool_grp': nrm(ks[9], (DEPTH, N_POOL_GROUPS, POOL_GROUP, POOL_GROUP), POOL_GROUP ** -0.5),
        's_pool': 1.0 + nrm(ks[10], (DEPTH, N_POOL_GROUPS, POOL_GROUP), 0.1),
        'w_up_attn': nrm(ks[11], (DEPTH, ATTN_WIDTH, D), ATTN_WIDTH ** -0.5),
        'w_up_pool': nrm(ks[12], (DEPTH, POOL_WIDTH, D), POOL_WIDTH ** -0.5),
        'w_out': nrm(ks[13], (DEPTH, D, D), D ** -0.5),
        'g_norm2': 1.0 + nrm(ks[14], (DEPTH, D), 0.02),
        'w_peer_q': nrm(ks[15], (DEPTH, D, PEER_HEADS * PEER_QDIM), D ** -0.5),
        'peer_subkeys': nrm(ks[16], (DEPTH, PEER_HEADS, 2, N_KEYS, PEER_HALF), PEER_HALF ** -0.5),
        'peer_u': nrm(ks[17], (DEPTH, N_EXPERTS, D), D ** -0.5),
        'peer_v': nrm(ks[18], (DEPTH, N_EXPERTS, D), 0.5),
    }


def reference(x, c, w_ada, b_ada, g_norm1, w_in, g_q, g_k, g_ik, w_pool_grp, s_pool,
              w_up_attn, w_up_pool, w_out, g_norm2, w_peer_q, peer_subkeys, peer_u, peer_v):
    B, S, D = x.shape
    cuts = [int(v) for v in np.cumsum(IN_SIZES)[:-1]]
    for layer in range(DEPTH):
        ada = c @ w_ada[layer] + b_ada[layer]
        shift1, scale1, gate1, shift2, scale2, gate2 = [a[:, None, :] for a in jnp.split(ada, 6, axis=-1)]

        h = rms_norm(x, g_norm1[layer]) * (1.0 + scale1) + shift1
        z = h @ w_in[layer]
        zq, zk, zv, ziq, zik, ziw, zp, zg = jnp.split(z, cuts, axis=-1)
        q = rms_norm(zq.reshape(B, S, N_HEADS, HEAD_DIM), g_q[layer])
        k = rms_norm(zk, g_k[layer])
        iq = ziq.reshape(B, S, N_IDX_HEADS, IDX_DIM)
        ik = rms_norm(zik, g_ik[layer])
        y_attn = dsa_attention(q, k, zv, iq, ik, ziw) @ w_up_attn[layer]
        y_pool = pool_mixer(zp, w_pool_grp[layer], s_pool[layer]) @ w_up_pool[layer]
        g_attn, g_pool = jnp.split(jax.nn.sigmoid(zg), 2, axis=-1)
        mixed = (g_attn * y_attn + g_pool * y_pool) @ w_out[layer]
        x = x + gate1 * mixed

        h2 = rms_norm(x, g_norm2[layer]) * (1.0 + scale2) + shift2
        x = x + gate2 * peer_ffn(h2, w_peer_q[layer], peer_subkeys[layer], peer_u[layer], peer_v[layer])
    return x
```

```python
from contextlib import ExitStack
import numpy as np
import concourse.bass as bass
import concourse.mybir as mybir
from concourse.bass_utils import run_bass_kernel_spmd

F32 = mybir.dt.float32
BF16 = mybir.dt.bfloat16
U32 = mybir.dt.uint32
ALU = mybir.AluOpType
AF = mybir.ActivationFunctionType
AX = mybir.AxisListType

D = 1024
NCORES = 8
INW = 3524
NEG = -1.0e30
C_Q, C_K, C_V, C_IQ, C_IK, C_IW, C_P, C_G = 0, 512, 576, 640, 896, 960, 964, 1476
POOLW = (2, 4, 8, 16)
NEXP = 16384


class Buf:
    __slots__ = ("name", "lw", "rd", "sem", "cnt")

    def __init__(self, name):
        self.name = name
        self.lw = None
        self.rd = {}
        self.sem = None
        self.cnt = 0


class Prog:
    ENG = ("pe", "act", "dve", "pool", "sp")

    def __init__(self):
        self.streams = {e: [] for e in self.ENG}
        self.n = {e: 0 for e in self.ENG}
        self.waited = {e: {} for e in self.ENG}
        self.dbufs = []

    def _need(self, eng, ev, waits):
        if ev is None:
            return
        key, val = ev
        if key[0] == "e" and key[1] == eng and eng == "pe":
            return
        w = self.waited[eng]
        if w.get(key, 0) >= val:
            return
        w[key] = val
        waits.append((key, val))

    def _deps(self, eng, R, W, waits):
        for b in R:
            self._need(eng, b.lw, waits)
        for b in W:
            self._need(eng, b.lw, waits)
            for k, v in b.rd.items():
                self._need(eng, (k, v), waits)

    def op(self, eng, fn, R=(), W=()):
        waits = []
        self._deps(eng, R, W, waits)
        self.n[eng] += 1
        key = ("e", eng)
        val = self.n[eng]
        for b in R:
            if b.rd.get(key, 0) < val:
                b.rd[key] = val
        for b in W:
            b.lw = (key, val)
            b.rd = {}
        self.streams[eng].append((waits, fn, None))

    def dma(self, q, fn, sb, R=(), W=()):
        waits = []
        self._deps(q, R, W, waits)
        if sb.cnt > 0:
            self._need(q, (("d", sb), 16 * sb.cnt), waits)
        if sb.sem is None:
            sb.sem = True
            self.dbufs.append(sb)
        sb.cnt += 1
        key = ("d", sb)
        val = 16 * sb.cnt
        for b in R:
            if b.rd.get(key, 0) < val:
                b.rd[key] = val
        for b in W:
            b.lw = (key, val)
            b.rd = {}
        self.streams[q].append((waits, fn, sb))

    def barrier(self):
        for e in self.ENG:
            waits = []
            for o in self.ENG:
                if o != e and self.n[o] > 0:
                    self._need(e, (("e", o), self.n[o]), waits)
            for b in self.dbufs:
                if b.cnt > 0:
                    self._need(e, (("d", b), 16 * b.cnt), waits)
            if e == "pe" and self.n["pe"] > 0:
                pass
            self.streams[e].append((waits, None, None))

    def final_wait(self, q, bufs):
        waits = []
        for b in bufs:
            self._need(q, b.lw, waits)
        self.streams[q].append((waits, None, None))

    def emit(self, nc, es):
        esem = {e: es.enter_context(nc.semaphore("S_" + e)) for e in self.ENG if self.n[e] > 0}
        for i, b in enumerate(self.dbufs):
            b.sem = es.enter_context(nc.semaphore("D%d" % i))

        def sem_of(key):
            return esem[key[1]] if key[0] == "e" else key[1].sem

        streams = self.streams

        def run(engname, eng):
            mysem = esem.get(engname)
            for waits, fn, sb in streams[engname]:
                for key, val in waits:
                    eng.wait_ge(sem_of(key), val)
                if fn is None:
                    continue
                ins = fn(eng)
                if sb is not None:
                    ins.then_inc(sb.sem, 16)
                else:
                    ins.then_inc(mysem, 1)

        with nc.Block() as block:
            if streams["pe"]:
                @block.tensor
                def _(e):
                    run("pe", e)
            if streams["act"]:
                @block.scalar
                def _(e):
                    run("act", e)
            if streams["dve"]:
                @block.vector
                def _(e):
                    run("dve", e)
            if streams["pool"]:
                @block.gpsimd
                def _(e):
                    run("pool", e)
            if streams["sp"]:
                @block.sync
                def _(e):
                    run("sp", e)


def _consts(S):
    ident = np.eye(128, dtype=np.float32)
    bands = np.zeros((128, 12, 128), np.float32)
    n = np.arange(128)
    for g, w in enumerate(POOLW):
        cur = ((n[:, None] <= n[None, :]) & (n[:, None] > n[None, :] - w)).astype(np.float32)
        bands[:, 0 * 4 + g, :] = cur / w - ident
        prv = ((n[:, None] - 128) > (n[None, :] - w)).astype(np.float32)
        bands[:, 1 * 4 + g, :] = prv / w
        cnt = np.minimum(w, n + 1).astype(np.float32)
        bands[:, 2 * 4 + g, :] = cur / cnt[None, :] - ident
    eps = np.broadcast_to(-(np.arange(S, dtype=np.float32)) * 1e-12, (128, S))
    iota = np.broadcast_to(np.arange(128, dtype=np.float32), (128, 128))
    return np.ascontiguousarray(
        np.concatenate([ident, bands.reshape(128, 12 * 128), eps, iota], axis=1).astype(np.float32))


def build(NB, S, NSEL, do_peer=True, dbg=None):
    NT = S // 128
    nc = bass.Bass("TRN2", target_bir_lowering=False)
    P = Prog()
    es = ExitStack()

    def din(name, shape, dt=F32):
        return nc.dram_tensor(name, list(shape), dt, kind="ExternalInput").ap()

    CW = 128 + 12 * 128 + S + 128
    x_d = din("x", [NB, S, D])
    crep_d = din("crep", [NB, 128, 1024])
    cst_d = din("cst", [128, CW])
    wada_d = din("w_ada", [D, 6 * D])
    bada_d = din("b_ada", [1, 6 * D])
    g1_d = din("g_norm1", [D])
    win_d = din("w_in", [D, INW])
    gq_d = din("g_q", [64])
    gk_d = din("g_k", [64])
    gik_d = din("g_ik", [64])
    wgrp_d = din("w_pool_grp", [4, 128, 128])
    spool_d = din("s_pool", [512])
    wua_d = din("w_up_attn", [512, D])
    wup_d = din("w_up_pool", [512, D])
    wout_d = din("w_out", [D, D])
    if do_peer:
        g2_d = din("g_norm2", [D])
        wpq_d = din("w_peer_q", [D, D])
        sk_d = din("peer_subkeys", [16, 128, 64])
        pu_d = din("peer_u", [NEXP, D])
        pv_d = din("peer_v", [NEXP, D])
    out_d = nc.dram_tensor("out", [NB, S, D], F32, kind="ExternalOutput").ap()
    dbg_d = None
    if dbg is not None:
        dbg_d = nc.dram_tensor("dbg", list(dbg), F32, kind="ExternalOutput").ap()

    def sb(name, shape, dt=F32):
        t = es.enter_context(nc.sbuf_tensor("s_" + name, list(shape), dt))
        return t, Buf(name)

    def ps(name):
        t = es.enter_context(nc.psum_tensor(name, [128, 512], F32))
        return t, Buf(name)

    OUTB = Buf("out_dram")

    cst, cstB = sb("cst_sb", [128, CW])
    ident_bf, identB = sb("ident_bf", [128, 128], BF16)
    irep, irepB = sb("irep", [128, 512], BF16)
    band, bandB = sb("band", [128, 12, 128], BF16)
    EPS0 = 128 + 12 * 128
    IOTA0 = EPS0 + S
    w_in, winB = sb("w_in", [128, 8, INW], BF16)
    w_out, woutB = sb("w_out", [128, 8, D], BF16)
    w_ua, wuaB = sb("w_ua", [128, 4, D], BF16)
    w_up, wupB = sb("w_up", [128, 4, D], BF16)
    w_grp, wgrpB = sb("w_grp", [128, 4, 128], BF16)
    g1bc, g1B = sb("g1bc", [128, D])
    gqbc, gqB = sb("gqbc", [128, 64])
    gkbc, gkB = sb("gkbc", [128, 64])
    gikbc, gikB = sb("gikbc", [128, 64])
    bbc, bbcB = sb("bbc", [128, 512])
    epst, epstB = sb("epst", [128, 1])
    stg = [sb("stg%d" % i, [128, 2048]) for i in range(2)]
    ada, adaB = sb("ada", [128, 3 * D])
    crep_bf, crepB = sb("crep_bf", [128, 8, 128], BF16)
    wada_bf, wadaB = sb("wada_bf", [128, 4, 512], BF16)
    kik, kikB = sb("kik", [128, S], BF16)
    kT, kTB = kik, kikB
    vaug, vaugB = sb("vaug", [128, NT, 65], BF16)
    xt = [sb("xt0", [128, D])] * 2
    junk, junkB = sb("junk", [128, D])
    tmpf, tmpfB = sb("tmpf", [128, D])
    tmpg, tmpgB = junk[:, 512:1024], junkB
    small, smallB = sb("small", [128, 64])
    h_bf, hbfB = sb("h_bf", [128, D], BF16)
    hT, hTB = sb("hT", [128, 8, 128], BF16)
    zq_f, zqB = sb("zq_f", [128, 512])
    qn_bf, qnB = sb("qn_bf", [128, 512], BF16)
    qT, qTB = sb("qT", [64, 1024], BF16)
    zb_f, zbB = sb("zb_f", [128, 452])
    knik, knikB = sb("knik", [128, 128], BF16)
    iq_pad, iqbB = sb("iq_pad", [128, 4, 128], BF16)
    iqT, iqTB = sb("iqT", [128, 4, 128], BF16)
    zp = [sb("zp%d" % i, [128, 512], BF16) for i in range(2)]
    gates, gatesB = sb("gates", [128, 2048], BF16)
    rbuf = [sb("rbuf%d" % i, [128, 512]) for i in range(2)] * 2
    sc, scB = stg[0]
    wk, wkB = stg[1]
    m8, m8B = sb("m8", [128, 8])
    Mb, MbB = sb("Mb", [128, S], BF16)
    pexp = [sb("pexp%d" % i, [128, 1024], BF16) for i in range(2)]
    attn, attnB = sb("attn", [128, 512], BF16)
    attnT, attnTB = sb("attnT", [128, 4, 128], BF16)
    mixT, mixTB = sb("mixT", [128, 4, 128], BF16)
    poolT, poolTB = sb("poolT", [128, 4, 128], BF16)
    m_bf, mbfB = sb("m_bf", [128, D], BF16)
    mT, mTB = sb("mT", [128, 8, 128], BF16)
    x1, x1B = tmpf, tmpfB

    bank = [ps("bank%d" % i) for i in range(8)]

    def bk(i):
        return bank[i][0]

    def bkB(i):
        return bank[i][1]

    def bkbf(i):
        return bank[i][0][:].bitcast(BF16)

    def dma(q, out_ap, in_ap, sbuf, R=(), W=()):
        P.dma(q, lambda e: e.dma_start(out=out_ap, in_=in_ap), sbuf, R=R, W=W)

    def mm(out_ap, lhsT, rhs, start, stop, R, W):
        P.op("pe", lambda e: e.matmul(out_ap, lhsT, rhs, start=start, stop=stop), R=R, W=W)

    def tr(out_ap, in_ap, R, W):
        P.op("pe", lambda e: e.transpose(out_ap, in_ap, ident_bf[:]), R=list(R) + [identB], W=W)

    def act(out_ap, in_ap, func, R, W, bias=None, scale=None, accum=None):
        kw = {}
        if bias is not None:
            kw["bias"] = bias
        if scale is not None:
            kw["scale"] = scale
        if accum is not None:
            kw["accum_out"] = accum
        P.op("act", lambda e: e.activation(out_ap, in_ap, func, **kw), R=R, W=W)

    def vcopy(eng, out_ap, in_ap, R, W):
        if eng == "act":
            P.op("act", lambda e: e.copy(out_ap, in_ap), R=R, W=W)
        else:
            P.op(eng, lambda e: e.tensor_copy(out_ap, in_ap), R=R, W=W)

    def tt(out_ap, a, b, op, R, W, eng="dve"):
        P.op(eng, lambda e: e.tensor_tensor(out_ap, a, b, op), R=R, W=W)

    def ts(out_ap, a, s1, s2, op0, op1, R, W, eng="dve", accum=None):
        if op1 is None:
            P.op(eng, lambda e: e.tensor_scalar(out_ap, a, s1, None, op0), R=R, W=W)
        elif accum is None:
            P.op(eng, lambda e: e.tensor_scalar(out_ap, a, s1, s2, op0, op1), R=R, W=W)
        else:
            P.op(eng, lambda e: e.tensor_scalar(out_ap, a, s1, s2, op0, op1, accum), R=R, W=W)

    def stt(out_ap, a, s, b, op0, op1, R, W, eng="dve"):
        P.op(eng, lambda e: e.scalar_tensor_tensor(out_ap, a, s, b, op0, op1), R=R, W=W)

    def rstd(out_ap, tmp_ap, ss_ap, n, RB, WB):
        act(tmp_ap, ss_ap, AF.Sqrt, [RB, epstB], [WB], bias=epst[:, 0:1], scale=1.0 / n)
        P.op("dve", lambda e: e.reciprocal(out_ap, tmp_ap), R=[WB], W=[WB])

    dma("sp", cst[:], cst_d, cstB, W=[cstB])
    vcopy("dve", ident_bf[:], cst[:, 0:128], [cstB], [identB])
    for i in range(4):
        vcopy("dve", irep[:, i * 128:(i + 1) * 128], cst[:, 0:128], [cstB], [irepB])
    vcopy("dve", band[:].rearrange("p a b -> p (a b)"), cst[:, 128:128 + 1536], [cstB], [bandB])
    P.op("dve", lambda e: e.memset(iq_pad[:], 0.0), W=[iqbB])
    P.op("dve", lambda e: e.memset(epst[:], 1e-6), W=[epstB])
    P.op("dve", lambda e: e.memset(vaug[:, :, 64:65], 1.0), W=[vaugB])
    dma("sp", g1bc[:], g1_d.partition_broadcast(128), g1B, W=[g1B])
    dma("sp", gqbc[:], gq_d.partition_broadcast(128), gqB, W=[gqB])
    dma("sp", gkbc[:], gk_d.partition_broadcast(128), gkB, W=[gkB])
    dma("sp", gikbc[:], gik_d.partition_broadcast(128), gikB, W=[gikB])

    nstg = [0]

    def load_cvt(dst_ap, src_ap, shape_free, dstB, eng=None):
        i = nstg[0] % 2
        nstg[0] += 1
        st, stB = stg[i]
        nel = int(np.prod(shape_free))
        view = st[:, 0:nel]
        if len(shape_free) == 2:
            view = view.rearrange("p (a b) -> p a b", a=shape_free[0])
        dma("sp", view, src_ap, stB, W=[stB])
        vcopy(eng or ("dve" if i == 0 else "act"), dst_ap, view, [stB], [dstB])

    win_v = win_d.rearrange("(kc p) n -> p kc n", p=128)
    for kc in range(8):
        load_cvt(w_in[:, kc, 0:2048], win_v[:, kc, 0:2048], [2048], winB)
        load_cvt(w_in[:, kc, 2048:INW], win_v[:, kc, 2048:INW], [INW - 2048], winB)
    wout_v = wout_d.rearrange("(kc p) n -> p kc n", p=128)
    for hh in range(4):
        load_cvt(w_out[:, hh * 2:(hh + 1) * 2, :], wout_v[:, hh * 2:(hh + 1) * 2, :], [2, D], woutB)
    wua_v = wua_d.rearrange("(kc p) n -> p kc n", p=128)
    wup_v = wup_d.rearrange("(kc p) n -> p kc n", p=128)
    for hh in range(2):
        load_cvt(w_ua[:, hh * 2:(hh + 1) * 2, :], wua_v[:, hh * 2:(hh + 1) * 2, :], [2, D], wuaB)
        load_cvt(w_up[:, hh * 2:(hh + 1) * 2, :], wup_v[:, hh * 2:(hh + 1) * 2, :], [2, D], wupB)
    st0, st0B = stg[0]
    st1, st1B = stg[1]
    dma("sp", st0[:, 0:512].rearrange("p (g d) -> p g d", g=4), wgrp_d.rearrange("g c d -> c g d"), st0B, W=[st0B])
    dma("sp", st1[:, 0:512], spool_d.partition_broadcast(128), st1B, W=[st1B])
    tt(w_grp[:].rearrange("p g d -> p (g d)"), st0[:, 0:512], st1[:, 0:512], ALU.mult, [st0B, st1B], [wgrpB])
    nstg[0] = 0

    def compute_ada(b, first_cg):
        st, stB = stg[0]
        dma("sp", st[:, 0:1024], crep_d[b], stB, W=[stB])
        vcopy("dve", crep_bf[:].rearrange("p a b -> p (a b)"), st[:, 0:1024], [stB], [crepB])
        nstg[0] = 1
        wada_v = wada_d.rearrange("(kc p) n -> p kc n", p=128)
        for ci in range(6):
            cg = first_cg + ci
            bi = ci % 2
            dma("sp", bbc[:], bada_d[0, cg * 512:(cg + 1) * 512].partition_broadcast(128), bbcB, W=[bbcB])
            for kh in range(2):
                load_cvt(wada_bf[:], wada_v[:, kh * 4:(kh + 1) * 4, cg * 512:(cg + 1) * 512], [4, 512], wadaB)
                for k4 in range(4):
                    kc = kh * 4 + k4
                    mm(bk(bi)[:], crep_bf[:, kc, :], wada_bf[:, k4, :], kc == 0, kc == 7,
                       [crepB, wadaB], [bkB(bi)])
            tt(ada[:, ci * 512:(ci + 1) * 512], bk(bi)[:], bbc[:], ALU.add, [bkB(bi), bbcB], [adaB])

    def ada_sl(i):
        return ada[:, i * D:(i + 1) * D]

    def rms_small(src_ap, width, gbc, gB, out_bf_ap, outB, srcB):
        tt(junk[:, 0:width], src_ap, src_ap, ALU.mult, [srcB], [junkB])
        P.op("dve", lambda e: e.tensor_reduce(small[:, 32:33], junk[:, 0:width], AX.X, ALU.add),
             R=[junkB], W=[smallB])
        rstd(small[:, 34:35], small[:, 33:34], small[:, 32:33], width, smallB, smallB)
        stt(out_bf_ap, src_ap, small[:, 34:35], gbc[:, 0:width], ALU.mult, ALU.mult,
            [srcB, smallB, gB], [outB])

    import os as _os
    _stage = int(_os.environ.get("KSTAGE", "99"))

    def token_tile(b, t):
        X, XB = xt[t % 2]
        dma("sp", X[:], x_d[b, t * 128:(t + 1) * 128, :], XB, W=[XB])
        act(junk[:], X[:], AF.Square, [XB], [junkB, smallB], accum=small[:, 0:1])
        rstd(small[:, 2:3], small[:, 1:2], small[:, 0:1], D, smallB, smallB)
        stt(tmpf[:], X[:], small[:, 2:3], ada_sl(1), ALU.mult, ALU.mult, [XB, smallB, adaB], [tmpfB])
        tt(h_bf[:], tmpf[:], ada_sl(0), ALU.add, [tmpfB, adaB], [hbfB])
        for kc in range(8):
            tr(bkbf(7)[:, kc * 128:(kc + 1) * 128], h_bf[:, kc * 128:(kc + 1) * 128], [hbfB], [bkB(7)])
        vcopy("act", hT[:].rearrange("p a b -> p (a b)"), bkbf(7)[:, 0:1024], [bkB(7)], [hTB])

        def zgroup(bi, c0, w):
            for kc in range(8):
                mm(bk(bi)[:, 0:w], hT[:, kc, :], w_in[:, kc, c0:c0 + w], kc == 0, kc == 7,
                   [hTB, winB], [bkB(bi)])

        if _stage < 3:
            return
        zgroup(0, C_Q, 512)
        vcopy("act", zq_f[:], bk(0)[:], [bkB(0)], [zqB])
        tt(junk[:, 0:512], zq_f[:], zq_f[:], ALU.mult, [zqB], [junkB])
        P.op("dve", lambda e: e.tensor_reduce(small[:, 8:16], junk[:, 0:512].rearrange("p (h d) -> p h d", h=8),
                                              AX.X, ALU.add), R=[junkB], W=[smallB])
        rstd(small[:, 24:32], small[:, 16:24], small[:, 8:16], 64, smallB, smallB)
        for h in range(8):
            stt(qn_bf[:, h * 64:(h + 1) * 64], zq_f[:, h * 64:(h + 1) * 64], small[:, 24 + h:25 + h],
                gqbc[:], ALU.mult, ALU.mult, [zqB, smallB, gqB], [qnB])
        for h in range(8):
            tr(bkbf(6)[0:64, h * 128:(h + 1) * 128], qn_bf[:, h * 64:(h + 1) * 64], [qnB], [bkB(6)])
        vcopy("act", qT[:], bkbf(6)[0:64, 0:1024], [bkB(6)], [qTB])
        if _stage < 4:
            return
        zgroup(1, C_K, 452)
        vcopy("act", zb_f[:], bk(1)[:, 0:452], [bkB(1)], [zbB])
        rms_small(zb_f[:, 0:64], 64, gkbc, gkB, knik[:, 0:64], knikB, zbB)
        rms_small(zb_f[:, 384:448], 64, gikbc, gikB, knik[:, 64:128], knikB, zbB)
        vcopy("dve", vaug[:, t, 0:64], zb_f[:, 64:128], [zbB], [vaugB])
        vcopy("dve", iq_pad[:, :, 64:128], zb_f[:, 128:384].rearrange("p (h d) -> p h d", h=4), [zbB], [iqbB])
        tr(bkbf(7)[:, 0:128], knik[:], [knikB], [bkB(7)])
        for h in range(4):
            tr(bkbf(7)[:, 128 + h * 128:128 + (h + 1) * 128], iq_pad[:, h, :], [iqbB], [bkB(7)])
        vcopy("act", kik[:, t * 128:(t + 1) * 128], bkbf(7)[:, 0:128], [bkB(7)], [kikB])
        vcopy("act", iqT[:].rearrange("p a b -> p (a b)"), bkbf(7)[:, 128:640], [bkB(7)], [iqTB])
        if _stage < 5:
            return
        ZP, ZPB = zp[t % 2]
        ZPp, ZPpB = zp[(t + 1) % 2]
        zgroup(2, C_P, 512)
        vcopy("act", ZP[:], bk(2)[:], [bkB(2)], [ZPB])
        for gi in range(4):
            bi = (3 + gi) % 4
            zgroup(bi, C_G + gi * 512, 512)
            act(gates[:, gi * 512:(gi + 1) * 512], bk(bi)[:], AF.Sigmoid, [bkB(bi)], [gatesB])

        if _stage < 6:
            return
        L = 128 * (t + 1)
        ngrp = (L + 511) // 512
        iw = zb_f[:, 448:452]
        for kg in range(ngrp):
            k0 = kg * 512
            w = min(512, L - k0)
            for h in range(4):
                mm(bk(h)[:, 0:w], iqT[64:128, h, :], kik[64:128, k0:k0 + w], True, True, [iqTB, kikB], [bkB(h)])
            for h in range(4):
                R_, RB_ = rbuf[h]
                act(R_[:, 0:w], bk(h)[:, 0:w], AF.Relu, [bkB(h)], [RB_])
                if h == 0:
                    stt(sc[:, k0:k0 + w], R_[:, 0:w], iw[:, 0:1], cst[:, EPS0 + k0:EPS0 + k0 + w],
                        ALU.mult, ALU.add, [RB_, zbB, cstB], [scB])
                else:
                    stt(sc[:, k0:k0 + w], R_[:, 0:w], iw[:, h:h + 1], sc[:, k0:k0 + w],
                        ALU.mult, ALU.add, [RB_, zbB, scB], [scB])
        if _stage < 7:
            return
        P.op("dve", lambda e: e.memset(sc[0:64, L - 64:L], NEG), W=[scB])
        if L - 64 >= NSEL:
            src = sc
            nr = NSEL // 8
            for r in range(nr):
                P.op("dve", (lambda s_: lambda e: e.max(m8[:], s_[:, 0:L]))(src), R=[scB, wkB], W=[m8B])
                if r < nr - 1:
                    P.op("dve", (lambda s_: lambda e: e.match_replace(wk[:, 0:L], m8[:], s_[:, 0:L], NEG))(src),
                         R=[scB, wkB, m8B], W=[wkB])
                    src = wk
            thr = m8[:, 7:8]
        else:
            P.op("dve", lambda e: e.memset(m8[:], -1.0e29), W=[m8B])
            thr = m8[:, 7:8]
        ts(Mb[:, 0:L], sc[:, 0:L], thr, -30000.0, ALU.is_lt, ALU.mult, [scB, m8B], [MbB])

        if _stage < 8:
            return
        def logits(j):
            lb = (0, 1) if j % 2 == 0 else (2, 3)
            for hf in range(2):
                bi = lb[hf]
                mm(bk(bi)[:], kik[0:64, j * 128:(j + 1) * 128], qT[:, hf * 512:(hf + 1) * 512], True, False,
                   [kTB, qTB], [bkB(bi)])
                mm(bk(bi)[:], Mb[:, j * 128:(j + 1) * 128], irep[:], False, True, [MbB, irepB], [bkB(bi)])

        logits(0)
        for j in range(t + 1):
            if j + 1 <= t:
                logits(j + 1)
            lb = (0, 1) if j % 2 == 0 else (2, 3)
            PX, PXB = pexp[j % 2]
            for hf in range(2):
                bi = lb[hf]
                act(PX[:, hf * 512:(hf + 1) * 512], bk(bi)[:], AF.Exp, [bkB(bi)], [PXB], scale=0.125)
            for h in range(8):
                ob = 4 + h // 4
                c0 = (h % 4) * 65
                mm(bk(ob)[:, c0:c0 + 65], PX[:, h * 128:(h + 1) * 128], vaug[:, j, :], (j == 0 and h % 4 == 0), (j == t and h % 4 == 3),
                   [PXB, vaugB], [bkB(ob)])
        if _stage < 9:
            return
        for hb in range(2):
            ov = bk(4 + hb)[:, 0:260].rearrange("p (h c) -> p h c", h=4)
            P.op("dve", (lambda o_, hb_: lambda e: e.reciprocal(small[:, 40 + 4 * hb_:44 + 4 * hb_].rearrange("p (h o) -> p h o", o=1),
                                                               o_[:, :, 64:65]))(ov, hb),
                 R=[bkB(4 + hb)], W=[smallB])
            tt(attn[:, hb * 256:(hb + 1) * 256].rearrange("p (h c) -> p h c", h=4), ov[:, :, 0:64],
               small[:, 40 + 4 * hb:44 + 4 * hb].rearrange("p (h o) -> p h o", o=1).to_broadcast([128, 4, 64]),
               ALU.mult, [bkB(4 + hb), smallB], [attnB])
        for c in range(4):
            tr(bkbf(6)[:, c * 128:(c + 1) * 128], attn[:, c * 128:(c + 1) * 128], [attnB], [bkB(6)])
        vcopy("act", attnT[:].rearrange("p a b -> p (a b)"), bkbf(6)[:, 0:512], [bkB(6)], [attnTB])
        for hf in range(2):
            for c in range(4):
                mm(bk(hf)[:], attnT[:, c, :], w_ua[:, c, hf * 512:(hf + 1) * 512], c == 0, c == 3,
                   [attnTB, wuaB], [bkB(hf)])
        if _stage < 10:
            return
        for g in range(4):
            kind = 2 if t == 0 else 0
            mm(bk(6)[:, g * 128:(g + 1) * 128], ZP[:, g * 128:(g + 1) * 128], band[:, kind * 4 + g, :], True, t == 0,
               [ZPB, bandB], [bkB(6)])
            if t > 0:
                mm(bk(6)[:, g * 128:(g + 1) * 128], ZPp[:, g * 128:(g + 1) * 128], band[:, 4 + g, :], False, True,
                   [ZPpB, bandB], [bkB(6)])
        vcopy("act", mixT[:].rearrange("p a b -> p (a b)"), bk(6)[:], [bkB(6)], [mixTB])
        for g in range(4):
            mm(bk(7)[:, g * 128:(g + 1) * 128], w_grp[:, g, :], mixT[:, g, :], True, True, [wgrpB, mixTB], [bkB(7)])
        vcopy("act", poolT[:].rearrange("p a b -> p (a b)"), bk(7)[:], [bkB(7)], [poolTB])
        for hf in range(2):
            for g in range(4):
                mm(bk(2 + hf)[:], poolT[:, g, :], w_up[:, g, hf * 512:(hf + 1) * 512], g == 0, g == 3,
                   [poolTB, wupB], [bkB(2 + hf)])
        if _stage < 11:
            return
        for hf in range(2):
            cs = slice(hf * 512, (hf + 1) * 512)
            tt(tmpf[:, cs], bk(hf)[:], gates[:, hf * 512:(hf + 1) * 512], ALU.mult, [bkB(hf), gatesB], [tmpfB])
            tt(tmpg[:], bk(2 + hf)[:], gates[:, 1024 + hf * 512:1024 + (hf + 1) * 512], ALU.mult,
               [bkB(2 + hf), gatesB], [tmpgB])
            tt(m_bf[:, cs], tmpf[:, cs], tmpg[:], ALU.add, [tmpfB, tmpgB], [mbfB])
        for kc in range(8):
            tr(bkbf(6)[:, kc * 128:(kc + 1) * 128], m_bf[:, kc * 128:(kc + 1) * 128], [mbfB], [bkB(6)])
        vcopy("act", mT[:].rearrange("p a b -> p (a b)"), bkbf(6)[:, 0:1024], [bkB(6)], [mTB])
        for hf in range(2):
            for kc in range(8):
                mm(bk(4 + hf)[:], mT[:, kc, :], w_out[:, kc, hf * 512:(hf + 1) * 512], kc == 0, kc == 7,
                   [mTB, woutB], [bkB(4 + hf)])
        for hf in range(2):
            cs = slice(hf * 512, (hf + 1) * 512)
            tt(tmpf[:, cs], bk(4 + hf)[:], ada[:, 2 * D + hf * 512:2 * D + (hf + 1) * 512], ALU.mult,
               [bkB(4 + hf), adaB], [tmpfB])
            tt(x1[:, cs], tmpf[:, cs], X[:, cs], ALU.add, [tmpfB, XB], [x1B])
        dma("sp", out_d[b, t * 128:(t + 1) * 128, :], x1[:], x1B, R=[x1B], W=[OUTB])

    for b in range(NB):
        if _stage < 1:
            break
        compute_ada(b, 0)
        if _stage < 2:
            break
        stt(ada_sl(1), ada_sl(1), 1.0, g1bc[:], ALU.add, ALU.mult, [adaB, g1B], [adaB])
        for t in range(NT):
            token_tile(b, t)

    P.final_wait("sp", [OUTB])
    P.emit(nc, es)
    es.close()
    if do_peer:
        nc.all_engine_barrier()
        _phase_p(nc, NB, S, dict(crep=crep_d, cst=cst_d, w_ada=wada_d, b_ada=bada_d, g2=g2_d, wpq=wpq_d,
                                 sk=sk_d, pu=pu_d, pv=pv_d, out=out_d, IOTA0=IOTA0))
    return nc


def _phase_p(nc, NB, S, dr):
    TG = 256
    NG = S // TG
    P = Prog()
    es = ExitStack()
    crep_d, cst_d, wada_d, bada_d, g2_d = dr["crep"], dr["cst"], dr["w_ada"], dr["b_ada"], dr["g2"]
    wpq_d, sk_d, pu_d, pv_d, out_d, IOTA0 = dr["wpq"], dr["sk"], dr["pu"], dr["pv"], dr["out"], dr["IOTA0"]
    uT_d = nc.dram_tensor("uT_scr", [128, 128, 1024], BF16, kind="Internal").ap()
    v_d = nc.dram_tensor("v_scr", [128, 128, 1024], BF16, kind="Internal").ap()
    UTD, VD, OUTB = Buf("uT_d"), Buf("v_d"), Buf("out2")

    def sb(name, shape, dt=F32):
        t = es.enter_context(nc.sbuf_tensor("p_" + name, list(shape), dt))
        return t, Buf(name)

    bank = []
    for i in range(8):
        t_ = es.enter_context(nc.psum_tensor("pbank%d" % i, [128, 512], F32))
        bank.append((t_, Buf("pbank%d" % i)))

    def bk(i):
        return bank[i][0]

    def bkB(i):
        return bank[i][1]

    def bkbf(i):
        return bank[i][0][:].bitcast(BF16)

    cst2, cst2B = sb("cst2", [128, 256])
    ident_bf, identB = sb("ident_bf", [128, 128], BF16)
    iota_bf, iotaB = sb("iota_bf", [128, 128], BF16)
    zero_bf, zeroB = sb("zero_bf", [128, 128], BF16)
    w_pq, wpqB = sb("w_pq", [128, 8, D], BF16)
    skf, skfB = sb("skf", [128, 8, 128], BF16)
    skbd, skbdB = sb("skbd", [128, 8, 256], BF16)
    g2bc, g2B = sb("g2bc", [128, D])
    ada, adaB = sb("ada", [128, 3 * D])
    epst, epstB = sb("epst", [128, 1])
    ssb, ssbB = sb("ssb", [128, 2048])
    wk2, wk2B = sb("wk2", [128, 2048])
    stg = [(ssb, ssbB), (wk2, wk2B)]
    Xa = sb("Xa", [128, D])
    Xe = sb("Xe", [128, D])
    tmpe, tmpeB = Xa
    OH_ENG = "pool"
    VQ = "pool"
    STEPS = 7
    NFILL = 5
    tmpf, tmpfB = sb("tmpf", [128, D])
    junk, junkB = tmpf, tmpfB
    small, smallB = sb("small", [128, 64])
    h2_bf, h2bB = sb("h2_bf", [128, D], BF16)
    h2T = [sb("h2T%d" % i, [128, 8, TG], BF16) for i in range(2)]
    qpT, qpTB = sb("qpT", [128, 8, 128], BF16)
    ts_, tsB = sb("ts", [128, 256])
    ti_, tiB = sb("ti", [128, 256], U32)
    tif, tifB = sb("tif", [128, 256])
    bs_, bsB = sb("bs", [128, 128])
    bp_, bpB = sb("bp", [128, 128], U32)
    bpf, bpfB = sb("bpf", [128, 3, 128])
    gx, gxB = sb("gx", [128, 2, 128])
    abg, abgB = sb("abg", [128, 3, 128], BF16)
    abgf, abgfB = sb("abgf", [128, 3, 128])
    abgT = [sb("abgT%d" % i, [128, 3, 128], BF16) for i in range(4)]
    Aoh = [sb("Aoh%d" % i, [128, 32, 128], BF16) for i in range(2)]
    Boh = [sb("Boh%d" % i, [128, 32, 128], BF16) for i in range(2)]
    _a0 = Aoh[0][0][:].rearrange("p n c -> p (n c)")
    crep_bf, crepB = _a0[:, 0:1024].rearrange("p (a b) -> p a b", a=8), Aoh[0][1]
    wada_bf, wadaB = _a0[:, 1024:3072].rearrange("p (a b) -> p a b", a=4), Aoh[0][1]
    bbc, bbcB = Boh[0][0][:].rearrange("p n c -> p (n c)").bitcast(F32)[:, 0:512], Boh[0][1]
    Gsb, GsbB = sb("Gsb", [128, 128, TG], BF16)
    NSL = 3
    uT_sb = [sb("uT_sb%d" % i, [128, D], BF16) for i in range(NSL)]
    v_sb = [sb("v_sb%d" % i, [128, D], BF16) for i in range(NSL)]
    ga = [sb("ga%d" % i, [128, TG]) for i in range(2)]
    GA = [sb("GA%d" % i, [128, TG], BF16) for i in range(2)]
    Gflat = Gsb[:].rearrange("p c n -> p (c n)")
    u_bf, ubfB = Gflat[:, 0:1024], Buf("u_bf")
    uT_st = [(Gflat[:, 1024 * (1 + i):1024 * (2 + i)], Buf("uT_st%d" % i)) for i in range(2)]
    v_st = [(Gflat[:, 1024 * (3 + i):1024 * (4 + i)], Buf("v_st%d" % i)) for i in range(2)]

    def dma(q, out_ap, in_ap, sbuf, R=(), W=()):
        P.dma(q, lambda e: e.dma_start(out=out_ap, in_=in_ap), sbuf, R=R, W=W)

    def mm(out_ap, lhsT, rhs, start, stop, R, W):
        P.op("pe", lambda e: e.matmul(out_ap, lhsT, rhs, start=start, stop=stop), R=R, W=W)

    def tr(out_ap, in_ap, R, W):
        P.op("pe", lambda e: e.transpose(out_ap, in_ap, ident_bf[:]), R=list(R) + [identB], W=W)

    def act(out_ap, in_ap, func, R, W, bias=None, scale=None, accum=None):
        kw = {}
        if bias is not None:
            kw["bias"] = bias
        if scale is not None:
            kw["scale"] = scale
        if accum is not None:
            kw["accum_out"] = accum
        P.op("act", lambda e: e.activation(out_ap, in_ap, func, **kw), R=R, W=W)

    def vcopy(eng, out_ap, in_ap, R, W):
        if eng == "act":
            P.op("act", lambda e: e.copy(out_ap, in_ap), R=R, W=W)
        else:
            P.op(eng, lambda e: e.tensor_copy(out_ap, in_ap), R=R, W=W)

    def tt(out_ap, a, b, op, R, W, eng="dve"):
        P.op(eng, lambda e: e.tensor_tensor(out_ap, a, b, op), R=R, W=W)

    def ts1(out_ap, a, s1, op0, R, W, eng="dve"):
        P.op(eng, lambda e: e.tensor_scalar(out_ap, a, s1, None, op0), R=R, W=W)

    def stt(out_ap, a, s_, b, op0, op1, R, W, eng="dve"):
        P.op(eng, lambda e: e.scalar_tensor_tensor(out_ap, a, s_, b, op0, op1), R=R, W=W)

    def rstd(out_ap, tmp_ap, ss_ap, n, RB, WB):
        act(tmp_ap, ss_ap, AF.Sqrt, [RB, epstB], [WB], bias=epst[:, 0:1], scale=1.0 / n)
        P.op("dve", lambda e: e.reciprocal(out_ap, tmp_ap), R=[WB], W=[WB])

    nstg = [0]

    def load_cvt(dst_ap, src_ap, shape_free, dstB, eng=None):
        i = nstg[0] % 2
        nstg[0] += 1
        st, stB = stg[i]
        nel = int(np.prod(shape_free))
        view = st[:, 0:nel]
        if len(shape_free) == 2:
            view = view.rearrange("p (a b) -> p a b", a=shape_free[0])
        dma("sp", view, src_ap, stB, W=[stB])
        vcopy(eng or ("dve" if i == 0 else "act"), dst_ap, view, [stB], [dstB])

    dma("sp", cst2[:, 0:128], cst_d[:, 0:128], cst2B, W=[cst2B])
    dma("sp", cst2[:, 128:256], cst_d[:, IOTA0:IOTA0 + 128], cst2B, W=[cst2B])
    vcopy("dve", ident_bf[:], cst2[:, 0:128], [cst2B], [identB])
    vcopy("dve", iota_bf[:], cst2[:, 128:256], [cst2B], [iotaB])
    P.op("dve", lambda e: e.memset(epst[:], 1e-6), W=[epstB])
    P.op("dve", lambda e: e.memset(skbd[:], 0.0), W=[skbdB])
    P.op("dve", lambda e: e.memset(zero_bf[:], 0.0), W=[zeroB])
    dma("sp", g2bc[:], g2_d.partition_broadcast(128), g2B, W=[g2B])
    wpq_v = wpq_d.rearrange("(kc p) n -> p kc n", p=128)
    for hh in range(4):
        load_cvt(w_pq[:, hh * 2:(hh + 1) * 2, :], wpq_v[:, hh * 2:(hh + 1) * 2, :], [2, D], wpqB)
    st0, st0B = stg[0]
    for p_ in range(2):
        dma("sp", st0[:, 0:1024].rearrange("k (h pd) -> k h pd", h=8)[:, :, p_ * 64:(p_ + 1) * 64],
            sk_d.rearrange("(h p) k d -> k h p d", p=2)[:, :, p_, :], st0B, W=[st0B])
    vcopy("dve", skf[:].rearrange("p a b -> p (a b)"), st0[:, 0:1024], [st0B], [skfB])
    for h in range(8):
        tr(bkbf(4)[:, h * 128:(h + 1) * 128], skf[:, h, :], [skfB], [bkB(4)])
    for h in range(8):
        vcopy("act", skbd[0:64, h, 0:128], bkbf(4)[0:64, h * 128:(h + 1) * 128], [bkB(4)], [skbdB])
        vcopy("act", skbd[64:128, h, 128:256], bkbf(4)[64:128, h * 128:(h + 1) * 128], [bkB(4)], [skbdB])

    for c in range(128):
        i = c % 2
        UT, UTB = uT_st[i]
        VS, VSB = v_st[i]
        dma("sp", ssb[:, 0:1024], pu_d[c * 128:(c + 1) * 128, :], ssbB, W=[ssbB])
        vcopy("dve", u_bf, ssb[:, 0:1024], [ssbB], [ubfB])
        for kc in range(8):
            tr(bkbf(4 + i)[:, kc * 128:(kc + 1) * 128], u_bf[:, kc * 128:(kc + 1) * 128], [ubfB], [bkB(4 + i)])
        vcopy("act", UT, bkbf(4 + i)[:, 0:1024], [bkB(4 + i)], [UTB])
        dma("sp", uT_d[c], UT, UTB, R=[UTB], W=[UTD])
        dma("sp", wk2[:, 0:1024], pv_d[c * 128:(c + 1) * 128, :], wk2B, W=[wk2B])
        vcopy("dve" if i == 0 else "act", VS, wk2[:, 0:1024], [wk2B], [VSB])
        dma("sp", v_d[c], VS, VSB, R=[VSB], W=[VD])
    P.barrier()

    def compute_ada(b, first_cg):
        st, stB = stg[0]
        dma("sp", st[:, 0:1024], crep_d[b], stB, W=[stB])
        vcopy("dve", crep_bf[:].rearrange("p a b -> p (a b)"), st[:, 0:1024], [stB], [crepB])
        nstg[0] = 1
        wada_v = wada_d.rearrange("(kc p) n -> p kc n", p=128)
        for ci in range(6):
            cg = first_cg + ci
            bi = 4 + ci % 2
            dma("sp", bbc[:], bada_d[0, cg * 512:(cg + 1) * 512].partition_broadcast(128), bbcB, W=[bbcB])
            for kh in range(2):
                load_cvt(wada_bf[:], wada_v[:, kh * 4:(kh + 1) * 4, cg * 512:(cg + 1) * 512], [4, 512], wadaB)
                for k4 in range(4):
                    kc = kh * 4 + k4
                    mm(bk(bi)[:], crep_bf[:, kc, :], wada_bf[:, k4, :], kc == 0, kc == 7,
                       [crepB, wadaB], [bkB(bi)])
            tt(ada[:, ci * 512:(ci + 1) * 512], bk(bi)[:], bbc[:], ALU.add, [bkB(bi), bbcB], [adaB])

    def bc_last(ap, shape):
        return ap.to_broadcast(list(shape))

    def peer_A1(b, t, tl, sl):
        X, XB = Xa
        H2T, H2TB = h2T[sl]
        ABT, ABTB = abgT[sl * 2 + tl]
        dma("sp", X[:], out_d[b, t * 128:(t + 1) * 128, :], XB, W=[XB])
        act(tmpf[:], X[:], AF.Square, [XB], [tmpfB, smallB], accum=small[:, 0:1])
        rstd(small[:, 2:3], small[:, 1:2], small[:, 0:1], D, smallB, smallB)
        stt(tmpf[:], X[:], small[:, 2:3], ada[:, D:2 * D], ALU.mult, ALU.mult, [XB, smallB, adaB], [tmpfB])
        tt(h2_bf[:], tmpf[:], ada[:, 0:D], ALU.add, [tmpfB, adaB], [h2bB])
        yield
        for kc in range(8):
            tr(bkbf(6)[:, kc * 128:(kc + 1) * 128], h2_bf[:, kc * 128:(kc + 1) * 128], [h2bB], [bkB(6)])
        vcopy("act", H2T[:, :, tl * 128:(tl + 1) * 128], bkbf(6)[:, 0:1024].rearrange("p (a b) -> p a b", a=8),
              [bkB(6)], [H2TB])
        yield
        for oc in range(8):
            bi = 6 + oc // 4
            for kc in range(8):
                mm(bk(bi)[:, (oc % 4) * 128:(oc % 4 + 1) * 128], w_pq[:, kc, oc * 128:(oc + 1) * 128],
                   H2T[:, kc, tl * 128:(tl + 1) * 128], kc == 0, kc == 7, [wpqB, H2TB], [bkB(bi)])
            yield
        for hh in range(2):
            vcopy("act", qpT[:, hh * 4:(hh + 1) * 4, :].rearrange("p a b -> p (a b)"), bk(6 + hh)[:],
                  [bkB(6 + hh)], [qpTB])
        yield
        for hq in range(2):
            for h4 in range(4):
                h = hq * 4 + h4
                mm(bk(6 + h4 // 2)[:, (h4 % 2) * 256:(h4 % 2 + 1) * 256], qpT[:, h, :], skbd[:, h, :], True, True,
                   [qpTB, skbdB], [bkB(6 + h4 // 2)])
            for q_ in range(2):
                vcopy("act", ssb[:, (hq * 2 + q_) * 512:(hq * 2 + q_ + 1) * 512], bk(6 + q_)[:], [bkB(6 + q_)], [ssbB])
            yield
        for hp in range(16):
            sl_ = slice(hp * 128, (hp + 1) * 128)
            o0 = slice(hp * 16, hp * 16 + 8)
            o1 = slice(hp * 16 + 8, hp * 16 + 16)
            P.op("dve", (lambda sl_=sl_, o0=o0: lambda e: e.max(ts_[:, o0], ssb[:, sl_]))(), R=[ssbB], W=[tsB])
            P.op("dve", (lambda sl_=sl_, o0=o0: lambda e: e.max_index(ti_[:, o0], ts_[:, o0], ssb[:, sl_]))(),
                 R=[ssbB, tsB], W=[tiB])
            P.op("dve", (lambda sl_=sl_, o0=o0: lambda e: e.match_replace(wk2[:, sl_], ts_[:, o0], ssb[:, sl_], NEG))(),
                 R=[ssbB, tsB], W=[wk2B])
            yield
            P.op("dve", (lambda sl_=sl_, o1=o1: lambda e: e.max(ts_[:, o1], wk2[:, sl_]))(), R=[wk2B], W=[tsB])
            P.op("dve", (lambda sl_=sl_, o1=o1: lambda e: e.max_index(ti_[:, o1], ts_[:, o1], wk2[:, sl_]))(),
                 R=[wk2B, tsB], W=[tiB])
            yield
        for h in range(8):
            a0 = ts_[:, (2 * h) * 16:(2 * h) * 16 + 16].rearrange("p (x o) -> p x o", o=1)
            a1 = ts_[:, (2 * h + 1) * 16:(2 * h + 1) * 16 + 16].rearrange("p (o y) -> p o y", o=1)
            tt(ssb[:, h * 256:(h + 1) * 256].rearrange("p (x y) -> p x y", x=16),
               bc_last(a0, [128, 16, 16]), bc_last(a1, [128, 16, 16]), ALU.add, [tsB], [ssbB])
            if h % 2:
                yield
        for h in range(8):
            sl_ = slice(h * 256, (h + 1) * 256)
            o0 = slice(h * 16, h * 16 + 8)
            o1 = slice(h * 16 + 8, h * 16 + 16)
            P.op("dve", (lambda sl_=sl_, o0=o0: lambda e: e.max(bs_[:, o0], ssb[:, sl_]))(), R=[ssbB], W=[bsB])
            P.op("dve", (lambda sl_=sl_, o0=o0: lambda e: e.max_index(bp_[:, o0], bs_[:, o0], ssb[:, sl_]))(),
                 R=[ssbB, bsB], W=[bpB])
            P.op("dve", (lambda sl_=sl_, o0=o0: lambda e: e.match_replace(wk2[:, sl_], bs_[:, o0], ssb[:, sl_], NEG))(),
                 R=[ssbB, bsB], W=[wk2B])
            yield
            P.op("dve", (lambda sl_=sl_, o1=o1: lambda e: e.max(bs_[:, o1], wk2[:, sl_]))(), R=[wk2B], W=[bsB])
            P.op("dve", (lambda sl_=sl_, o1=o1: lambda e: e.max_index(bp_[:, o1], bs_[:, o1], wk2[:, sl_]))(),
                 R=[wk2B, bsB], W=[bpB])
            yield
        bs3 = bs_[:].rearrange("p (h j) -> p h j", h=8)
        tt(gx[:, 1, :].rearrange("p (h j) -> p h j", h=8), bs3, bc_last(bs3[:, :, 0:1], [128, 8, 16]),
           ALU.subtract, [bsB], [gxB])
        act(gx[:, 0, :], gx[:, 1, :], AF.Exp, [gxB], [gxB])
        P.op("dve", lambda e: e.tensor_reduce(small[:, 8:16], gx[:, 0, :].rearrange("p (h j) -> p h j", h=8),
                                              AX.X, ALU.add), R=[gxB], W=[smallB])
        P.op("dve", lambda e: e.reciprocal(small[:, 16:24], small[:, 8:16]), R=[smallB], W=[smallB])
        tt(abgf[:, 2, :].rearrange("p (h j) -> p h j", h=8), gx[:, 0, :].rearrange("p (h j) -> p h j", h=8),
           bc_last(small[:, 16:24].rearrange("p (h o) -> p h o", o=1), [128, 8, 16]), ALU.mult,
           [gxB, smallB], [abgfB])
        yield
        vcopy("dve", bpf[:, 0, :], bp_[:], [bpB], [bpfB])
        vcopy("dve", tif[:], ti_[:], [tiB], [tifB])
        ts1(bpf[:, 2, :], bpf[:, 0, :], 16.0, ALU.is_ge, [bpfB], [bpfB])
        yield
        for k_ in range(2, 16):
            stt(bpf[:, 2, :], bpf[:, 0, :], 16.0 * k_, bpf[:, 2, :], ALU.is_ge, ALU.add, [bpfB], [bpfB])
            if k_ % 4 == 0:
                yield
        stt(bpf[:, 1, :], bpf[:, 2, :], -16.0, bpf[:, 0, :], ALU.mult, ALU.add, [bpfB], [bpfB])
        iota16 = cst2[:, 128:144]
        tif4 = tif[:].rearrange("p (h q x) -> p h q x", h=8, q=2)
        for which, (src_i, half) in enumerate(((2, 0), (1, 1))):
            for h in range(8):
                pv_ = bpf[:, src_i, h * 16:(h + 1) * 16].rearrange("p (j o) -> p j o", o=1)
                eqv = ssb[:, h * 256:(h + 1) * 256].rearrange("p (j x) -> p j x", j=16)
                tt(eqv, bc_last(pv_, [128, 16, 16]),
                   bc_last(iota16.rearrange("p (o x) -> p o x", o=1), [128, 16, 16]), ALU.is_equal,
                   [bpfB, cst2B], [ssbB])
                tt(wk2[:, h * 256:(h + 1) * 256].rearrange("p (j x) -> p j x", j=16), eqv,
                   bc_last(tif4[:, h, half, :].rearrange("p (o x) -> p o x", o=1), [128, 16, 16]), ALU.mult,
                   [ssbB, tifB], [wk2B])
                if h % 2:
                    yield
            P.op("dve", (lambda which=which: lambda e: e.tensor_reduce(
                abgf[:, which, :], wk2[:, 0:2048].rearrange("p (r x) -> p r x", x=16), AX.X, ALU.add))(),
                R=[wk2B], W=[abgfB])
            yield
        vcopy("dve", abg[:].rearrange("p a b -> p (a b)"), abgf[:].rearrange("p a b -> p (a b)"), [abgfB], [abgB])
        for i in range(3):
            tr(bkbf(7)[:, i * 128:(i + 1) * 128], abg[:, i, :], [abgB], [bkB(7)])
        vcopy("act", ABT[:].rearrange("p a b -> p (a b)"), bkbf(7)[:, 0:384], [bkB(7)], [ABTB])
        yield

    noh = [0]

    def peer_A2(sl):
        for tl in range(2):
            ABT, ABTB = abgT[sl * 2 + tl]
            for hs in range(4):
                n0 = hs * 32
                k = noh[0] % 2
                noh[0] += 1
                A_, AB_ = Aoh[k]
                B_, BB_ = Boh[k]
                io = bc_last(iota_bf[:].rearrange("p (o c) -> p o c", o=1), [128, 32, 128])
                aT = bc_last(ABT[:, 0, n0:n0 + 32].rearrange("p (n o) -> p n o", o=1), [128, 32, 128])
                bT = bc_last(ABT[:, 1, n0:n0 + 32].rearrange("p (n o) -> p n o", o=1), [128, 32, 128])
                gT = bc_last(ABT[:, 2, n0:n0 + 32].rearrange("p (n o) -> p n o", o=1), [128, 32, 128])
                tt(A_[:], io, aT, ALU.is_equal, [iotaB, ABTB], [AB_])
                tt(A_[:], A_[:], gT, ALU.mult, [AB_, ABTB], [AB_], eng=OH_ENG)
                tt(B_[:], io, bT, ALU.is_equal, [iotaB, ABTB], [BB_])
                for q4 in range(8):
                    bi = 6 + q4 % 2
                    for i in range(4):
                        n = q4 * 4 + i
                        mm(bk(bi)[:, i * 128:(i + 1) * 128], B_[:, n, :], A_[:, n, :], True, True,
                           [BB_, AB_], [bkB(bi)])
                    tok0 = tl * 128 + n0 + q4 * 4
                    vcopy("act", Gsb[:, :, tok0:tok0 + 4].rearrange("p c n -> p n c"),
                          bk(bi)[:].rearrange("p (n c) -> p n c", n=4), [bkB(bi)], [GsbB])

    cnt = [0]

    def peer_B(b, gi, sl, nxt):
        H2T, H2TB = h2T[sl]
        base = cnt[0]
        cnt[0] += 128

        def loads(c):
            UT, UTB = uT_sb[(base + c) % NSL]
            VS, VSB = v_sb[(base + c) % NSL]
            dma("sp", UT[:], uT_d[c], UTB, R=[UTD], W=[UTB])
            dma(VQ, VS[:], v_d[c], VSB, R=[VD], W=[VSB])

        def actmm(c):
            UT, UTB = uT_sb[(base + c) % NSL]
            bi = 4 + c % 2
            for kc in range(8):
                mm(bk(bi)[:, 0:TG], UT[:, kc * 128:(kc + 1) * 128], H2T[:, kc, :], kc == 0, kc == 7,
                   [UTB, H2TB], [bkB(bi)])

        loads(0)
        loads(1)
        actmm(0)
        for c in range(128):
            if c + 2 < 128:
                loads(c + 2)
            if c + 1 < 128:
                actmm(c + 1)
            VS, VSB = v_sb[(base + c) % NSL]
            bi = 4 + c % 2
            g_, gB_ = ga[c % 2]
            G_, GB_ = GA[c % 2]
            act(g_[:], bk(bi)[:, 0:TG], AF.Gelu, [bkB(bi)], [gB_])
            tt(G_[:], g_[:], Gsb[:, c, :], ALU.mult, [gB_, GsbB], [GB_], eng="pool")
            if 0 < c:
                for f_ in range(NFILL):
                    mm(bk(f_ % 4)[:, 0:128], zero_bf[:], iota_bf[:], False, False, [zeroB, iotaB], [bkB(f_ % 4)])
            for tl in range(2):
                for hf in range(2):
                    yb = tl * 2 + hf
                    mm(bk(yb)[:], G_[:, tl * 128:(tl + 1) * 128], VS[:, hf * 512:(hf + 1) * 512], c == 0, c == 127,
                       [GB_, VSB], [bkB(yb)])
            if nxt is not None:
                for _ in range(STEPS):
                    if next(nxt, "end") == "end":
                        nxt = None
                        break
        if nxt is not None:
            for _ in nxt:
                pass
        X, XB = Xe
        for tl in range(2):
            t = gi * 2 + tl
            dma("sp", X[:], out_d[b, t * 128:(t + 1) * 128, :], XB, W=[XB])
            for hf in range(2):
                cs = slice(hf * 512, (hf + 1) * 512)
                yb = tl * 2 + hf
                tt(tmpe[:, cs], bk(yb)[:], ada[:, 2 * D + hf * 512:2 * D + (hf + 1) * 512], ALU.mult,
                   [bkB(yb), adaB], [tmpeB])
                tt(X[:, cs], tmpe[:, cs], X[:, cs], ALU.add, [tmpeB, XB], [XB])
            dma("sp", out_d[b, t * 128:(t + 1) * 128, :], X[:], XB, R=[XB], W=[OUTB])

    def a1_group(b, gi, sl):
        for tl in range(2):
            yield from peer_A1(b, gi * 2 + tl, tl, sl)

    for b in range(NB):
        compute_ada(b, 6)
        stt(ada[:, D:2 * D], ada[:, D:2 * D], 1.0, g2bc[:], ALU.add, ALU.mult, [adaB, g2B], [adaB])
        for _ in a1_group(b, 0, 0):
            pass
        for gi in range(NG):
            sl = gi % 2
            peer_A2(sl)
            nxt = a1_group(b, gi + 1, 1 - sl) if gi + 1 < NG else None
            peer_B(b, gi, sl, nxt)
    P.barrier()
    P.emit(nc, es)
    es.close()


def _prep_inputs(inp, NB, S, core, ncores_total):
    f = lambda a: np.ascontiguousarray(np.asarray(a, dtype=np.float32))
    b0 = core * NB
    c = f(inp["c"])[b0:b0 + NB]
    crep = np.ascontiguousarray(
        np.broadcast_to(c.reshape(NB, 8, 128).transpose(0, 2, 1)[:, :, :, None], (NB, 128, 8, 128)).reshape(NB, 128, 1024))
    m = {
        "x": f(inp["x"])[b0:b0 + NB],
        "crep": crep,
        "cst": _consts(S),
        "w_ada": f(inp["w_ada"])[0],
        "b_ada": f(inp["b_ada"])[0].reshape(1, 6 * D),
        "g_norm1": f(inp["g_norm1"])[0],
        "w_in": f(inp["w_in"])[0],
        "g_q": f(inp["g_q"])[0],
        "g_k": f(inp["g_k"])[0],
        "g_ik": f(inp["g_ik"])[0],
        "w_pool_grp": f(inp["w_pool_grp"])[0],
        "s_pool": f(inp["s_pool"])[0].reshape(512),
        "w_up_attn": f(inp["w_up_attn"])[0],
        "w_up_pool": f(inp["w_up_pool"])[0],
        "w_out": f(inp["w_out"])[0],
        "g_norm2": f(inp["g_norm2"])[0],
        "w_peer_q": f(inp["w_peer_q"])[0],
        "peer_subkeys": f(inp["peer_subkeys"])[0].reshape(16, 128, 64),
        "peer_u": f(inp["peer_u"])[0],
        "peer_v": f(inp["peer_v"])[0],
    }
    return m


def kernel(**inputs):
    x = np.asarray(inputs["x"])
    B, S, _ = x.shape
    NB = B // NCORES
    NSEL = min(256, S // 4)
    nc = build(NB, S, NSEL)
    in_maps = [_prep_inputs(inputs, NB, S, i, NCORES) for i in range(NCORES)]
    res = run_bass_kernel_spmd(nc, in_maps, core_ids=list(range(NCORES)))
    out = np.concatenate([np.asarray(r["out"]) for r in res.results], axis=0)
    return out.astype(np.float32)
```

```python
from contextlib import ExitStack
import numpy as np
import concourse.bass as bass
import concourse.mybir as mybir
from concourse.bass_utils import run_bass_kernel_spmd

F32 = mybir.dt.float32
BF16 = mybir.dt.bfloat16
U32 = mybir.dt.uint32
ALU = mybir.AluOpType
AF = mybir.ActivationFunctionType
AX = mybir.AxisListType

D = 1024
NCORES = 8
INW = 3524
NEG = -1.0e30
C_Q, C_K, C_V, C_IQ, C_IK, C_IW, C_P, C_G = 0, 512, 576, 640, 896, 960, 964, 1476
POOLW = (2, 4, 8, 16)
NEXP = 16384


class Buf:
    __slots__ = ("name", "lw", "rd", "sem", "cnt")

    def __init__(self, name):
        self.name = name
        self.lw = None
        self.rd = {}
        self.sem = None
        self.cnt = 0


class Prog:
    ENG = ("pe", "act", "dve", "pool", "sp")

    def __init__(self):
        self.streams = {e: [] for e in self.ENG}
        self.n = {e: 0 for e in self.ENG}
        self.waited = {e: {} for e in self.ENG}
        self.dbufs = []

    def _need(self, eng, ev, waits):
        if ev is None:
            return
        key, val = ev
        if key[0] == "e" and key[1] == eng and eng == "pe":
            return
        w = self.waited[eng]
        if w.get(key, 0) >= val:
            return
        w[key] = val
        waits.append((key, val))

    def _deps(self, eng, R, W, waits):
        for b in R:
            self._need(eng, b.lw, waits)
        for b in W:
            self._need(eng, b.lw, waits)
            for k, v in b.rd.items():
                self._need(eng, (k, v), waits)

    def op(self, eng, fn, R=(), W=()):
        waits = []
        self._deps(eng, R, W, waits)
        self.n[eng] += 1
        key = ("e", eng)
        val = self.n[eng]
        for b in R:
            if b.rd.get(key, 0) < val:
                b.rd[key] = val
        for b in W:
            b.lw = (key, val)
            b.rd = {}
        self.streams[eng].append((waits, fn, None))

    def dma(self, q, fn, sb, R=(), W=()):
        waits = []
        self._deps(q, R, W, waits)
        if sb.cnt > 0:
            self._need(q, (("d", sb), 16 * sb.cnt), waits)
        if sb.sem is None:
            sb.sem = True
            self.dbufs.append(sb)
        sb.cnt += 1
        key = ("d", sb)
        val = 16 * sb.cnt
        for b in R:
            if b.rd.get(key, 0) < val:
                b.rd[key] = val
        for b in W:
            b.lw = (key, val)
            b.rd = {}
        self.streams[q].append((waits, fn, sb))

    def barrier(self):
        for e in self.ENG:
            waits = []
            for o in self.ENG:
                if o != e and self.n[o] > 0:
                    self._need(e, (("e", o), self.n[o]), waits)
            for b in self.dbufs:
                if b.cnt > 0:
                    self._need(e, (("d", b), 16 * b.cnt), waits)
            if e == "pe" and self.n["pe"] > 0:
                pass
            self.streams[e].append((waits, None, None))

    def final_wait(self, q, bufs):
        waits = []
        for b in bufs:
            self._need(q, b.lw, waits)
        self.streams[q].append((waits, None, None))

    def emit(self, nc, es):
        esem = {e: es.enter_context(nc.semaphore("S_" + e)) for e in self.ENG if self.n[e] > 0}
        for i, b in enumerate(self.dbufs):
            b.sem = es.enter_context(nc.semaphore("D%d" % i))

        def sem_of(key):
            return esem[key[1]] if key[0] == "e" else key[1].sem

        streams = self.streams

        def run(engname, eng):
            mysem = esem.get(engname)
            for waits, fn, sb in streams[engname]:
                for key, val in waits:
                    eng.wait_ge(sem_of(key), val)
                if fn is None:
                    continue
                ins = fn(eng)
                if sb is not None:
                    ins.then_inc(sb.sem, 16)
                else:
                    ins.then_inc(mysem, 1)

        with nc.Block() as block:
            if streams["pe"]:
                @block.tensor
                def _(e):
                    run("pe", e)
            if streams["act"]:
                @block.scalar
                def _(e):
                    run("act", e)
            if streams["dve"]:
                @block.vector
                def _(e):
                    run("dve", e)
            if streams["pool"]:
                @block.gpsimd
                def _(e):
                    run("pool", e)
            if streams["sp"]:
                @block.sync
                def _(e):
                    run("sp", e)


def _consts(S):
    ident = np.eye(128, dtype=np.float32)
    bands = np.zeros((128, 12, 128), np.float32)
    n = np.arange(128)
    for g, w in enumerate(POOLW):
        cur = ((n[:, None] <= n[None, :]) & (n[:, None] > n[None, :] - w)).astype(np.float32)
        bands[:, 0 * 4 + g, :] = cur / w - ident
        prv = ((n[:, None] - 128) > (n[None, :] - w)).astype(np.float32)
        bands[:, 1 * 4 + g, :] = prv / w
        cnt = np.minimum(w, n + 1).astype(np.float32)
        bands[:, 2 * 4 + g, :] = cur / cnt[None, :] - ident
    eps = np.broadcast_to(-(np.arange(S, dtype=np.float32)) * 1e-12, (128, S))
    iota = np.broadcast_to(np.arange(128, dtype=np.float32), (128, 128))
    return np.ascontiguousarray(
        np.concatenate([ident, bands.reshape(128, 12 * 128), eps, iota], axis=1).astype(np.float32))


def build(NB, S, NSEL, do_peer=True, dbg=None):
    NT = S // 128
    nc = bass.Bass("TRN2", target_bir_lowering=False)
    P = Prog()
    es = ExitStack()

    def din(name, shape, dt=F32):
        return nc.dram_tensor(name, list(shape), dt, kind="ExternalInput").ap()

    CW = 128 + 12 * 128 + S + 128
    x_d = din("x", [NB, S, D])
    crep_d = din("crep", [NB, 128, 1024])
    cst_d = din("cst", [128, CW])
    wada_d = din("w_ada", [D, 6 * D])
    bada_d = din("b_ada", [1, 6 * D])
    g1_d = din("g_norm1", [D])
    win_d = din("w_in", [D, INW])
    gq_d = din("g_q", [64])
    gk_d = din("g_k", [64])
    gik_d = din("g_ik", [64])
    wgrp_d = din("w_pool_grp", [4, 128, 128])
    spool_d = din("s_pool", [512])
    wua_d = din("w_up_attn", [512, D])
    wup_d = din("w_up_pool", [512, D])
    wout_d = din("w_out", [D, D])
    if do_peer:
        g2_d = din("g_norm2", [D])
        wpq_d = din("w_peer_q", [D, D])
        sk_d = din("peer_subkeys", [16, 128, 64])
        pu_d = din("peer_u", [NEXP, D])
        pv_d = din("peer_v", [NEXP, D])
    out_d = nc.dram_tensor("out", [NB, S, D], F32, kind="ExternalOutput").ap()
    dbg_d = None
    if dbg is not None:
        dbg_d = nc.dram_tensor("dbg", list(dbg), F32, kind="ExternalOutput").ap()

    def sb(name, shape, dt=F32):
        t = es.enter_context(nc.sbuf_tensor("s_" + name, list(shape), dt))
        return t, Buf(name)

    def ps(name):
        t = es.enter_context(nc.psum_tensor(name, [128, 512], F32))
        return t, Buf(name)

    OUTB = Buf("out_dram")

    cst, cstB = sb("cst_sb", [128, CW])
    ident_bf, identB = sb("ident_bf", [128, 128], BF16)
    irep, irepB = sb("irep", [128, 512], BF16)
    band, bandB = sb("band", [128, 12, 128], BF16)
    EPS0 = 128 + 12 * 128
    IOTA0 = EPS0 + S
    w_in, winB = sb("w_in", [128, 8, INW], BF16)
    w_out, woutB = sb("w_out", [128, 8, D], BF16)
    w_ua, wuaB = sb("w_ua", [128, 4, D], BF16)
    w_up, wupB = sb("w_up", [128, 4, D], BF16)
    w_grp, wgrpB = sb("w_grp", [128, 4, 128], BF16)
    g1bc, g1B = sb("g1bc", [128, D])
    gqbc, gqB = sb("gqbc", [128, 64])
    gkbc, gkB = sb("gkbc", [128, 64])
    gikbc, gikB = sb("gikbc", [128, 64])
    bbc, bbcB = sb("bbc", [128, 512])
    epst, epstB = sb("epst", [128, 1])
    stg = [sb("stg%d" % i, [128, 2048]) for i in range(2)]
    ada, adaB = sb("ada", [128, 3 * D])
    crep_bf, crepB = sb("crep_bf", [128, 8, 128], BF16)
    wada_bf, wadaB = sb("wada_bf", [128, 4, 512], BF16)
    kik, kikB = sb("kik", [128, S], BF16)
    kT, kTB = kik, kikB
    vaug, vaugB = sb("vaug", [128, NT, 65], BF16)
    xt = [sb("xt0", [128, D])] * 2
    junk, junkB = sb("junk", [128, D])
    tmpf, tmpfB = sb("tmpf", [128, D])
    tmpg, tmpgB = junk[:, 512:1024], junkB
    small, smallB = sb("small", [128, 64])
    h_bf, hbfB = sb("h_bf", [128, D], BF16)
    hT, hTB = sb("hT", [128, 8, 128], BF16)
    zq_f, zqB = sb("zq_f", [128, 512])
    qn_bf, qnB = sb("qn_bf", [128, 512], BF16)
    qT, qTB = sb("qT", [64, 1024], BF16)
    zb_f, zbB = sb("zb_f", [128, 452])
    knik, knikB = sb("knik", [128, 128], BF16)
    iq_pad, iqbB = sb("iq_pad", [128, 4, 128], BF16)
    iqT, iqTB = sb("iqT", [128, 4, 128], BF16)
    zp = [sb("zp%d" % i, [128, 512], BF16) for i in range(2)]
    gates, gatesB = sb("gates", [128, 2048], BF16)
    rbuf = [sb("rbuf%d" % i, [128, 512]) for i in range(2)] * 2
    sc, scB = stg[0]
    wk, wkB = stg[1]
    m8, m8B = sb("m8", [128, 8])
    Mb, MbB = sb("Mb", [128, S], BF16)
    pexp = [sb("pexp%d" % i, [128, 1024], BF16) for i in range(2)]
    attn, attnB = sb("attn", [128, 512], BF16)
    attnT, attnTB = sb("attnT", [128, 4, 128], BF16)
    mixT, mixTB = sb("mixT", [128, 4, 128], BF16)
    poolT, poolTB = sb("poolT", [128, 4, 128], BF16)
    m_bf, mbfB = sb("m_bf", [128, D], BF16)
    mT, mTB = sb("mT", [128, 8, 128], BF16)
    x1, x1B = tmpf, tmpfB

    bank = [ps("bank%d" % i) for i in range(8)]

    def bk(i):
        return bank[i][0]

    def bkB(i):
        return bank[i][1]

    def bkbf(i):
        return bank[i][0][:].bitcast(BF16)

    def dma(q, out_ap, in_ap, sbuf, R=(), W=()):
        P.dma(q, lambda e: e.dma_start(out=out_ap, in_=in_ap), sbuf, R=R, W=W)

    def mm(out_ap, lhsT, rhs, start, stop, R, W):
        P.op("pe", lambda e: e.matmul(out_ap, lhsT, rhs, start=start, stop=stop), R=R, W=W)

    def tr(out_ap, in_ap, R, W):
        P.op("pe", lambda e: e.transpose(out_ap, in_ap, ident_bf[:]), R=list(R) + [identB], W=W)

    def act(out_ap, in_ap, func, R, W, bias=None, scale=None, accum=None):
        kw = {}
        if bias is not None:
            kw["bias"] = bias
        if scale is not None:
            kw["scale"] = scale
        if accum is not None:
            kw["accum_out"] = accum
        P.op("act", lambda e: e.activation(out_ap, in_ap, func, **kw), R=R, W=W)

    def vcopy(eng, out_ap, in_ap, R, W):
        if eng == "act":
            P.op("act", lambda e: e.copy(out_ap, in_ap), R=R, W=W)
        else:
            P.op(eng, lambda e: e.tensor_copy(out_ap, in_ap), R=R, W=W)

    def tt(out_ap, a, b, op, R, W, eng="dve"):
        P.op(eng, lambda e: e.tensor_tensor(out_ap, a, b, op), R=R, W=W)

    def ts(out_ap, a, s1, s2, op0, op1, R, W, eng="dve", accum=None):
        if op1 is None:
            P.op(eng, lambda e: e.tensor_scalar(out_ap, a, s1, None, op0), R=R, W=W)
        elif accum is None:
            P.op(eng, lambda e: e.tensor_scalar(out_ap, a, s1, s2, op0, op1), R=R, W=W)
        else:
            P.op(eng, lambda e: e.tensor_scalar(out_ap, a, s1, s2, op0, op1, accum), R=R, W=W)

    def stt(out_ap, a, s, b, op0, op1, R, W, eng="dve"):
        P.op(eng, lambda e: e.scalar_tensor_tensor(out_ap, a, s, b, op0, op1), R=R, W=W)

    def rstd(out_ap, tmp_ap, ss_ap, n, RB, WB):
        act(tmp_ap, ss_ap, AF.Sqrt, [RB, epstB], [WB], bias=epst[:, 0:1], scale=1.0 / n)
        P.op("dve", lambda e: e.reciprocal(out_ap, tmp_ap), R=[WB], W=[WB])

    dma("sp", cst[:], cst_d, cstB, W=[cstB])
    vcopy("dve", ident_bf[:], cst[:, 0:128], [cstB], [identB])
    for i in range(4):
        vcopy("dve", irep[:, i * 128:(i + 1) * 128], cst[:, 0:128], [cstB], [irepB])
    vcopy("dve", band[:].rearrange("p a b -> p (a b)"), cst[:, 128:128 + 1536], [cstB], [bandB])
    P.op("dve", lambda e: e.memset(iq_pad[:], 0.0), W=[iqbB])
    P.op("dve", lambda e: e.memset(epst[:], 1e-6), W=[epstB])
    P.op("dve", lambda e: e.memset(vaug[:, :, 64:65], 1.0), W=[vaugB])
    dma("sp", g1bc[:], g1_d.partition_broadcast(128), g1B, W=[g1B])
    dma("sp", gqbc[:], gq_d.partition_broadcast(128), gqB, W=[gqB])
    dma("sp", gkbc[:], gk_d.partition_broadcast(128), gkB, W=[gkB])
    dma("sp", gikbc[:], gik_d.partition_broadcast(128), gikB, W=[gikB])

    nstg = [0]

    def load_cvt(dst_ap, src_ap, shape_free, dstB, eng=None):
        i = nstg[0] % 2
        nstg[0] += 1
        st, stB = stg[i]
        nel = int(np.prod(shape_free))
        view = st[:, 0:nel]
        if len(shape_free) == 2:
            view = view.rearrange("p (a b) -> p a b", a=shape_free[0])
        dma("sp", view, src_ap, stB, W=[stB])
        vcopy(eng or ("dve" if i == 0 else "act"), dst_ap, view, [stB], [dstB])

    win_v = win_d.rearrange("(kc p) n -> p kc n", p=128)
    for kc in range(8):
        load_cvt(w_in[:, kc, 0:2048], win_v[:, kc, 0:2048], [2048], winB)
        load_cvt(w_in[:, kc, 2048:INW], win_v[:, kc, 2048:INW], [INW - 2048], winB)
    wout_v = wout_d.rearrange("(kc p) n -> p kc n", p=128)
    for hh in range(4):
        load_cvt(w_out[:, hh * 2:(hh + 1) * 2, :], wout_v[:, hh * 2:(hh + 1) * 2, :], [2, D], woutB)
    wua_v = wua_d.rearrange("(kc p) n -> p kc n", p=128)
    wup_v = wup_d.rearrange("(kc p) n -> p kc n", p=128)
    for hh in range(2):
        load_cvt(w_ua[:, hh * 2:(hh + 1) * 2, :], wua_v[:, hh * 2:(hh + 1) * 2, :], [2, D], wuaB)
        load_cvt(w_up[:, hh * 2:(hh + 1) * 2, :], wup_v[:, hh * 2:(hh + 1) * 2, :], [2, D], wupB)
    st0, st0B = stg[0]
    st1, st1B = stg[1]
    dma("sp", st0[:, 0:512].rearrange("p (g d) -> p g d", g=4), wgrp_d.rearrange("g c d -> c g d"), st0B, W=[st0B])
    dma("sp", st1[:, 0:512], spool_d.partition_broadcast(128), st1B, W=[st1B])
    tt(w_grp[:].rearrange("p g d -> p (g d)"), st0[:, 0:512], st1[:, 0:512], ALU.mult, [st0B, st1B], [wgrpB])
    nstg[0] = 0

    def compute_ada(b, first_cg):
        st, stB = stg[0]
        dma("sp", st[:, 0:1024], crep_d[b], stB, W=[stB])
        vcopy("dve", crep_bf[:].rearrange("p a b -> p (a b)"), st[:, 0:1024], [stB], [crepB])
        nstg[0] = 1
        wada_v = wada_d.rearrange("(kc p) n -> p kc n", p=128)
        for ci in range(6):
            cg = first_cg + ci
            bi = ci % 2
            dma("sp", bbc[:], bada_d[0, cg * 512:(cg + 1) * 512].partition_broadcast(128), bbcB, W=[bbcB])
            for kh in range(2):
                load_cvt(wada_bf[:], wada_v[:, kh * 4:(kh + 1) * 4, cg * 512:(cg + 1) * 512], [4, 512], wadaB)
                for k4 in range(4):
                    kc = kh * 4 + k4
                    mm(bk(bi)[:], crep_bf[:, kc, :], wada_bf[:, k4, :], kc == 0, kc == 7,
                       [crepB, wadaB], [bkB(bi)])
            tt(ada[:, ci * 512:(ci + 1) * 512], bk(bi)[:], bbc[:], ALU.add, [bkB(bi), bbcB], [adaB])

    def ada_sl(i):
        return ada[:, i * D:(i + 1) * D]

    def rms_small(src_ap, width, gbc, gB, out_bf_ap, outB, srcB):
        tt(junk[:, 0:width], src_ap, src_ap, ALU.mult, [srcB], [junkB])
        P.op("dve", lambda e: e.tensor_reduce(small[:, 32:33], junk[:, 0:width], AX.X, ALU.add),
             R=[junkB], W=[smallB])
        rstd(small[:, 34:35], small[:, 33:34], small[:, 32:33], width, smallB, smallB)
        stt(out_bf_ap, src_ap, small[:, 34:35], gbc[:, 0:width], ALU.mult, ALU.mult,
            [srcB, smallB, gB], [outB])

    import os as _os
    _stage = int(_os.environ.get("KSTAGE", "99"))

    def token_tile(b, t):
        X, XB = xt[t % 2]
        dma("sp", X[:], x_d[b, t * 128:(t + 1) * 128, :], XB, W=[XB])
        act(junk[:], X[:], AF.Square, [XB], [junkB, smallB], accum=small[:, 0:1])
        rstd(small[:, 2:3], small[:, 1:2], small[:, 0:1], D, smallB, smallB)
        stt(tmpf[:], X[:], small[:, 2:3], ada_sl(1), ALU.mult, ALU.mult, [XB, smallB, adaB], [tmpfB])
        tt(h_bf[:], tmpf[:], ada_sl(0), ALU.add, [tmpfB, adaB], [hbfB])
        for kc in range(8):
            tr(bkbf(7)[:, kc * 128:(kc + 1) * 128], h_bf[:, kc * 128:(kc + 1) * 128], [hbfB], [bkB(7)])
        vcopy("act", hT[:].rearrange("p a b -> p (a b)"), bkbf(7)[:, 0:1024], [bkB(7)], [hTB])

        def zgroup(bi, c0, w):
            for kc in range(8):
                mm(bk(bi)[:, 0:w], hT[:, kc, :], w_in[:, kc, c0:c0 + w], kc == 0, kc == 7,
                   [hTB, winB], [bkB(bi)])

        if _stage < 3:
            return
        zgroup(0, C_Q, 512)
        vcopy("act", zq_f[:], bk(0)[:], [bkB(0)], [zqB])
        tt(junk[:, 0:512], zq_f[:], zq_f[:], ALU.mult, [zqB], [junkB])
        P.op("dve", lambda e: e.tensor_reduce(small[:, 8:16], junk[:, 0:512].rearrange("p (h d) -> p h d", h=8),
                                              AX.X, ALU.add), R=[junkB], W=[smallB])
        rstd(small[:, 24:32], small[:, 16:24], small[:, 8:16], 64, smallB, smallB)
        for h in range(8):
            stt(qn_bf[:, h * 64:(h + 1) * 64], zq_f[:, h * 64:(h + 1) * 64], small[:, 24 + h:25 + h],
                gqbc[:], ALU.mult, ALU.mult, [zqB, smallB, gqB], [qnB])
        for h in range(8):
            tr(bkbf(6)[0:64, h * 128:(h + 1) * 128], qn_bf[:, h * 64:(h + 1) * 64], [qnB], [bkB(6)])
        vcopy("act", qT[:], bkbf(6)[0:64, 0:1024], [bkB(6)], [qTB])
        if _stage < 4:
            return
        zgroup(1, C_K, 452)
        vcopy("act", zb_f[:], bk(1)[:, 0:452], [bkB(1)], [zbB])
        rms_small(zb_f[:, 0:64], 64, gkbc, gkB, knik[:, 0:64], knikB, zbB)
        rms_small(zb_f[:, 384:448], 64, gikbc, gikB, knik[:, 64:128], knikB, zbB)
        vcopy("dve", vaug[:, t, 0:64], zb_f[:, 64:128], [zbB], [vaugB])
        vcopy("dve", iq_pad[:, :, 64:128], zb_f[:, 128:384].rearrange("p (h d) -> p h d", h=4), [zbB], [iqbB])
        tr(bkbf(7)[:, 0:128], knik[:], [knikB], [bkB(7)])
        for h in range(4):
            tr(bkbf(7)[:, 128 + h * 128:128 + (h + 1) * 128], iq_pad[:, h, :], [iqbB], [bkB(7)])
        vcopy("act", kik[:, t * 128:(t + 1) * 128], bkbf(7)[:, 0:128], [bkB(7)], [kikB])
        vcopy("act", iqT[:].rearrange("p a b -> p (a b)"), bkbf(7)[:, 128:640], [bkB(7)], [iqTB])
        if _stage < 5:
            return
        ZP, ZPB = zp[t % 2]
        ZPp, ZPpB = zp[(t + 1) % 2]
        zgroup(2, C_P, 512)
        vcopy("act", ZP[:], bk(2)[:], [bkB(2)], [ZPB])
        for gi in range(4):
            bi = (3 + gi) % 4
            zgroup(bi, C_G + gi * 512, 512)
            act(gates[:, gi * 512:(gi + 1) * 512], bk(bi)[:], AF.Sigmoid, [bkB(bi)], [gatesB])

        if _stage < 6:
            return
        L = 128 * (t + 1)
        ngrp = (L + 511) // 512
        iw = zb_f[:, 448:452]
        for kg in range(ngrp):
            k0 = kg * 512
            w = min(512, L - k0)
            for h in range(4):
                mm(bk(h)[:, 0:w], iqT[64:128, h, :], kik[64:128, k0:k0 + w], True, True, [iqTB, kikB], [bkB(h)])
            for h in range(4):
                R_, RB_ = rbuf[h]
                act(R_[:, 0:w], bk(h)[:, 0:w], AF.Relu, [bkB(h)], [RB_])
                if h == 0:
                    stt(sc[:, k0:k0 + w], R_[:, 0:w], iw[:, 0:1], cst[:, EPS0 + k0:EPS0 + k0 + w],
                        ALU.mult, ALU.add, [RB_, zbB, cstB], [scB])
                else:
                    stt(sc[:, k0:k0 + w], R_[:, 0:w], iw[:, h:h + 1], sc[:, k0:k0 + w],
                        ALU.mult, ALU.add, [RB_, zbB, scB], [scB])
        if _stage < 7:
            return
        P.op("dve", lambda e: e.memset(sc[0:64, L - 64:L], NEG), W=[scB])
        if L - 64 >= NSEL:
            src = sc
            nr = NSEL // 8
            for r in range(nr):
                P.op("dve", (lambda s_: lambda e: e.max(m8[:], s_[:, 0:L]))(src), R=[scB, wkB], W=[m8B])
                if r < nr - 1:
                    P.op("dve", (lambda s_: lambda e: e.match_replace(wk[:, 0:L], m8[:], s_[:, 0:L], NEG))(src),
                         R=[scB, wkB, m8B], W=[wkB])
                    src = wk
            thr = m8[:, 7:8]
        else:
            P.op("dve", lambda e: e.memset(m8[:], -1.0e29), W=[m8B])
            thr = m8[:, 7:8]
        ts(Mb[:, 0:L], sc[:, 0:L], thr, -30000.0, ALU.is_lt, ALU.mult, [scB, m8B], [MbB])

        if _stage < 8:
            return
        def logits(j):
            lb = (0, 1) if j % 2 == 0 else (2, 3)
            for hf in range(2):
                bi = lb[hf]
                mm(bk(bi)[:], kik[0:64, j * 128:(j + 1) * 128], qT[:, hf * 512:(hf + 1) * 512], True, False,
                   [kTB, qTB], [bkB(bi)])
                mm(bk(bi)[:], Mb[:, j * 128:(j + 1) * 128], irep[:], False, True, [MbB, irepB], [bkB(bi)])

        logits(0)
        for j in range(t + 1):
            if j + 1 <= t:
                logits(j + 1)
            lb = (0, 1) if j % 2 == 0 else (2, 3)
            PX, PXB = pexp[j % 2]
            for hf in range(2):
                bi = lb[hf]
                act(PX[:, hf * 512:(hf + 1) * 512], bk(bi)[:], AF.Exp, [bkB(bi)], [PXB], scale=0.125)
            for h in range(8):
                ob = 4 + h // 4
                c0 = (h % 4) * 65
                mm(bk(ob)[:, c0:c0 + 65], PX[:, h * 128:(h + 1) * 128], vaug[:, j, :], (j == 0 and h % 4 == 0), (j == t and h % 4 == 3),
                   [PXB, vaugB], [bkB(ob)])
        if _stage < 9:
            return
        for hb in range(2):
            ov = bk(4 + hb)[:, 0:260].rearrange("p (h c) -> p h c", h=4)
            P.op("dve", (lambda o_, hb_: lambda e: e.reciprocal(small[:, 40 + 4 * hb_:44 + 4 * hb_].rearrange("p (h o) -> p h o", o=1),
                                                               o_[:, :, 64:65]))(ov, hb),
                 R=[bkB(4 + hb)], W=[smallB])
            tt(attn[:, hb * 256:(hb + 1) * 256].rearrange("p (h c) -> p h c", h=4), ov[:, :, 0:64],
               small[:, 40 + 4 * hb:44 + 4 * hb].rearrange("p (h o) -> p h o", o=1).to_broadcast([128, 4, 64]),
               ALU.mult, [bkB(4 + hb), smallB], [attnB])
        for c in range(4):
            tr(bkbf(6)[:, c * 128:(c + 1) * 128], attn[:, c * 128:(c + 1) * 128], [attnB], [bkB(6)])
        vcopy("act", attnT[:].rearrange("p a b -> p (a b)"), bkbf(6)[:, 0:512], [bkB(6)], [attnTB])
        for hf in range(2):
            for c in range(4):
                mm(bk(hf)[:], attnT[:, c, :], w_ua[:, c, hf * 512:(hf + 1) * 512], c == 0, c == 3,
                   [attnTB, wuaB], [bkB(hf)])
        if _stage < 10:
            return
        for g in range(4):
            kind = 2 if t == 0 else 0
            mm(bk(6)[:, g * 128:(g + 1) * 128], ZP[:, g * 128:(g + 1) * 128], band[:, kind * 4 + g, :], True, t == 0,
               [ZPB, bandB], [bkB(6)])
            if t > 0:
                mm(bk(6)[:, g * 128:(g + 1) * 128], ZPp[:, g * 128:(g + 1) * 128], band[:, 4 + g, :], False, True,
                   [ZPpB, bandB], [bkB(6)])
        vcopy("act", mixT[:].rearrange("p a b -> p (a b)"), bk(6)[:], [bkB(6)], [mixTB])
        for g in range(4):
            mm(bk(7)[:, g * 128:(g + 1) * 128], w_grp[:, g, :], mixT[:, g, :], True, True, [wgrpB, mixTB], [bkB(7)])
        vcopy("act", poolT[:].rearrange("p a b -> p (a b)"), bk(7)[:], [bkB(7)], [poolTB])
        for hf in range(2):
            for g in range(4):
                mm(bk(2 + hf)[:], poolT[:, g, :], w_up[:, g, hf * 512:(hf + 1) * 512], g == 0, g == 3,
                   [poolTB, wupB], [bkB(2 + hf)])
        if _stage < 11:
            return
        for hf in range(2):
            cs = slice(hf * 512, (hf + 1) * 512)
            tt(tmpf[:, cs], bk(hf)[:], gates[:, hf * 512:(hf + 1) * 512], ALU.mult, [bkB(hf), gatesB], [tmpfB])
            tt(tmpg[:], bk(2 + hf)[:], gates[:, 1024 + hf * 512:1024 + (hf + 1) * 512], ALU.mult,
               [bkB(2 + hf), gatesB], [tmpgB])
            tt(m_bf[:, cs], tmpf[:, cs], tmpg[:], ALU.add, [tmpfB, tmpgB], [mbfB])
        for kc in range(8):
            tr(bkbf(6)[:, kc * 128:(kc + 1) * 128], m_bf[:, kc * 128:(kc + 1) * 128], [mbfB], [bkB(6)])
        vcopy("act", mT[:].rearrange("p a b -> p (a b)"), bkbf(6)[:, 0:1024], [bkB(6)], [mTB])
        for hf in range(2):
            for kc in range(8):
                mm(bk(4 + hf)[:], mT[:, kc, :], w_out[:, kc, hf * 512:(hf + 1) * 512], kc == 0, kc == 7,
                   [mTB, woutB], [bkB(4 + hf)])
        for hf in range(2):
            cs = slice(hf * 512, (hf + 1) * 512)
            tt(tmpf[:, cs], bk(4 + hf)[:], ada[:, 2 * D + hf * 512:2 * D + (hf + 1) * 512], ALU.mult,
               [bkB(4 + hf), adaB], [tmpfB])
            tt(x1[:, cs], tmpf[:, cs], X[:, cs], ALU.add, [tmpfB, XB], [x1B])
        dma("sp", out_d[b, t * 128:(t + 1) * 128, :], x1[:], x1B, R=[x1B], W=[OUTB])

    for b in range(NB):
        if _stage < 1:
            break
        compute_ada(b, 0)
        if _stage < 2:
            break
        stt(ada_sl(1), ada_sl(1), 1.0, g1bc[:], ALU.add, ALU.mult, [adaB, g1B], [adaB])
        for t in range(NT):
            token_tile(b, t)

    P.final_wait("sp", [OUTB])
    P.emit(nc, es)
    es.close()
    if do_peer:
        nc.all_engine_barrier()
        _phase_p(nc, NB, S, dict(crep=crep_d, cst=cst_d, w_ada=wada_d, b_ada=bada_d, g2=g2_d, wpq=wpq_d,
                                 sk=sk_d, pu=pu_d, pv=pv_d, out=out_d, IOTA0=IOTA0))
    return nc


def _phase_p(nc, NB, S, dr):
    TG = 256
    NG = S // TG
    P = Prog()
    es = ExitStack()
    crep_d, cst_d, wada_d, bada_d, g2_d = dr["crep"], dr["cst"], dr["w_ada"], dr["b_ada"], dr["g2"]
    wpq_d, sk_d, pu_d, pv_d, out_d, IOTA0 = dr["wpq"], dr["sk"], dr["pu"], dr["pv"], dr["out"], dr["IOTA0"]
    uT_d = nc.dram_tensor("uT_scr", [128, 128, 1024], BF16, kind="Internal").ap()
    v_d = nc.dram_tensor("v_scr", [128, 128, 1024], BF16, kind="Internal").ap()
    UTD, VD, OUTB = Buf("uT_d"), Buf("v_d"), Buf("out2")

    def sb(name, shape, dt=F32):
        t = es.enter_context(nc.sbuf_tensor("p_" + name, list(shape), dt))
        return t, Buf(name)

    bank = []
    for i in range(8):
        t_ = es.enter_context(nc.psum_tensor("pbank%d" % i, [128, 512], F32))
        bank.append((t_, Buf("pbank%d" % i)))

    def bk(i):
        return bank[i][0]

    def bkB(i):
        return bank[i][1]

    def bkbf(i):
        return bank[i][0][:].bitcast(BF16)

    cst2, cst2B = sb("cst2", [128, 256])
    ident_bf, identB = sb("ident_bf", [128, 128], BF16)
    iota_bf, iotaB = sb("iota_bf", [128, 128], BF16)
    w_pq, wpqB = sb("w_pq", [128, 8, D], BF16)
    skf, skfB = sb("skf", [128, 8, 128], BF16)
    skbd, skbdB = sb("skbd", [128, 8, 256], BF16)
    g2bc, g2B = sb("g2bc", [128, D])
    ada, adaB = sb("ada", [128, 3 * D])
    epst, epstB = sb("epst", [128, 1])
    ssb, ssbB = sb("ssb", [128, 2048])
    wk2, wk2B = sb("wk2", [128, 2048])
    stg = [(ssb, ssbB), (wk2, wk2B)]
    Xa = sb("Xa", [128, D])
    Xe = sb("Xe", [128, D])
    tmpe, tmpeB = Xa
    OH_ENG = "pool"
    VQ = "pool"
    STEPS = 2
    tmpf, tmpfB = sb("tmpf", [128, D])
    junk, junkB = tmpf, tmpfB
    small, smallB = sb("small", [128, 64])
    h2_bf, h2bB = sb("h2_bf", [128, D], BF16)
    h2T = [sb("h2T%d" % i, [128, 8, TG], BF16) for i in range(2)]
    qpT, qpTB = sb("qpT", [128, 8, 128], BF16)
    ts_, tsB = sb("ts", [128, 256])
    ti_, tiB = sb("ti", [128, 256], U32)
    tif, tifB = sb("tif", [128, 256])
    bs_, bsB = sb("bs", [128, 128])
    bp_, bpB = sb("bp", [128, 128], U32)
    bpf, bpfB = sb("bpf", [128, 3, 128])
    gx, gxB = sb("gx", [128, 2, 128])
    abg, abgB = sb("abg", [128, 3, 128], BF16)
    abgf, abgfB = sb("abgf", [128, 3, 128])
    abgT = [sb("abgT%d" % i, [128, 3, 128], BF16) for i in range(4)]
    Aoh = [sb("Aoh%d" % i, [128, 32, 128], BF16) for i in range(2)]
    Boh = [sb("Boh%d" % i, [128, 32, 128], BF16) for i in range(2)]
    _a0 = Aoh[0][0][:].rearrange("p n c -> p (n c)")
    crep_bf, crepB = _a0[:, 0:1024].rearrange("p (a b) -> p a b", a=8), Aoh[0][1]
    wada_bf, wadaB = _a0[:, 1024:3072].rearrange("p (a b) -> p a b", a=4), Aoh[0][1]
    bbc, bbcB = Boh[0][0][:].rearrange("p n c -> p (n c)").bitcast(F32)[:, 0:512], Boh[0][1]
    Gsb, GsbB = sb("Gsb", [128, 128, TG], BF16)
    NSL = 3
    uT_sb = [sb("uT_sb%d" % i, [128, D], BF16) for i in range(NSL)]
    v_sb = [sb("v_sb%d" % i, [128, D], BF16) for i in range(NSL)]
    ga = [sb("ga%d" % i, [128, TG]) for i in range(2)]
    GA = [sb("GA%d" % i, [128, TG], BF16) for i in range(2)]
    Gflat = Gsb[:].rearrange("p c n -> p (c n)")
    u_bf, ubfB = Gflat[:, 0:1024], Buf("u_bf")
    uT_st = [(Gflat[:, 1024 * (1 + i):1024 * (2 + i)], Buf("uT_st%d" % i)) for i in range(2)]
    v_st = [(Gflat[:, 1024 * (3 + i):1024 * (4 + i)], Buf("v_st%d" % i)) for i in range(2)]

    def dma(q, out_ap, in_ap, sbuf, R=(), W=()):
        P.dma(q, lambda e: e.dma_start(out=out_ap, in_=in_ap), sbuf, R=R, W=W)

    def mm(out_ap, lhsT, rhs, start, stop, R, W):
        P.op("pe", lambda e: e.matmul(out_ap, lhsT, rhs, start=start, stop=stop), R=R, W=W)

    def tr(out_ap, in_ap, R, W):
        P.op("pe", lambda e: e.transpose(out_ap, in_ap, ident_bf[:]), R=list(R) + [identB], W=W)

    def act(out_ap, in_ap, func, R, W, bias=None, scale=None, accum=None):
        kw = {}
        if bias is not None:
            kw["bias"] = bias
        if scale is not None:
            kw["scale"] = scale
        if accum is not None:
            kw["accum_out"] = accum
        P.op("act", lambda e: e.activation(out_ap, in_ap, func, **kw), R=R, W=W)

    def vcopy(eng, out_ap, in_ap, R, W):
        if eng == "act":
            P.op("act", lambda e: e.copy(out_ap, in_ap), R=R, W=W)
        else:
            P.op(eng, lambda e: e.tensor_copy(out_ap, in_ap), R=R, W=W)

    def tt(out_ap, a, b, op, R, W, eng="dve"):
        P.op(eng, lambda e: e.tensor_tensor(out_ap, a, b, op), R=R, W=W)

    def ts1(out_ap, a, s1, op0, R, W, eng="dve"):
        P.op(eng, lambda e: e.tensor_scalar(out_ap, a, s1, None, op0), R=R, W=W)

    def stt(out_ap, a, s_, b, op0, op1, R, W, eng="dve"):
        P.op(eng, lambda e: e.scalar_tensor_tensor(out_ap, a, s_, b, op0, op1), R=R, W=W)

    def rstd(out_ap, tmp_ap, ss_ap, n, RB, WB):
        act(tmp_ap, ss_ap, AF.Sqrt, [RB, epstB], [WB], bias=epst[:, 0:1], scale=1.0 / n)
        P.op("dve", lambda e: e.reciprocal(out_ap, tmp_ap), R=[WB], W=[WB])

    nstg = [0]

    def load_cvt(dst_ap, src_ap, shape_free, dstB, eng=None):
        i = nstg[0] % 2
        nstg[0] += 1
        st, stB = stg[i]
        nel = int(np.prod(shape_free))
        view = st[:, 0:nel]
        if len(shape_free) == 2:
            view = view.rearrange("p (a b) -> p a b", a=shape_free[0])
        dma("sp", view, src_ap, stB, W=[stB])
        vcopy(eng or ("dve" if i == 0 else "act"), dst_ap, view, [stB], [dstB])

    dma("sp", cst2[:, 0:128], cst_d[:, 0:128], cst2B, W=[cst2B])
    dma("sp", cst2[:, 128:256], cst_d[:, IOTA0:IOTA0 + 128], cst2B, W=[cst2B])
    vcopy("dve", ident_bf[:], cst2[:, 0:128], [cst2B], [identB])
    vcopy("dve", iota_bf[:], cst2[:, 128:256], [cst2B], [iotaB])
    P.op("dve", lambda e: e.memset(epst[:], 1e-6), W=[epstB])
    P.op("dve", lambda e: e.memset(skbd[:], 0.0), W=[skbdB])
    dma("sp", g2bc[:], g2_d.partition_broadcast(128), g2B, W=[g2B])
    wpq_v = wpq_d.rearrange("(kc p) n -> p kc n", p=128)
    for hh in range(4):
        load_cvt(w_pq[:, hh * 2:(hh + 1) * 2, :], wpq_v[:, hh * 2:(hh + 1) * 2, :], [2, D], wpqB)
    st0, st0B = stg[0]
    for p_ in range(2):
        dma("sp", st0[:, 0:1024].rearrange("k (h pd) -> k h pd", h=8)[:, :, p_ * 64:(p_ + 1) * 64],
            sk_d.rearrange("(h p) k d -> k h p d", p=2)[:, :, p_, :], st0B, W=[st0B])
    vcopy("dve", skf[:].rearrange("p a b -> p (a b)"), st0[:, 0:1024], [st0B], [skfB])
    for h in range(8):
        tr(bkbf(4)[:, h * 128:(h + 1) * 128], skf[:, h, :], [skfB], [bkB(4)])
    for h in range(8):
        vcopy("act", skbd[0:64, h, 0:128], bkbf(4)[0:64, h * 128:(h + 1) * 128], [bkB(4)], [skbdB])
        vcopy("act", skbd[64:128, h, 128:256], bkbf(4)[64:128, h * 128:(h + 1) * 128], [bkB(4)], [skbdB])

    for c in range(128):
        i = c % 2
        UT, UTB = uT_st[i]
        VS, VSB = v_st[i]
        dma("sp", ssb[:, 0:1024], pu_d[c * 128:(c + 1) * 128, :], ssbB, W=[ssbB])
        vcopy("dve", u_bf, ssb[:, 0:1024], [ssbB], [ubfB])
        for kc in range(8):
            tr(bkbf(4 + i)[:, kc * 128:(kc + 1) * 128], u_bf[:, kc * 128:(kc + 1) * 128], [ubfB], [bkB(4 + i)])
        vcopy("act", UT, bkbf(4 + i)[:, 0:1024], [bkB(4 + i)], [UTB])
        dma("sp", uT_d[c], UT, UTB, R=[UTB], W=[UTD])
        dma("sp", wk2[:, 0:1024], pv_d[c * 128:(c + 1) * 128, :], wk2B, W=[wk2B])
        vcopy("dve" if i == 0 else "act", VS, wk2[:, 0:1024], [wk2B], [VSB])
        dma("sp", v_d[c], VS, VSB, R=[VSB], W=[VD])
    P.barrier()

    def compute_ada(b, first_cg):
        st, stB = stg[0]
        dma("sp", st[:, 0:1024], crep_d[b], stB, W=[stB])
        vcopy("dve", crep_bf[:].rearrange("p a b -> p (a b)"), st[:, 0:1024], [stB], [crepB])
        nstg[0] = 1
        wada_v = wada_d.rearrange("(kc p) n -> p kc n", p=128)
        for ci in range(6):
            cg = first_cg + ci
            bi = 4 + ci % 2
            dma("sp", bbc[:], bada_d[0, cg * 512:(cg + 1) * 512].partition_broadcast(128), bbcB, W=[bbcB])
            for kh in range(2):
                load_cvt(wada_bf[:], wada_v[:, kh * 4:(kh + 1) * 4, cg * 512:(cg + 1) * 512], [4, 512], wadaB)
                for k4 in range(4):
                    kc = kh * 4 + k4
                    mm(bk(bi)[:], crep_bf[:, kc, :], wada_bf[:, k4, :], kc == 0, kc == 7,
                       [crepB, wadaB], [bkB(bi)])
            tt(ada[:, ci * 512:(ci + 1) * 512], bk(bi)[:], bbc[:], ALU.add, [bkB(bi), bbcB], [adaB])

    def bc_last(ap, shape):
        return ap.to_broadcast(list(shape))

    def peer_A1(b, t, tl, sl):
        X, XB = Xa
        H2T, H2TB = h2T[sl]
        ABT, ABTB = abgT[sl * 2 + tl]
        dma("sp", X[:], out_d[b, t * 128:(t + 1) * 128, :], XB, W=[XB])
        act(tmpf[:], X[:], AF.Square, [XB], [tmpfB, smallB], accum=small[:, 0:1])
        rstd(small[:, 2:3], small[:, 1:2], small[:, 0:1], D, smallB, smallB)
        stt(tmpf[:], X[:], small[:, 2:3], ada[:, D:2 * D], ALU.mult, ALU.mult, [XB, smallB, adaB], [tmpfB])
        tt(h2_bf[:], tmpf[:], ada[:, 0:D], ALU.add, [tmpfB, adaB], [h2bB])
        yield
        for kc in range(8):
            tr(bkbf(6)[:, kc * 128:(kc + 1) * 128], h2_bf[:, kc * 128:(kc + 1) * 128], [h2bB], [bkB(6)])
        vcopy("act", H2T[:, :, tl * 128:(tl + 1) * 128], bkbf(6)[:, 0:1024].rearrange("p (a b) -> p a b", a=8),
              [bkB(6)], [H2TB])
        yield
        for oc in range(8):
            bi = 6 + oc // 4
            for kc in range(8):
                mm(bk(bi)[:, (oc % 4) * 128:(oc % 4 + 1) * 128], w_pq[:, kc, oc * 128:(oc + 1) * 128],
                   H2T[:, kc, tl * 128:(tl + 1) * 128], kc == 0, kc == 7, [wpqB, H2TB], [bkB(bi)])
            yield
        for hh in range(2):
            vcopy("act", qpT[:, hh * 4:(hh + 1) * 4, :].rearrange("p a b -> p (a b)"), bk(6 + hh)[:],
                  [bkB(6 + hh)], [qpTB])
        yield
        for hq in range(2):
            for h4 in range(4):
                h = hq * 4 + h4
                mm(bk(6 + h4 // 2)[:, (h4 % 2) * 256:(h4 % 2 + 1) * 256], qpT[:, h, :], skbd[:, h, :], True, True,
                   [qpTB, skbdB], [bkB(6 + h4 // 2)])
            for q_ in range(2):
                vcopy("act", ssb[:, (hq * 2 + q_) * 512:(hq * 2 + q_ + 1) * 512], bk(6 + q_)[:], [bkB(6 + q_)], [ssbB])
            yield
        for hp in range(16):
            sl_ = slice(hp * 128, (hp + 1) * 128)
            o0 = slice(hp * 16, hp * 16 + 8)
            o1 = slice(hp * 16 + 8, hp * 16 + 16)
            P.op("dve", (lambda sl_=sl_, o0=o0: lambda e: e.max(ts_[:, o0], ssb[:, sl_]))(), R=[ssbB], W=[tsB])
            P.op("dve", (lambda sl_=sl_, o0=o0: lambda e: e.max_index(ti_[:, o0], ts_[:, o0], ssb[:, sl_]))(),
                 R=[ssbB, tsB], W=[tiB])
            P.op("dve", (lambda sl_=sl_, o0=o0: lambda e: e.match_replace(wk2[:, sl_], ts_[:, o0], ssb[:, sl_], NEG))(),
                 R=[ssbB, tsB], W=[wk2B])
            yield
            P.op("dve", (lambda sl_=sl_, o1=o1: lambda e: e.max(ts_[:, o1], wk2[:, sl_]))(), R=[wk2B], W=[tsB])
            P.op("dve", (lambda sl_=sl_, o1=o1: lambda e: e.max_index(ti_[:, o1], ts_[:, o1], wk2[:, sl_]))(),
                 R=[wk2B, tsB], W=[tiB])
            yield
        for h in range(8):
            a0 = ts_[:, (2 * h) * 16:(2 * h) * 16 + 16].rearrange("p (x o) -> p x o", o=1)
            a1 = ts_[:, (2 * h + 1) * 16:(2 * h + 1) * 16 + 16].rearrange("p (o y) -> p o y", o=1)
            tt(ssb[:, h * 256:(h + 1) * 256].rearrange("p (x y) -> p x y", x=16),
               bc_last(a0, [128, 16, 16]), bc_last(a1, [128, 16, 16]), ALU.add, [tsB], [ssbB])
            if h % 2:
                yield
        for h in range(8):
            sl_ = slice(h * 256, (h + 1) * 256)
            o0 = slice(h * 16, h * 16 + 8)
            o1 = slice(h * 16 + 8, h * 16 + 16)
            P.op("dve", (lambda sl_=sl_, o0=o0: lambda e: e.max(bs_[:, o0], ssb[:, sl_]))(), R=[ssbB], W=[bsB])
            P.op("dve", (lambda sl_=sl_, o0=o0: lambda e: e.max_index(bp_[:, o0], bs_[:, o0], ssb[:, sl_]))(),
                 R=[ssbB, bsB], W=[bpB])
            P.op("dve", (lambda sl_=sl_, o0=o0: lambda e: e.match_replace(wk2[:, sl_], bs_[:, o0], ssb[:, sl_], NEG))(),
                 R=[ssbB, bsB], W=[wk2B])
            yield
            P.op("dve", (lambda sl_=sl_, o1=o1: lambda e: e.max(bs_[:, o1], wk2[:, sl_]))(), R=[wk2B], W=[bsB])
            P.op("dve", (lambda sl_=sl_, o1=o1: lambda e: e.max_index(bp_[:, o1], bs_[:, o1], wk2[:, sl_]))(),
                 R=[wk2B, bsB], W=[bpB])
            yield
        bs3 = bs_[:].rearrange("p (h j) -> p h j", h=8)
        tt(gx[:, 1, :].rearrange("p (h j) -> p h j", h=8), bs3, bc_last(bs3[:, :, 0:1], [128, 8, 16]),
           ALU.subtract, [bsB], [gxB])
        act(gx[:, 0, :], gx[:, 1, :], AF.Exp, [gxB], [gxB])
        P.op("dve", lambda e: e.tensor_reduce(small[:, 8:16], gx[:, 0, :].rearrange("p (h j) -> p h j", h=8),
                                              AX.X, ALU.add), R=[gxB], W=[smallB])
        P.op("dve", lambda e: e.reciprocal(small[:, 16:24], small[:, 8:16]), R=[smallB], W=[smallB])
        tt(abgf[:, 2, :].rearrange("p (h j) -> p h j", h=8), gx[:, 0, :].rearrange("p (h j) -> p h j", h=8),
           bc_last(small[:, 16:24].rearrange("p (h o) -> p h o", o=1), [128, 8, 16]), ALU.mult,
           [gxB, smallB], [abgfB])
        yield
        vcopy("dve", bpf[:, 0, :], bp_[:], [bpB], [bpfB])
        vcopy("dve", tif[:], ti_[:], [tiB], [tifB])
        ts1(bpf[:, 2, :], bpf[:, 0, :], 16.0, ALU.is_ge, [bpfB], [bpfB])
        yield
        for k_ in range(2, 16):
            stt(bpf[:, 2, :], bpf[:, 0, :], 16.0 * k_, bpf[:, 2, :], ALU.is_ge, ALU.add, [bpfB], [bpfB])
            if k_ % 4 == 0:
                yield
        stt(bpf[:, 1, :], bpf[:, 2, :], -16.0, bpf[:, 0, :], ALU.mult, ALU.add, [bpfB], [bpfB])
        iota16 = cst2[:, 128:144]
        tif4 = tif[:].rearrange("p (h q x) -> p h q x", h=8, q=2)
        for which, (src_i, half) in enumerate(((2, 0), (1, 1))):
            for h in range(8):
                pv_ = bpf[:, src_i, h * 16:(h + 1) * 16].rearrange("p (j o) -> p j o", o=1)
                eqv = ssb[:, h * 256:(h + 1) * 256].rearrange("p (j x) -> p j x", j=16)
                tt(eqv, bc_last(pv_, [128, 16, 16]),
                   bc_last(iota16.rearrange("p (o x) -> p o x", o=1), [128, 16, 16]), ALU.is_equal,
                   [bpfB, cst2B], [ssbB])
                tt(wk2[:, h * 256:(h + 1) * 256].rearrange("p (j x) -> p j x", j=16), eqv,
                   bc_last(tif4[:, h, half, :].rearrange("p (o x) -> p o x", o=1), [128, 16, 16]), ALU.mult,
                   [ssbB, tifB], [wk2B])
                if h % 2:
                    yield
            P.op("dve", (lambda which=which: lambda e: e.tensor_reduce(
                abgf[:, which, :], wk2[:, 0:2048].rearrange("p (r x) -> p r x", x=16), AX.X, ALU.add))(),
                R=[wk2B], W=[abgfB])
            yield
        vcopy("dve", abg[:].rearrange("p a b -> p (a b)"), abgf[:].rearrange("p a b -> p (a b)"), [abgfB], [abgB])
        for i in range(3):
            tr(bkbf(7)[:, i * 128:(i + 1) * 128], abg[:, i, :], [abgB], [bkB(7)])
        vcopy("act", ABT[:].rearrange("p a b -> p (a b)"), bkbf(7)[:, 0:384], [bkB(7)], [ABTB])
        yield

    noh = [0]

    def peer_A2(sl):
        for tl in range(2):
            ABT, ABTB = abgT[sl * 2 + tl]
            for hs in range(4):
                n0 = hs * 32
                k = noh[0] % 2
                noh[0] += 1
                A_, AB_ = Aoh[k]
                B_, BB_ = Boh[k]
                io = bc_last(iota_bf[:].rearrange("p (o c) -> p o c", o=1), [128, 32, 128])
                aT = bc_last(ABT[:, 0, n0:n0 + 32].rearrange("p (n o) -> p n o", o=1), [128, 32, 128])
                bT = bc_last(ABT[:, 1, n0:n0 + 32].rearrange("p (n o) -> p n o", o=1), [128, 32, 128])
                gT = bc_last(ABT[:, 2, n0:n0 + 32].rearrange("p (n o) -> p n o", o=1), [128, 32, 128])
                tt(A_[:], io, aT, ALU.is_equal, [iotaB, ABTB], [AB_])
                tt(A_[:], A_[:], gT, ALU.mult, [AB_, ABTB], [AB_], eng=OH_ENG)
                tt(B_[:], io, bT, ALU.is_equal, [iotaB, ABTB], [BB_])
                for q4 in range(8):
                    bi = 6 + q4 % 2
                    for i in range(4):
                        n = q4 * 4 + i
                        mm(bk(bi)[:, i * 128:(i + 1) * 128], B_[:, n, :], A_[:, n, :], True, True,
                           [BB_, AB_], [bkB(bi)])
                    tok0 = tl * 128 + n0 + q4 * 4
                    vcopy("act", Gsb[:, :, tok0:tok0 + 4].rearrange("p c n -> p n c"),
                          bk(bi)[:].rearrange("p (n c) -> p n c", n=4), [bkB(bi)], [GsbB])

    cnt = [0]

    def peer_B(b, gi, sl, nxt):
        H2T, H2TB = h2T[sl]
        base = cnt[0]
        cnt[0] += 128

        def loads(c):
            UT, UTB = uT_sb[(base + c) % NSL]
            VS, VSB = v_sb[(base + c) % NSL]
            dma("sp", UT[:], uT_d[c], UTB, R=[UTD], W=[UTB])
            dma(VQ, VS[:], v_d[c], VSB, R=[VD], W=[VSB])

        def actmm(c):
            UT, UTB = uT_sb[(base + c) % NSL]
            bi = 4 + c % 2
            for kc in range(8):
                mm(bk(bi)[:, 0:TG], UT[:, kc * 128:(kc + 1) * 128], H2T[:, kc, :], kc == 0, kc == 7,
                   [UTB, H2TB], [bkB(bi)])

        loads(0)
        loads(1)
        actmm(0)
        for c in range(128):
            if c + 2 < 128:
                loads(c + 2)
            if c + 1 < 128:
                actmm(c + 1)
            VS, VSB = v_sb[(base + c) % NSL]
            bi = 4 + c % 2
            g_, gB_ = ga[c % 2]
            G_, GB_ = GA[c % 2]
            act(g_[:], bk(bi)[:, 0:TG], AF.Gelu, [bkB(bi)], [gB_])
            tt(G_[:], g_[:], Gsb[:, c, :], ALU.mult, [gB_, GsbB], [GB_])
            for tl in range(2):
                for hf in range(2):
                    yb = tl * 2 + hf
                    mm(bk(yb)[:], G_[:, tl * 128:(tl + 1) * 128], VS[:, hf * 512:(hf + 1) * 512], c == 0, c == 127,
                       [GB_, VSB], [bkB(yb)])
            if nxt is not None:
                for _ in range(STEPS):
                    if next(nxt, "end") == "end":
                        nxt = None
                        break
        if nxt is not None:
            for _ in nxt:
                pass
        X, XB = Xe
        for tl in range(2):
            t = gi * 2 + tl
            dma("sp", X[:], out_d[b, t * 128:(t + 1) * 128, :], XB, W=[XB])
            for hf in range(2):
                cs = slice(hf * 512, (hf + 1) * 512)
                yb = tl * 2 + hf
                tt(tmpe[:, cs], bk(yb)[:], ada[:, 2 * D + hf * 512:2 * D + (hf + 1) * 512], ALU.mult,
                   [bkB(yb), adaB], [tmpeB])
                tt(X[:, cs], tmpe[:, cs], X[:, cs], ALU.add, [tmpeB, XB], [XB])
            dma("sp", out_d[b, t * 128:(t + 1) * 128, :], X[:], XB, R=[XB], W=[OUTB])

    def a1_group(b, gi, sl):
        for tl in range(2):
            yield from peer_A1(b, gi * 2 + tl, tl, sl)

    for b in range(NB):
        compute_ada(b, 6)
        stt(ada[:, D:2 * D], ada[:, D:2 * D], 1.0, g2bc[:], ALU.add, ALU.mult, [adaB, g2B], [adaB])
        for _ in a1_group(b, 0, 0):
            pass
        for gi in range(NG):
            sl = gi % 2
            peer_A2(sl)
            nxt = a1_group(b, gi + 1, 1 - sl) if gi + 1 < NG else None
            peer_B(b, gi, sl, nxt)
    P.barrier()
    P.emit(nc, es)
    es.close()


def _prep_inputs(inp, NB, S, core, ncores_total):
    f = lambda a: np.ascontiguousarray(np.asarray(a, dtype=np.float32))
    b0 = core * NB
    c = f(inp["c"])[b0:b0 + NB]
    crep = np.ascontiguousarray(
        np.broadcast_to(c.reshape(NB, 8, 128).transpose(0, 2, 1)[:, :, :, None], (NB, 128, 8, 128)).reshape(NB, 128, 1024))
    m = {
        "x": f(inp["x"])[b0:b0 + NB],
        "crep": crep,
        "cst": _consts(S),
        "w_ada": f(inp["w_ada"])[0],
        "b_ada": f(inp["b_ada"])[0].reshape(1, 6 * D),
        "g_norm1": f(inp["g_norm1"])[0],
        "w_in": f(inp["w_in"])[0],
        "g_q": f(inp["g_q"])[0],
        "g_k": f(inp["g_k"])[0],
        "g_ik": f(inp["g_ik"])[0],
        "w_pool_grp": f(inp["w_pool_grp"])[0],
        "s_pool": f(inp["s_pool"])[0].reshape(512),
        "w_up_attn": f(inp["w_up_attn"])[0],
        "w_up_pool": f(inp["w_up_pool"])[0],
        "w_out": f(inp["w_out"])[0],
        "g_norm2": f(inp["g_norm2"])[0],
        "w_peer_q": f(inp["w_peer_q"])[0],
        "peer_subkeys": f(inp["peer_subkeys"])[0].reshape(16, 128, 64),
        "peer_u": f(inp["peer_u"])[0],
        "peer_v": f(inp["peer_v"])[0],
    }
    return m


def kernel(**inputs):
    x = np.asarray(inputs["x"])
    B, S, _ = x.shape
    NB = B // NCORES
    NSEL = min(256, S // 4)
    nc = build(NB, S, NSEL)
    in_maps = [_prep_inputs(inputs, NB, S, i, NCORES) for i in range(NCORES)]
    res = run_bass_kernel_spmd(nc, in_maps, core_ids=list(range(NCORES)))
    out = np.concatenate([np.asarray(r["out"]) for r in res.results], axis=0)
    return out.astype(np.float32)
```

```python
from contextlib import ExitStack
import numpy as np
import concourse.bass as bass
import concourse.mybir as mybir
from concourse.bass_utils import run_bass_kernel_spmd

F32 = mybir.dt.float32
BF16 = mybir.dt.bfloat16
U32 = mybir.dt.uint32
ALU = mybir.AluOpType
AF = mybir.ActivationFunctionType
AX = mybir.AxisListType

D = 1024
NCORES = 8
INW = 3524
NEG = -1.0e30
C_Q, C_K, C_V, C_IQ, C_IK, C_IW, C_P, C_G = 0, 512, 576, 640, 896, 960, 964, 1476
POOLW = (2, 4, 8, 16)
NEXP = 16384


class Buf:
    __slots__ = ("name", "lw", "rd", "sem", "cnt")

    def __init__(self, name):
        self.name = name
        self.lw = None
        self.rd = {}
        self.sem = None
        self.cnt = 0


class Prog:
    ENG = ("pe", "act", "dve", "pool", "sp")

    def __init__(self):
        self.streams = {e: [] for e in self.ENG}
        self.n = {e: 0 for e in self.ENG}
        self.waited = {e: {} for e in self.ENG}
        self.dbufs = []

    def _need(self, eng, ev, waits):
        if ev is None:
            return
        key, val = ev
        if key[0] == "e" and key[1] == eng and eng == "pe":
            return
        w = self.waited[eng]
        if w.get(key, 0) >= val:
            return
        w[key] = val
        waits.append((key, val))

    def _deps(self, eng, R, W, waits):
        for b in R:
            self._need(eng, b.lw, waits)
        for b in W:
            self._need(eng, b.lw, waits)
            for k, v in b.rd.items():
                self._need(eng, (k, v), waits)

    def op(self, eng, fn, R=(), W=()):
        waits = []
        self._deps(eng, R, W, waits)
        self.n[eng] += 1
        key = ("e", eng)
        val = self.n[eng]
        for b in R:
            if b.rd.get(key, 0) < val:
                b.rd[key] = val
        for b in W:
            b.lw = (key, val)
            b.rd = {}
        self.streams[eng].append((waits, fn, None))

    def dma(self, q, fn, sb, R=(), W=()):
        waits = []
        self._deps(q, R, W, waits)
        if sb.cnt > 0:
            self._need(q, (("d", sb), 16 * sb.cnt), waits)
        if sb.sem is None:
            sb.sem = True
            self.dbufs.append(sb)
        sb.cnt += 1
        key = ("d", sb)
        val = 16 * sb.cnt
        for b in R:
            if b.rd.get(key, 0) < val:
                b.rd[key] = val
        for b in W:
            b.lw = (key, val)
            b.rd = {}
        self.streams[q].append((waits, fn, sb))

    def barrier(self):
        for e in self.ENG:
            waits = []
            for o in self.ENG:
                if o != e and self.n[o] > 0:
                    self._need(e, (("e", o), self.n[o]), waits)
            for b in self.dbufs:
                if b.cnt > 0:
                    self._need(e, (("d", b), 16 * b.cnt), waits)
            if e == "pe" and self.n["pe"] > 0:
                pass
            self.streams[e].append((waits, None, None))

    def final_wait(self, q, bufs):
        waits = []
        for b in bufs:
            self._need(q, b.lw, waits)
        self.streams[q].append((waits, None, None))

    def emit(self, nc, es):
        esem = {e: es.enter_context(nc.semaphore("S_" + e)) for e in self.ENG if self.n[e] > 0}
        for i, b in enumerate(self.dbufs):
            b.sem = es.enter_context(nc.semaphore("D%d" % i))

        def sem_of(key):
            return esem[key[1]] if key[0] == "e" else key[1].sem

        streams = self.streams

        def run(engname, eng):
            mysem = esem.get(engname)
            for waits, fn, sb in streams[engname]:
                for key, val in waits:
                    eng.wait_ge(sem_of(key), val)
                if fn is None:
                    continue
                ins = fn(eng)
                if sb is not None:
                    ins.then_inc(sb.sem, 16)
                else:
                    ins.then_inc(mysem, 1)

        with nc.Block() as block:
            if streams["pe"]:
                @block.tensor
                def _(e):
                    run("pe", e)
            if streams["act"]:
                @block.scalar
                def _(e):
                    run("act", e)
            if streams["dve"]:
                @block.vector
                def _(e):
                    run("dve", e)
            if streams["pool"]:
                @block.gpsimd
                def _(e):
                    run("pool", e)
            if streams["sp"]:
                @block.sync
                def _(e):
                    run("sp", e)


def _consts(S):
    ident = np.eye(128, dtype=np.float32)
    bands = np.zeros((128, 12, 128), np.float32)
    n = np.arange(128)
    for g, w in enumerate(POOLW):
        cur = ((n[:, None] <= n[None, :]) & (n[:, None] > n[None, :] - w)).astype(np.float32)
        bands[:, 0 * 4 + g, :] = cur / w - ident
        prv = ((n[:, None] - 128) > (n[None, :] - w)).astype(np.float32)
        bands[:, 1 * 4 + g, :] = prv / w
        cnt = np.minimum(w, n + 1).astype(np.float32)
        bands[:, 2 * 4 + g, :] = cur / cnt[None, :] - ident
    eps = np.broadcast_to(-(np.arange(S, dtype=np.float32)) * 1e-12, (128, S))
    iota = np.broadcast_to(np.arange(128, dtype=np.float32), (128, 128))
    return np.ascontiguousarray(
        np.concatenate([ident, bands.reshape(128, 12 * 128), eps, iota], axis=1).astype(np.float32))


def build(NB, S, NSEL, do_peer=True, dbg=None):
    NT = S // 128
    nc = bass.Bass("TRN2", target_bir_lowering=False)
    P = Prog()
    es = ExitStack()

    def din(name, shape, dt=F32):
        return nc.dram_tensor(name, list(shape), dt, kind="ExternalInput").ap()

    CW = 128 + 12 * 128 + S + 128
    x_d = din("x", [NB, S, D])
    crep_d = din("crep", [NB, 128, 1024])
    cst_d = din("cst", [128, CW])
    wada_d = din("w_ada", [D, 6 * D])
    bada_d = din("b_ada", [1, 6 * D])
    g1_d = din("g_norm1", [D])
    win_d = din("w_in", [D, INW])
    gq_d = din("g_q", [64])
    gk_d = din("g_k", [64])
    gik_d = din("g_ik", [64])
    wgrp_d = din("w_pool_grp", [4, 128, 128])
    spool_d = din("s_pool", [512])
    wua_d = din("w_up_attn", [512, D])
    wup_d = din("w_up_pool", [512, D])
    wout_d = din("w_out", [D, D])
    if do_peer:
        g2_d = din("g_norm2", [D])
        wpq_d = din("w_peer_q", [D, D])
        sk_d = din("peer_subkeys", [16, 128, 64])
        pu_d = din("peer_u", [NEXP, D])
        pv_d = din("peer_v", [NEXP, D])
    out_d = nc.dram_tensor("out", [NB, S, D], F32, kind="ExternalOutput").ap()
    dbg_d = None
    if dbg is not None:
        dbg_d = nc.dram_tensor("dbg", list(dbg), F32, kind="ExternalOutput").ap()

    def sb(name, shape, dt=F32):
        t = es.enter_context(nc.sbuf_tensor("s_" + name, list(shape), dt))
        return t, Buf(name)

    def ps(name):
        t = es.enter_context(nc.psum_tensor(name, [128, 512], F32))
        return t, Buf(name)

    OUTB = Buf("out_dram")

    cst, cstB = sb("cst_sb", [128, CW])
    ident_bf, identB = sb("ident_bf", [128, 128], BF16)
    irep, irepB = sb("irep", [128, 512], BF16)
    band, bandB = sb("band", [128, 12, 128], BF16)
    EPS0 = 128 + 12 * 128
    IOTA0 = EPS0 + S
    w_in, winB = sb("w_in", [128, 8, INW], BF16)
    w_out, woutB = sb("w_out", [128, 8, D], BF16)
    w_ua, wuaB = sb("w_ua", [128, 4, D], BF16)
    w_up, wupB = sb("w_up", [128, 4, D], BF16)
    w_grp, wgrpB = sb("w_grp", [128, 4, 128], BF16)
    g1bc, g1B = sb("g1bc", [128, D])
    gqbc, gqB = sb("gqbc", [128, 64])
    gkbc, gkB = sb("gkbc", [128, 64])
    gikbc, gikB = sb("gikbc", [128, 64])
    bbc, bbcB = sb("bbc", [128, 512])
    epst, epstB = sb("epst", [128, 1])
    stg = [sb("stg%d" % i, [128, 2048]) for i in range(2)]
    ada, adaB = sb("ada", [128, 3 * D])
    crep_bf, crepB = sb("crep_bf", [128, 8, 128], BF16)
    wada_bf, wadaB = sb("wada_bf", [128, 4, 512], BF16)
    kik, kikB = sb("kik", [128, S], BF16)
    kT, kTB = kik, kikB
    vaug, vaugB = sb("vaug", [128, NT, 65], BF16)
    xt = [sb("xt0", [128, D])] * 2
    junk, junkB = sb("junk", [128, D])
    tmpf, tmpfB = sb("tmpf", [128, D])
    tmpg, tmpgB = junk[:, 512:1024], junkB
    small, smallB = sb("small", [128, 64])
    h_bf, hbfB = sb("h_bf", [128, D], BF16)
    hT, hTB = sb("hT", [128, 8, 128], BF16)
    zq_f, zqB = sb("zq_f", [128, 512])
    qn_bf, qnB = sb("qn_bf", [128, 512], BF16)
    qT, qTB = sb("qT", [64, 1024], BF16)
    zb_f, zbB = sb("zb_f", [128, 452])
    knik, knikB = sb("knik", [128, 128], BF16)
    iq_pad, iqbB = sb("iq_pad", [128, 4, 128], BF16)
    iqT, iqTB = sb("iqT", [128, 4, 128], BF16)
    zp = [sb("zp%d" % i, [128, 512], BF16) for i in range(2)]
    gates, gatesB = sb("gates", [128, 2048], BF16)
    rbuf = [sb("rbuf%d" % i, [128, 512]) for i in range(2)] * 2
    sc, scB = stg[0]
    wk, wkB = stg[1]
    m8, m8B = sb("m8", [128, 8])
    Mb, MbB = sb("Mb", [128, S], BF16)
    pexp = [sb("pexp%d" % i, [128, 1024], BF16) for i in range(2)]
    attn, attnB = sb("attn", [128, 512], BF16)
    attnT, attnTB = sb("attnT", [128, 4, 128], BF16)
    mixT, mixTB = sb("mixT", [128, 4, 128], BF16)
    poolT, poolTB = sb("poolT", [128, 4, 128], BF16)
    m_bf, mbfB = sb("m_bf", [128, D], BF16)
    mT, mTB = sb("mT", [128, 8, 128], BF16)
    x1, x1B = tmpf, tmpfB

    bank = [ps("bank%d" % i) for i in range(8)]

    def bk(i):
        return bank[i][0]

    def bkB(i):
        return bank[i][1]

    def bkbf(i):
        return bank[i][0][:].bitcast(BF16)

    def dma(q, out_ap, in_ap, sbuf, R=(), W=()):
        P.dma(q, lambda e: e.dma_start(out=out_ap, in_=in_ap), sbuf, R=R, W=W)

    def mm(out_ap, lhsT, rhs, start, stop, R, W):
        P.op("pe", lambda e: e.matmul(out_ap, lhsT, rhs, start=start, stop=stop), R=R, W=W)

    def tr(out_ap, in_ap, R, W):
        P.op("pe", lambda e: e.transpose(out_ap, in_ap, ident_bf[:]), R=list(R) + [identB], W=W)

    def act(out_ap, in_ap, func, R, W, bias=None, scale=None, accum=None):
        kw = {}
        if bias is not None:
            kw["bias"] = bias
        if scale is not None:
            kw["scale"] = scale
        if accum is not None:
            kw["accum_out"] = accum
        P.op("act", lambda e: e.activation(out_ap, in_ap, func, **kw), R=R, W=W)

    def vcopy(eng, out_ap, in_ap, R, W):
        if eng == "act":
            P.op("act", lambda e: e.copy(out_ap, in_ap), R=R, W=W)
        else:
            P.op(eng, lambda e: e.tensor_copy(out_ap, in_ap), R=R, W=W)

    def tt(out_ap, a, b, op, R, W, eng="dve"):
        P.op(eng, lambda e: e.tensor_tensor(out_ap, a, b, op), R=R, W=W)

    def ts(out_ap, a, s1, s2, op0, op1, R, W, eng="dve", accum=None):
        if op1 is None:
            P.op(eng, lambda e: e.tensor_scalar(out_ap, a, s1, None, op0), R=R, W=W)
        elif accum is None:
            P.op(eng, lambda e: e.tensor_scalar(out_ap, a, s1, s2, op0, op1), R=R, W=W)
        else:
            P.op(eng, lambda e: e.tensor_scalar(out_ap, a, s1, s2, op0, op1, accum), R=R, W=W)

    def stt(out_ap, a, s, b, op0, op1, R, W, eng="dve"):
        P.op(eng, lambda e: e.scalar_tensor_tensor(out_ap, a, s, b, op0, op1), R=R, W=W)

    def rstd(out_ap, tmp_ap, ss_ap, n, RB, WB):
        act(tmp_ap, ss_ap, AF.Sqrt, [RB, epstB], [WB], bias=epst[:, 0:1], scale=1.0 / n)
        P.op("dve", lambda e: e.reciprocal(out_ap, tmp_ap), R=[WB], W=[WB])

    dma("sp", cst[:], cst_d, cstB, W=[cstB])
    vcopy("dve", ident_bf[:], cst[:, 0:128], [cstB], [identB])
    for i in range(4):
        vcopy("dve", irep[:, i * 128:(i + 1) * 128], cst[:, 0:128], [cstB], [irepB])
    vcopy("dve", band[:].rearrange("p a b -> p (a b)"), cst[:, 128:128 + 1536], [cstB], [bandB])
    P.op("dve", lambda e: e.memset(iq_pad[:], 0.0), W=[iqbB])
    P.op("dve", lambda e: e.memset(epst[:], 1e-6), W=[epstB])
    P.op("dve", lambda e: e.memset(vaug[:, :, 64:65], 1.0), W=[vaugB])
    dma("sp", g1bc[:], g1_d.partition_broadcast(128), g1B, W=[g1B])
    dma("sp", gqbc[:], gq_d.partition_broadcast(128), gqB, W=[gqB])
    dma("sp", gkbc[:], gk_d.partition_broadcast(128), gkB, W=[gkB])
    dma("sp", gikbc[:], gik_d.partition_broadcast(128), gikB, W=[gikB])

    nstg = [0]

    def load_cvt(dst_ap, src_ap, shape_free, dstB, eng=None):
        i = nstg[0] % 2
        nstg[0] += 1
        st, stB = stg[i]
        nel = int(np.prod(shape_free))
        view = st[:, 0:nel]
        if len(shape_free) == 2:
            view = view.rearrange("p (a b) -> p a b", a=shape_free[0])
        dma("sp", view, src_ap, stB, W=[stB])
        vcopy(eng or ("dve" if i == 0 else "act"), dst_ap, view, [stB], [dstB])

    win_v = win_d.rearrange("(kc p) n -> p kc n", p=128)
    for kc in range(8):
        load_cvt(w_in[:, kc, 0:2048], win_v[:, kc, 0:2048], [2048], winB)
        load_cvt(w_in[:, kc, 2048:INW], win_v[:, kc, 2048:INW], [INW - 2048], winB)
    wout_v = wout_d.rearrange("(kc p) n -> p kc n", p=128)
    for hh in range(4):
        load_cvt(w_out[:, hh * 2:(hh + 1) * 2, :], wout_v[:, hh * 2:(hh + 1) * 2, :], [2, D], woutB)
    wua_v = wua_d.rearrange("(kc p) n -> p kc n", p=128)
    wup_v = wup_d.rearrange("(kc p) n -> p kc n", p=128)
    for hh in range(2):
        load_cvt(w_ua[:, hh * 2:(hh + 1) * 2, :], wua_v[:, hh * 2:(hh + 1) * 2, :], [2, D], wuaB)
        load_cvt(w_up[:, hh * 2:(hh + 1) * 2, :], wup_v[:, hh * 2:(hh + 1) * 2, :], [2, D], wupB)
    st0, st0B = stg[0]
    st1, st1B = stg[1]
    dma("sp", st0[:, 0:512].rearrange("p (g d) -> p g d", g=4), wgrp_d.rearrange("g c d -> c g d"), st0B, W=[st0B])
    dma("sp", st1[:, 0:512], spool_d.partition_broadcast(128), st1B, W=[st1B])
    tt(w_grp[:].rearrange("p g d -> p (g d)"), st0[:, 0:512], st1[:, 0:512], ALU.mult, [st0B, st1B], [wgrpB])
    nstg[0] = 0

    def compute_ada(b, first_cg):
        st, stB = stg[0]
        dma("sp", st[:, 0:1024], crep_d[b], stB, W=[stB])
        vcopy("dve", crep_bf[:].rearrange("p a b -> p (a b)"), st[:, 0:1024], [stB], [crepB])
        nstg[0] = 1
        wada_v = wada_d.rearrange("(kc p) n -> p kc n", p=128)
        for ci in range(6):
            cg = first_cg + ci
            bi = ci % 2
            dma("sp", bbc[:], bada_d[0, cg * 512:(cg + 1) * 512].partition_broadcast(128), bbcB, W=[bbcB])
            for kh in range(2):
                load_cvt(wada_bf[:], wada_v[:, kh * 4:(kh + 1) * 4, cg * 512:(cg + 1) * 512], [4, 512], wadaB)
                for k4 in range(4):
                    kc = kh * 4 + k4
                    mm(bk(bi)[:], crep_bf[:, kc, :], wada_bf[:, k4, :], kc == 0, kc == 7,
                       [crepB, wadaB], [bkB(bi)])
            tt(ada[:, ci * 512:(ci + 1) * 512], bk(bi)[:], bbc[:], ALU.add, [bkB(bi), bbcB], [adaB])

    def ada_sl(i):
        return ada[:, i * D:(i + 1) * D]

    def rms_small(src_ap, width, gbc, gB, out_bf_ap, outB, srcB):
        tt(junk[:, 0:width], src_ap, src_ap, ALU.mult, [srcB], [junkB])
        P.op("dve", lambda e: e.tensor_reduce(small[:, 32:33], junk[:, 0:width], AX.X, ALU.add),
             R=[junkB], W=[smallB])
        rstd(small[:, 34:35], small[:, 33:34], small[:, 32:33], width, smallB, smallB)
        stt(out_bf_ap, src_ap, small[:, 34:35], gbc[:, 0:width], ALU.mult, ALU.mult,
            [srcB, smallB, gB], [outB])

    import os as _os
    _stage = int(_os.environ.get("KSTAGE", "99"))

    def token_tile(b, t):
        X, XB = xt[t % 2]
        dma("sp", X[:], x_d[b, t * 128:(t + 1) * 128, :], XB, W=[XB])
        act(junk[:], X[:], AF.Square, [XB], [junkB, smallB], accum=small[:, 0:1])
        rstd(small[:, 2:3], small[:, 1:2], small[:, 0:1], D, smallB, smallB)
        stt(tmpf[:], X[:], small[:, 2:3], ada_sl(1), ALU.mult, ALU.mult, [XB, smallB, adaB], [tmpfB])
        tt(h_bf[:], tmpf[:], ada_sl(0), ALU.add, [tmpfB, adaB], [hbfB])
        for kc in range(8):
            tr(bkbf(7)[:, kc * 128:(kc + 1) * 128], h_bf[:, kc * 128:(kc + 1) * 128], [hbfB], [bkB(7)])
        vcopy("act", hT[:].rearrange("p a b -> p (a b)"), bkbf(7)[:, 0:1024], [bkB(7)], [hTB])

        def zgroup(bi, c0, w):
            for kc in range(8):
                mm(bk(bi)[:, 0:w], hT[:, kc, :], w_in[:, kc, c0:c0 + w], kc == 0, kc == 7,
                   [hTB, winB], [bkB(bi)])

        if _stage < 3:
            return
        zgroup(0, C_Q, 512)
        vcopy("act", zq_f[:], bk(0)[:], [bkB(0)], [zqB])
        tt(junk[:, 0:512], zq_f[:], zq_f[:], ALU.mult, [zqB], [junkB])
        P.op("dve", lambda e: e.tensor_reduce(small[:, 8:16], junk[:, 0:512].rearrange("p (h d) -> p h d", h=8),
                                              AX.X, ALU.add), R=[junkB], W=[smallB])
        rstd(small[:, 24:32], small[:, 16:24], small[:, 8:16], 64, smallB, smallB)
        for h in range(8):
            stt(qn_bf[:, h * 64:(h + 1) * 64], zq_f[:, h * 64:(h + 1) * 64], small[:, 24 + h:25 + h],
                gqbc[:], ALU.mult, ALU.mult, [zqB, smallB, gqB], [qnB])
        for h in range(8):
            tr(bkbf(6)[0:64, h * 128:(h + 1) * 128], qn_bf[:, h * 64:(h + 1) * 64], [qnB], [bkB(6)])
        vcopy("act", qT[:], bkbf(6)[0:64, 0:1024], [bkB(6)], [qTB])
        if _stage < 4:
            return
        zgroup(1, C_K, 452)
        vcopy("act", zb_f[:], bk(1)[:, 0:452], [bkB(1)], [zbB])
        rms_small(zb_f[:, 0:64], 64, gkbc, gkB, knik[:, 0:64], knikB, zbB)
        rms_small(zb_f[:, 384:448], 64, gikbc, gikB, knik[:, 64:128], knikB, zbB)
        vcopy("dve", vaug[:, t, 0:64], zb_f[:, 64:128], [zbB], [vaugB])
        vcopy("dve", iq_pad[:, :, 64:128], zb_f[:, 128:384].rearrange("p (h d) -> p h d", h=4), [zbB], [iqbB])
        tr(bkbf(7)[:, 0:128], knik[:], [knikB], [bkB(7)])
        for h in range(4):
            tr(bkbf(7)[:, 128 + h * 128:128 + (h + 1) * 128], iq_pad[:, h, :], [iqbB], [bkB(7)])
        vcopy("act", kik[:, t * 128:(t + 1) * 128], bkbf(7)[:, 0:128], [bkB(7)], [kikB])
        vcopy("act", iqT[:].rearrange("p a b -> p (a b)"), bkbf(7)[:, 128:640], [bkB(7)], [iqTB])
        if _stage < 5:
            return
        ZP, ZPB = zp[t % 2]
        ZPp, ZPpB = zp[(t + 1) % 2]
        zgroup(2, C_P, 512)
        vcopy("act", ZP[:], bk(2)[:], [bkB(2)], [ZPB])
        for gi in range(4):
            bi = (3 + gi) % 4
            zgroup(bi, C_G + gi * 512, 512)
            act(gates[:, gi * 512:(gi + 1) * 512], bk(bi)[:], AF.Sigmoid, [bkB(bi)], [gatesB])

        if _stage < 6:
            return
        L = 128 * (t + 1)
        ngrp = (L + 511) // 512
        iw = zb_f[:, 448:452]
        for kg in range(ngrp):
            k0 = kg * 512
            w = min(512, L - k0)
            for h in range(4):
                mm(bk(h)[:, 0:w], iqT[64:128, h, :], kik[64:128, k0:k0 + w], True, True, [iqTB, kikB], [bkB(h)])
            for h in range(4):
                R_, RB_ = rbuf[h]
                act(R_[:, 0:w], bk(h)[:, 0:w], AF.Relu, [bkB(h)], [RB_])
                if h == 0:
                    stt(sc[:, k0:k0 + w], R_[:, 0:w], iw[:, 0:1], cst[:, EPS0 + k0:EPS0 + k0 + w],
                        ALU.mult, ALU.add, [RB_, zbB, cstB], [scB])
                else:
                    stt(sc[:, k0:k0 + w], R_[:, 0:w], iw[:, h:h + 1], sc[:, k0:k0 + w],
                        ALU.mult, ALU.add, [RB_, zbB, scB], [scB])
        if _stage < 7:
            return
        P.op("dve", lambda e: e.memset(sc[0:64, L - 64:L], NEG), W=[scB])
        if L - 64 >= NSEL:
            src = sc
            nr = NSEL // 8
            for r in range(nr):
                P.op("dve", (lambda s_: lambda e: e.max(m8[:], s_[:, 0:L]))(src), R=[scB, wkB], W=[m8B])
                if r < nr - 1:
                    P.op("dve", (lambda s_: lambda e: e.match_replace(wk[:, 0:L], m8[:], s_[:, 0:L], NEG))(src),
                         R=[scB, wkB, m8B], W=[wkB])
                    src = wk
            thr = m8[:, 7:8]
        else:
            P.op("dve", lambda e: e.memset(m8[:], -1.0e29), W=[m8B])
            thr = m8[:, 7:8]
        ts(Mb[:, 0:L], sc[:, 0:L], thr, -30000.0, ALU.is_lt, ALU.mult, [scB, m8B], [MbB])

        if _stage < 8:
            return
        def logits(j):
            lb = (0, 1) if j % 2 == 0 else (2, 3)
            for hf in range(2):
                bi = lb[hf]
                mm(bk(bi)[:], kik[0:64, j * 128:(j + 1) * 128], qT[:, hf * 512:(hf + 1) * 512], True, False,
                   [kTB, qTB], [bkB(bi)])
                mm(bk(bi)[:], Mb[:, j * 128:(j + 1) * 128], irep[:], False, True, [MbB, irepB], [bkB(bi)])

        logits(0)
        for j in range(t + 1):
            if j + 1 <= t:
                logits(j + 1)
            lb = (0, 1) if j % 2 == 0 else (2, 3)
            PX, PXB = pexp[j % 2]
            for hf in range(2):
                bi = lb[hf]
                act(PX[:, hf * 512:(hf + 1) * 512], bk(bi)[:], AF.Exp, [bkB(bi)], [PXB], scale=0.125)
            for h in range(8):
                ob = 4 + h // 4
                c0 = (h % 4) * 65
                mm(bk(ob)[:, c0:c0 + 65], PX[:, h * 128:(h + 1) * 128], vaug[:, j, :], (j == 0 and h % 4 == 0), (j == t and h % 4 == 3),
                   [PXB, vaugB], [bkB(ob)])
        if _stage < 9:
            return
        for hb in range(2):
            ov = bk(4 + hb)[:, 0:260].rearrange("p (h c) -> p h c", h=4)
            P.op("dve", (lambda o_, hb_: lambda e: e.reciprocal(small[:, 40 + 4 * hb_:44 + 4 * hb_].rearrange("p (h o) -> p h o", o=1),
                                                               o_[:, :, 64:65]))(ov, hb),
                 R=[bkB(4 + hb)], W=[smallB])
            tt(attn[:, hb * 256:(hb + 1) * 256].rearrange("p (h c) -> p h c", h=4), ov[:, :, 0:64],
               small[:, 40 + 4 * hb:44 + 4 * hb].rearrange("p (h o) -> p h o", o=1).to_broadcast([128, 4, 64]),
               ALU.mult, [bkB(4 + hb), smallB], [attnB])
        for c in range(4):
            tr(bkbf(6)[:, c * 128:(c + 1) * 128], attn[:, c * 128:(c + 1) * 128], [attnB], [bkB(6)])
        vcopy("act", attnT[:].rearrange("p a b -> p (a b)"), bkbf(6)[:, 0:512], [bkB(6)], [attnTB])
        for hf in range(2):
            for c in range(4):
                mm(bk(hf)[:], attnT[:, c, :], w_ua[:, c, hf * 512:(hf + 1) * 512], c == 0, c == 3,
                   [attnTB, wuaB], [bkB(hf)])
        if _stage < 10:
            return
        for g in range(4):
            kind = 2 if t == 0 else 0
            mm(bk(6)[:, g * 128:(g + 1) * 128], ZP[:, g * 128:(g + 1) * 128], band[:, kind * 4 + g, :], True, t == 0,
               [ZPB, bandB], [bkB(6)])
            if t > 0:
                mm(bk(6)[:, g * 128:(g + 1) * 128], ZPp[:, g * 128:(g + 1) * 128], band[:, 4 + g, :], False, True,
                   [ZPpB, bandB], [bkB(6)])
        vcopy("act", mixT[:].rearrange("p a b -> p (a b)"), bk(6)[:], [bkB(6)], [mixTB])
        for g in range(4):
            mm(bk(7)[:, g * 128:(g + 1) * 128], w_grp[:, g, :], mixT[:, g, :], True, True, [wgrpB, mixTB], [bkB(7)])
        vcopy("act", poolT[:].rearrange("p a b -> p (a b)"), bk(7)[:], [bkB(7)], [poolTB])
        for hf in range(2):
            for g in range(4):
                mm(bk(2 + hf)[:], poolT[:, g, :], w_up[:, g, hf * 512:(hf + 1) * 512], g == 0, g == 3,
                   [poolTB, wupB], [bkB(2 + hf)])
        if _stage < 11:
            return
        for hf in range(2):
            cs = slice(hf * 512, (hf + 1) * 512)
            tt(tmpf[:, cs], bk(hf)[:], gates[:, hf * 512:(hf + 1) * 512], ALU.mult, [bkB(hf), gatesB], [tmpfB])
            tt(tmpg[:], bk(2 + hf)[:], gates[:, 1024 + hf * 512:1024 + (hf + 1) * 512], ALU.mult,
               [bkB(2 + hf), gatesB], [tmpgB])
            tt(m_bf[:, cs], tmpf[:, cs], tmpg[:], ALU.add, [tmpfB, tmpgB], [mbfB])
        for kc in range(8):
            tr(bkbf(6)[:, kc * 128:(kc + 1) * 128], m_bf[:, kc * 128:(kc + 1) * 128], [mbfB], [bkB(6)])
        vcopy("act", mT[:].rearrange("p a b -> p (a b)"), bkbf(6)[:, 0:1024], [bkB(6)], [mTB])
        for hf in range(2):
            for kc in range(8):
                mm(bk(4 + hf)[:], mT[:, kc, :], w_out[:, kc, hf * 512:(hf + 1) * 512], kc == 0, kc == 7,
                   [mTB, woutB], [bkB(4 + hf)])
        for hf in range(2):
            cs = slice(hf * 512, (hf + 1) * 512)
            tt(tmpf[:, cs], bk(4 + hf)[:], ada[:, 2 * D + hf * 512:2 * D + (hf + 1) * 512], ALU.mult,
               [bkB(4 + hf), adaB], [tmpfB])
            tt(x1[:, cs], tmpf[:, cs], X[:, cs], ALU.add, [tmpfB, XB], [x1B])
        dma("sp", out_d[b, t * 128:(t + 1) * 128, :], x1[:], x1B, R=[x1B], W=[OUTB])

    for b in range(NB):
        if _stage < 1:
            break
        compute_ada(b, 0)
        if _stage < 2:
            break
        stt(ada_sl(1), ada_sl(1), 1.0, g1bc[:], ALU.add, ALU.mult, [adaB, g1B], [adaB])
        for t in range(NT):
            token_tile(b, t)

    P.final_wait("sp", [OUTB])
    P.emit(nc, es)
    es.close()
    if do_peer:
        nc.all_engine_barrier()
        _phase_p(nc, NB, S, dict(crep=crep_d, cst=cst_d, w_ada=wada_d, b_ada=bada_d, g2=g2_d, wpq=wpq_d,
                                 sk=sk_d, pu=pu_d, pv=pv_d, out=out_d, IOTA0=IOTA0))
    return nc


def _phase_p(nc, NB, S, dr):
    TG = 256
    NG = S // TG
    P = Prog()
    es = ExitStack()
    crep_d, cst_d, wada_d, bada_d, g2_d = dr["crep"], dr["cst"], dr["w_ada"], dr["b_ada"], dr["g2"]
    wpq_d, sk_d, pu_d, pv_d, out_d, IOTA0 = dr["wpq"], dr["sk"], dr["pu"], dr["pv"], dr["out"], dr["IOTA0"]
    uT_d = nc.dram_tensor("uT_scr", [128, 128, 1024], BF16, kind="Internal").ap()
    v_d = nc.dram_tensor("v_scr", [128, 128, 1024], BF16, kind="Internal").ap()
    UTD, VD, OUTB = Buf("uT_d"), Buf("v_d"), Buf("out2")

    def sb(name, shape, dt=F32):
        t = es.enter_context(nc.sbuf_tensor("p_" + name, list(shape), dt))
        return t, Buf(name)

    bank = []
    for i in range(8):
        t_ = es.enter_context(nc.psum_tensor("pbank%d" % i, [128, 512], F32))
        bank.append((t_, Buf("pbank%d" % i)))

    def bk(i):
        return bank[i][0]

    def bkB(i):
        return bank[i][1]

    def bkbf(i):
        return bank[i][0][:].bitcast(BF16)

    cst2, cst2B = sb("cst2", [128, 256])
    ident_bf, identB = sb("ident_bf", [128, 128], BF16)
    iota_bf, iotaB = sb("iota_bf", [128, 128], BF16)
    w_pq, wpqB = sb("w_pq", [128, 8, D], BF16)
    skf, skfB = sb("skf", [128, 8, 128], BF16)
    skbd, skbdB = sb("skbd", [128, 8, 256], BF16)
    g2bc, g2B = sb("g2bc", [128, D])
    ada, adaB = sb("ada", [128, 3 * D])
    epst, epstB = sb("epst", [128, 1])
    ssb, ssbB = sb("ssb", [128, 2048])
    wk2, wk2B = sb("wk2", [128, 2048])
    stg = [(ssb, ssbB), (wk2, wk2B)]
    Xa = sb("Xa", [128, D])
    Xe = sb("Xe", [128, D])
    tmpe, tmpeB = Xa
    OH_ENG = "pool"
    VQ = "pool"
    RATE = 1.3
    acc = [0.0]
    tmpf, tmpfB = sb("tmpf", [128, D])
    junk, junkB = tmpf, tmpfB
    small, smallB = sb("small", [128, 64])
    h2_bf, h2bB = sb("h2_bf", [128, D], BF16)
    h2T = [sb("h2T%d" % i, [128, 8, TG], BF16) for i in range(2)]
    qpT, qpTB = sb("qpT", [128, 8, 128], BF16)
    ts_, tsB = sb("ts", [128, 256])
    ti_, tiB = sb("ti", [128, 256], U32)
    tif, tifB = sb("tif", [128, 256])
    bs_, bsB = sb("bs", [128, 128])
    bp_, bpB = sb("bp", [128, 128], U32)
    bpf, bpfB = sb("bpf", [128, 3, 128])
    gx, gxB = sb("gx", [128, 2, 128])
    abg, abgB = sb("abg", [128, 3, 128], BF16)
    abgf, abgfB = sb("abgf", [128, 3, 128])
    abgT = [sb("abgT%d" % i, [128, 3, 128], BF16) for i in range(4)]
    Aoh = [sb("Aoh%d" % i, [128, 32, 128], BF16) for i in range(2)]
    Boh = [sb("Boh%d" % i, [128, 32, 128], BF16) for i in range(2)]
    _a0 = Aoh[0][0][:].rearrange("p n c -> p (n c)")
    crep_bf, crepB = _a0[:, 0:1024].rearrange("p (a b) -> p a b", a=8), Aoh[0][1]
    wada_bf, wadaB = _a0[:, 1024:3072].rearrange("p (a b) -> p a b", a=4), Aoh[0][1]
    bbc, bbcB = Boh[0][0][:].rearrange("p n c -> p (n c)").bitcast(F32)[:, 0:512], Boh[0][1]
    Gsb, GsbB = sb("Gsb", [128, 128, TG], BF16)
    NSL = 3
    uT_sb = [sb("uT_sb%d" % i, [128, D], BF16) for i in range(NSL)]
    v_sb = [sb("v_sb%d" % i, [128, D], BF16) for i in range(NSL)]
    ga = [sb("ga%d" % i, [128, TG]) for i in range(2)]
    GA = [sb("GA%d" % i, [128, TG], BF16) for i in range(2)]
    Gflat = Gsb[:].rearrange("p c n -> p (c n)")
    u_bf, ubfB = Gflat[:, 0:1024], Buf("u_bf")
    uT_st = [(Gflat[:, 1024 * (1 + i):1024 * (2 + i)], Buf("uT_st%d" % i)) for i in range(2)]
    v_st = [(Gflat[:, 1024 * (3 + i):1024 * (4 + i)], Buf("v_st%d" % i)) for i in range(2)]

    def dma(q, out_ap, in_ap, sbuf, R=(), W=()):
        P.dma(q, lambda e: e.dma_start(out=out_ap, in_=in_ap), sbuf, R=R, W=W)

    def mm(out_ap, lhsT, rhs, start, stop, R, W):
        P.op("pe", lambda e: e.matmul(out_ap, lhsT, rhs, start=start, stop=stop), R=R, W=W)

    def tr(out_ap, in_ap, R, W):
        P.op("pe", lambda e: e.transpose(out_ap, in_ap, ident_bf[:]), R=list(R) + [identB], W=W)

    def act(out_ap, in_ap, func, R, W, bias=None, scale=None, accum=None):
        kw = {}
        if bias is not None:
            kw["bias"] = bias
        if scale is not None:
            kw["scale"] = scale
        if accum is not None:
            kw["accum_out"] = accum
        P.op("act", lambda e: e.activation(out_ap, in_ap, func, **kw), R=R, W=W)

    def vcopy(eng, out_ap, in_ap, R, W):
        if eng == "act":
            P.op("act", lambda e: e.copy(out_ap, in_ap), R=R, W=W)
        else:
            P.op(eng, lambda e: e.tensor_copy(out_ap, in_ap), R=R, W=W)

    def tt(out_ap, a, b, op, R, W, eng="dve"):
        P.op(eng, lambda e: e.tensor_tensor(out_ap, a, b, op), R=R, W=W)

    def ts1(out_ap, a, s1, op0, R, W, eng="dve"):
        P.op(eng, lambda e: e.tensor_scalar(out_ap, a, s1, None, op0), R=R, W=W)

    def stt(out_ap, a, s_, b, op0, op1, R, W, eng="dve"):
        P.op(eng, lambda e: e.scalar_tensor_tensor(out_ap, a, s_, b, op0, op1), R=R, W=W)

    def rstd(out_ap, tmp_ap, ss_ap, n, RB, WB):
        act(tmp_ap, ss_ap, AF.Sqrt, [RB, epstB], [WB], bias=epst[:, 0:1], scale=1.0 / n)
        P.op("dve", lambda e: e.reciprocal(out_ap, tmp_ap), R=[WB], W=[WB])

    nstg = [0]

    def load_cvt(dst_ap, src_ap, shape_free, dstB, eng=None):
        i = nstg[0] % 2
        nstg[0] += 1
        st, stB = stg[i]
        nel = int(np.prod(shape_free))
        view = st[:, 0:nel]
        if len(shape_free) == 2:
            view = view.rearrange("p (a b) -> p a b", a=shape_free[0])
        dma("sp", view, src_ap, stB, W=[stB])
        vcopy(eng or ("dve" if i == 0 else "act"), dst_ap, view, [stB], [dstB])

    dma("sp", cst2[:, 0:128], cst_d[:, 0:128], cst2B, W=[cst2B])
    dma("sp", cst2[:, 128:256], cst_d[:, IOTA0:IOTA0 + 128], cst2B, W=[cst2B])
    vcopy("dve", ident_bf[:], cst2[:, 0:128], [cst2B], [identB])
    vcopy("dve", iota_bf[:], cst2[:, 128:256], [cst2B], [iotaB])
    P.op("dve", lambda e: e.memset(epst[:], 1e-6), W=[epstB])
    P.op("dve", lambda e: e.memset(skbd[:], 0.0), W=[skbdB])
    dma("sp", g2bc[:], g2_d.partition_broadcast(128), g2B, W=[g2B])
    wpq_v = wpq_d.rearrange("(kc p) n -> p kc n", p=128)
    for hh in range(4):
        load_cvt(w_pq[:, hh * 2:(hh + 1) * 2, :], wpq_v[:, hh * 2:(hh + 1) * 2, :], [2, D], wpqB)
    st0, st0B = stg[0]
    for p_ in range(2):
        dma("sp", st0[:, 0:1024].rearrange("k (h pd) -> k h pd", h=8)[:, :, p_ * 64:(p_ + 1) * 64],
            sk_d.rearrange("(h p) k d -> k h p d", p=2)[:, :, p_, :], st0B, W=[st0B])
    vcopy("dve", skf[:].rearrange("p a b -> p (a b)"), st0[:, 0:1024], [st0B], [skfB])
    for h in range(8):
        tr(bkbf(4)[:, h * 128:(h + 1) * 128], skf[:, h, :], [skfB], [bkB(4)])
    for h in range(8):
        vcopy("act", skbd[0:64, h, 0:128], bkbf(4)[0:64, h * 128:(h + 1) * 128], [bkB(4)], [skbdB])
        vcopy("act", skbd[64:128, h, 128:256], bkbf(4)[64:128, h * 128:(h + 1) * 128], [bkB(4)], [skbdB])

    for c in range(128):
        i = c % 2
        UT, UTB = uT_st[i]
        VS, VSB = v_st[i]
        dma("sp", ssb[:, 0:1024], pu_d[c * 128:(c + 1) * 128, :], ssbB, W=[ssbB])
        vcopy("dve", u_bf, ssb[:, 0:1024], [ssbB], [ubfB])
        for kc in range(8):
            tr(bkbf(4 + i)[:, kc * 128:(kc + 1) * 128], u_bf[:, kc * 128:(kc + 1) * 128], [ubfB], [bkB(4 + i)])
        vcopy("act", UT, bkbf(4 + i)[:, 0:1024], [bkB(4 + i)], [UTB])
        dma("sp", uT_d[c], UT, UTB, R=[UTB], W=[UTD])
        dma("sp", wk2[:, 0:1024], pv_d[c * 128:(c + 1) * 128, :], wk2B, W=[wk2B])
        vcopy("dve" if i == 0 else "act", VS, wk2[:, 0:1024], [wk2B], [VSB])
        dma("sp", v_d[c], VS, VSB, R=[VSB], W=[VD])
    P.barrier()

    def compute_ada(b, first_cg):
        st, stB = stg[0]
        dma("sp", st[:, 0:1024], crep_d[b], stB, W=[stB])
        vcopy("dve", crep_bf[:].rearrange("p a b -> p (a b)"), st[:, 0:1024], [stB], [crepB])
        nstg[0] = 1
        wada_v = wada_d.rearrange("(kc p) n -> p kc n", p=128)
        for ci in range(6):
            cg = first_cg + ci
            bi = 4 + ci % 2
            dma("sp", bbc[:], bada_d[0, cg * 512:(cg + 1) * 512].partition_broadcast(128), bbcB, W=[bbcB])
            for kh in range(2):
                load_cvt(wada_bf[:], wada_v[:, kh * 4:(kh + 1) * 4, cg * 512:(cg + 1) * 512], [4, 512], wadaB)
                for k4 in range(4):
                    kc = kh * 4 + k4
                    mm(bk(bi)[:], crep_bf[:, kc, :], wada_bf[:, k4, :], kc == 0, kc == 7,
                       [crepB, wadaB], [bkB(bi)])
            tt(ada[:, ci * 512:(ci + 1) * 512], bk(bi)[:], bbc[:], ALU.add, [bkB(bi), bbcB], [adaB])

    def bc_last(ap, shape):
        return ap.to_broadcast(list(shape))

    def peer_A1(b, t, tl, sl):
        X, XB = Xa
        H2T, H2TB = h2T[sl]
        ABT, ABTB = abgT[sl * 2 + tl]
        dma("sp", X[:], out_d[b, t * 128:(t + 1) * 128, :], XB, W=[XB])
        act(tmpf[:], X[:], AF.Square, [XB], [tmpfB, smallB], accum=small[:, 0:1])
        rstd(small[:, 2:3], small[:, 1:2], small[:, 0:1], D, smallB, smallB)
        stt(tmpf[:], X[:], small[:, 2:3], ada[:, D:2 * D], ALU.mult, ALU.mult, [XB, smallB, adaB], [tmpfB])
        tt(h2_bf[:], tmpf[:], ada[:, 0:D], ALU.add, [tmpfB, adaB], [h2bB])
        yield
        for kc in range(8):
            tr(bkbf(6)[:, kc * 128:(kc + 1) * 128], h2_bf[:, kc * 128:(kc + 1) * 128], [h2bB], [bkB(6)])
        vcopy("act", H2T[:, :, tl * 128:(tl + 1) * 128], bkbf(6)[:, 0:1024].rearrange("p (a b) -> p a b", a=8),
              [bkB(6)], [H2TB])
        yield
        for oc in range(8):
            bi = 6 + oc // 4
            for kc in range(8):
                mm(bk(bi)[:, (oc % 4) * 128:(oc % 4 + 1) * 128], w_pq[:, kc, oc * 128:(oc + 1) * 128],
                   H2T[:, kc, tl * 128:(tl + 1) * 128], kc == 0, kc == 7, [wpqB, H2TB], [bkB(bi)])
            yield
        for hh in range(2):
            vcopy("act", qpT[:, hh * 4:(hh + 1) * 4, :].rearrange("p a b -> p (a b)"), bk(6 + hh)[:],
                  [bkB(6 + hh)], [qpTB])
        yield
        for hq in range(2):
            for h4 in range(4):
                h = hq * 4 + h4
                mm(bk(6 + h4 // 2)[:, (h4 % 2) * 256:(h4 % 2 + 1) * 256], qpT[:, h, :], skbd[:, h, :], True, True,
                   [qpTB, skbdB], [bkB(6 + h4 // 2)])
            for q_ in range(2):
                vcopy("act", ssb[:, (hq * 2 + q_) * 512:(hq * 2 + q_ + 1) * 512], bk(6 + q_)[:], [bkB(6 + q_)], [ssbB])
            yield
        for hp in range(16):
            sl_ = slice(hp * 128, (hp + 1) * 128)
            o0 = slice(hp * 16, hp * 16 + 8)
            o1 = slice(hp * 16 + 8, hp * 16 + 16)
            P.op("dve", (lambda sl_=sl_, o0=o0: lambda e: e.max(ts_[:, o0], ssb[:, sl_]))(), R=[ssbB], W=[tsB])
            P.op("dve", (lambda sl_=sl_, o0=o0: lambda e: e.max_index(ti_[:, o0], ts_[:, o0], ssb[:, sl_]))(),
                 R=[ssbB, tsB], W=[tiB])
            P.op("dve", (lambda sl_=sl_, o0=o0: lambda e: e.match_replace(wk2[:, sl_], ts_[:, o0], ssb[:, sl_], NEG))(),
                 R=[ssbB, tsB], W=[wk2B])
            yield
            P.op("dve", (lambda sl_=sl_, o1=o1: lambda e: e.max(ts_[:, o1], wk2[:, sl_]))(), R=[wk2B], W=[tsB])
            P.op("dve", (lambda sl_=sl_, o1=o1: lambda e: e.max_index(ti_[:, o1], ts_[:, o1], wk2[:, sl_]))(),
                 R=[wk2B, tsB], W=[tiB])
            yield
        for h in range(8):
            a0 = ts_[:, (2 * h) * 16:(2 * h) * 16 + 16].rearrange("p (x o) -> p x o", o=1)
            a1 = ts_[:, (2 * h + 1) * 16:(2 * h + 1) * 16 + 16].rearrange("p (o y) -> p o y", o=1)
            tt(ssb[:, h * 256:(h + 1) * 256].rearrange("p (x y) -> p x y", x=16),
               bc_last(a0, [128, 16, 16]), bc_last(a1, [128, 16, 16]), ALU.add, [tsB], [ssbB])
            if h % 2:
                yield
        for h in range(8):
            sl_ = slice(h * 256, (h + 1) * 256)
            o0 = slice(h * 16, h * 16 + 8)
            o1 = slice(h * 16 + 8, h * 16 + 16)
            P.op("dve", (lambda sl_=sl_, o0=o0: lambda e: e.max(bs_[:, o0], ssb[:, sl_]))(), R=[ssbB], W=[bsB])
            P.op("dve", (lambda sl_=sl_, o0=o0: lambda e: e.max_index(bp_[:, o0], bs_[:, o0], ssb[:, sl_]))(),
                 R=[ssbB, bsB], W=[bpB])
            P.op("dve", (lambda sl_=sl_, o0=o0: lambda e: e.match_replace(wk2[:, sl_], bs_[:, o0], ssb[:, sl_], NEG))(),
                 R=[ssbB, bsB], W=[wk2B])
            yield
            P.op("dve", (lambda sl_=sl_, o1=o1: lambda e: e.max(bs_[:, o1], wk2[:, sl_]))(), R=[wk2B], W=[bsB])
            P.op("dve", (lambda sl_=sl_, o1=o1: lambda e: e.max_index(bp_[:, o1], bs_[:, o1], wk2[:, sl_]))(),
                 R=[wk2B, bsB], W=[bpB])
            yield
        bs3 = bs_[:].rearrange("p (h j) -> p h j", h=8)
        tt(gx[:, 1, :].rearrange("p (h j) -> p h j", h=8), bs3, bc_last(bs3[:, :, 0:1], [128, 8, 16]),
           ALU.subtract, [bsB], [gxB])
        act(gx[:, 0, :], gx[:, 1, :], AF.Exp, [gxB], [gxB])
        P.op("dve", lambda e: e.tensor_reduce(small[:, 8:16], gx[:, 0, :].rearrange("p (h j) -> p h j", h=8),
                                              AX.X, ALU.add), R=[gxB], W=[smallB])
        P.op("dve", lambda e: e.reciprocal(small[:, 16:24], small[:, 8:16]), R=[smallB], W=[smallB])
        tt(abgf[:, 2, :].rearrange("p (h j) -> p h j", h=8), gx[:, 0, :].rearrange("p (h j) -> p h j", h=8),
           bc_last(small[:, 16:24].rearrange("p (h o) -> p h o", o=1), [128, 8, 16]), ALU.mult,
           [gxB, smallB], [abgfB])
        yield
        vcopy("dve", bpf[:, 0, :], bp_[:], [bpB], [bpfB])
        vcopy("dve", tif[:], ti_[:], [tiB], [tifB])
        ts1(bpf[:, 2, :], bpf[:, 0, :], 16.0, ALU.is_ge, [bpfB], [bpfB])
        yield
        for k_ in range(2, 16):
            stt(bpf[:, 2, :], bpf[:, 0, :], 16.0 * k_, bpf[:, 2, :], ALU.is_ge, ALU.add, [bpfB], [bpfB])
            if k_ % 4 == 0:
                yield
        stt(bpf[:, 1, :], bpf[:, 2, :], -16.0, bpf[:, 0, :], ALU.mult, ALU.add, [bpfB], [bpfB])
        iota16 = cst2[:, 128:144]
        tif4 = tif[:].rearrange("p (h q x) -> p h q x", h=8, q=2)
        for which, (src_i, half) in enumerate(((2, 0), (1, 1))):
            for h in range(8):
                pv_ = bpf[:, src_i, h * 16:(h + 1) * 16].rearrange("p (j o) -> p j o", o=1)
                eqv = ssb[:, h * 256:(h + 1) * 256].rearrange("p (j x) -> p j x", j=16)
                tt(eqv, bc_last(pv_, [128, 16, 16]),
                   bc_last(iota16.rearrange("p (o x) -> p o x", o=1), [128, 16, 16]), ALU.is_equal,
                   [bpfB, cst2B], [ssbB])
                tt(wk2[:, h * 256:(h + 1) * 256].rearrange("p (j x) -> p j x", j=16), eqv,
                   bc_last(tif4[:, h, half, :].rearrange("p (o x) -> p o x", o=1), [128, 16, 16]), ALU.mult,
                   [ssbB, tifB], [wk2B])
                if h % 2:
                    yield
            P.op("dve", (lambda which=which: lambda e: e.tensor_reduce(
                abgf[:, which, :], wk2[:, 0:2048].rearrange("p (r x) -> p r x", x=16), AX.X, ALU.add))(),
                R=[wk2B], W=[abgfB])
            yield
        vcopy("dve", abg[:].rearrange("p a b -> p (a b)"), abgf[:].rearrange("p a b -> p (a b)"), [abgfB], [abgB])
        for i in range(3):
            tr(bkbf(7)[:, i * 128:(i + 1) * 128], abg[:, i, :], [abgB], [bkB(7)])
        vcopy("act", ABT[:].rearrange("p a b -> p (a b)"), bkbf(7)[:, 0:384], [bkB(7)], [ABTB])
        yield

    noh = [0]

    def peer_A2(sl):
        for tl in range(2):
            ABT, ABTB = abgT[sl * 2 + tl]
            for hs in range(4):
                n0 = hs * 32
                k = noh[0] % 2
                noh[0] += 1
                A_, AB_ = Aoh[k]
                B_, BB_ = Boh[k]
                io = bc_last(iota_bf[:].rearrange("p (o c) -> p o c", o=1), [128, 32, 128])
                aT = bc_last(ABT[:, 0, n0:n0 + 32].rearrange("p (n o) -> p n o", o=1), [128, 32, 128])
                bT = bc_last(ABT[:, 1, n0:n0 + 32].rearrange("p (n o) -> p n o", o=1), [128, 32, 128])
                gT = bc_last(ABT[:, 2, n0:n0 + 32].rearrange("p (n o) -> p n o", o=1), [128, 32, 128])
                tt(A_[:], io, aT, ALU.is_equal, [iotaB, ABTB], [AB_])
                tt(A_[:], A_[:], gT, ALU.mult, [AB_, ABTB], [AB_], eng=OH_ENG)
                tt(B_[:], io, bT, ALU.is_equal, [iotaB, ABTB], [BB_])
                for q4 in range(8):
                    bi = 6 + q4 % 2
                    for i in range(4):
                        n = q4 * 4 + i
                        mm(bk(bi)[:, i * 128:(i + 1) * 128], B_[:, n, :], A_[:, n, :], True, True,
                           [BB_, AB_], [bkB(bi)])
                    tok0 = tl * 128 + n0 + q4 * 4
                    vcopy("act", Gsb[:, :, tok0:tok0 + 4].rearrange("p c n -> p n c"),
                          bk(bi)[:].rearrange("p (n c) -> p n c", n=4), [bkB(bi)], [GsbB])

    cnt = [0]

    def peer_B(b, gi, sl, nxt):
        H2T, H2TB = h2T[sl]
        base = cnt[0]
        cnt[0] += 128

        def loads(c):
            UT, UTB = uT_sb[(base + c) % NSL]
            VS, VSB = v_sb[(base + c) % NSL]
            dma("sp", UT[:], uT_d[c], UTB, R=[UTD], W=[UTB])
            dma(VQ, VS[:], v_d[c], VSB, R=[VD], W=[VSB])

        def actmm(c):
            UT, UTB = uT_sb[(base + c) % NSL]
            bi = 4 + c % 2
            for kc in range(8):
                mm(bk(bi)[:, 0:TG], UT[:, kc * 128:(kc + 1) * 128], H2T[:, kc, :], kc == 0, kc == 7,
                   [UTB, H2TB], [bkB(bi)])

        loads(0)
        loads(1)
        actmm(0)
        for c in range(128):
            if c + 2 < 128:
                loads(c + 2)
            if c + 1 < 128:
                actmm(c + 1)
            VS, VSB = v_sb[(base + c) % NSL]
            bi = 4 + c % 2
            g_, gB_ = ga[c % 2]
            G_, GB_ = GA[c % 2]
            act(g_[:], bk(bi)[:, 0:TG], AF.Gelu, [bkB(bi)], [gB_])
            tt(G_[:], g_[:], Gsb[:, c, :], ALU.mult, [gB_, GsbB], [GB_])
            for tl in range(2):
                for hf in range(2):
                    yb = tl * 2 + hf
                    mm(bk(yb)[:], G_[:, tl * 128:(tl + 1) * 128], VS[:, hf * 512:(hf + 1) * 512], c == 0, c == 127,
                       [GB_, VSB], [bkB(yb)])
            if nxt is not None:
                acc[0] += RATE
                while nxt is not None and acc[0] >= 1.0:
                    acc[0] -= 1.0
                    if next(nxt, "end") == "end":
                        nxt = None
        if nxt is not None:
            for _ in nxt:
                pass
        X, XB = Xe
        for tl in range(2):
            t = gi * 2 + tl
            dma("sp", X[:], out_d[b, t * 128:(t + 1) * 128, :], XB, W=[XB])
            for hf in range(2):
                cs = slice(hf * 512, (hf + 1) * 512)
                yb = tl * 2 + hf
                tt(tmpe[:, cs], bk(yb)[:], ada[:, 2 * D + hf * 512:2 * D + (hf + 1) * 512], ALU.mult,
                   [bkB(yb), adaB], [tmpeB])
                tt(X[:, cs], tmpe[:, cs], X[:, cs], ALU.add, [tmpeB, XB], [XB])
            dma("sp", out_d[b, t * 128:(t + 1) * 128, :], X[:], XB, R=[XB], W=[OUTB])

    def a1_group(b, gi, sl):
        for tl in range(2):
            yield from peer_A1(b, gi * 2 + tl, tl, sl)

    for b in range(NB):
        compute_ada(b, 6)
        stt(ada[:, D:2 * D], ada[:, D:2 * D], 1.0, g2bc[:], ALU.add, ALU.mult, [adaB, g2B], [adaB])
        for _ in a1_group(b, 0, 0):
            pass
        for gi in range(NG):
            sl = gi % 2
            peer_A2(sl)
            nxt = a1_group(b, gi + 1, 1 - sl) if gi + 1 < NG else None
            peer_B(b, gi, sl, nxt)
    P.barrier()
    P.emit(nc, es)
    es.close()


def _prep_inputs(inp, NB, S, core, ncores_total):
    f = lambda a: np.ascontiguousarray(np.asarray(a, dtype=np.float32))
    b0 = core * NB
    c = f(inp["c"])[b0:b0 + NB]
    crep = np.ascontiguousarray(
        np.broadcast_to(c.reshape(NB, 8, 128).transpose(0, 2, 1)[:, :, :, None], (NB, 128, 8, 128)).reshape(NB, 128, 1024))
    m = {
        "x": f(inp["x"])[b0:b0 + NB],
        "crep": crep,
        "cst": _consts(S),
        "w_ada": f(inp["w_ada"])[0],
        "b_ada": f(inp["b_ada"])[0].reshape(1, 6 * D),
        "g_norm1": f(inp["g_norm1"])[0],
        "w_in": f(inp["w_in"])[0],
        "g_q": f(inp["g_q"])[0],
        "g_k": f(inp["g_k"])[0],
        "g_ik": f(inp["g_ik"])[0],
        "w_pool_grp": f(inp["w_pool_grp"])[0],
        "s_pool": f(inp["s_pool"])[0].reshape(512),
        "w_up_attn": f(inp["w_up_attn"])[0],
        "w_up_pool": f(inp["w_up_pool"])[0],
        "w_out": f(inp["w_out"])[0],
        "g_norm2": f(inp["g_norm2"])[0],
        "w_peer_q": f(inp["w_peer_q"])[0],
        "peer_subkeys": f(inp["peer_subkeys"])[0].reshape(16, 128, 64),
        "peer_u": f(inp["peer_u"])[0],
        "peer_v": f(inp["peer_v"])[0],
    }
    return m


def kernel(**inputs):
    x = np.asarray(inputs["x"])
    B, S, _ = x.shape
    NB = B // NCORES
    NSEL = min(256, S // 4)
    nc = build(NB, S, NSEL)
    in_maps = [_prep_inputs(inputs, NB, S, i, NCORES) for i in range(NCORES)]
    res = run_bass_kernel_spmd(nc, in_maps, core_ids=list(range(NCORES)))
    out = np.concatenate([np.asarray(r["out"]) for r in res.results], axis=0)
    return out.astype(np.float32)
```

```python
from contextlib import ExitStack
import numpy as np
import concourse.bass as bass
import concourse.mybir as mybir
from concourse.bass_utils import run_bass_kernel_spmd

F32 = mybir.dt.float32
BF16 = mybir.dt.bfloat16
U32 = mybir.dt.uint32
ALU = mybir.AluOpType
AF = mybir.ActivationFunctionType
AX = mybir.AxisListType

D = 1024
NCORES = 8
INW = 3524
NEG = -1.0e30
C_Q, C_K, C_V, C_IQ, C_IK, C_IW, C_P, C_G = 0, 512, 576, 640, 896, 960, 964, 1476
POOLW = (2, 4, 8, 16)
NEXP = 16384


class Buf:
    __slots__ = ("name", "lw", "rd", "sem", "cnt")

    def __init__(self, name):
        self.name = name
        self.lw = None
        self.rd = {}
        self.sem = None
        self.cnt = 0


class Prog:
    ENG = ("pe", "act", "dve", "pool", "sp")

    def __init__(self):
        self.streams = {e: [] for e in self.ENG}
        self.n = {e: 0 for e in self.ENG}
        self.waited = {e: {} for e in self.ENG}
        self.dbufs = []

    def _need(self, eng, ev, waits):
        if ev is None:
            return
        key, val = ev
        if key[0] == "e" and key[1] == eng and eng == "pe":
            return
        w = self.waited[eng]
        if w.get(key, 0) >= val:
            return
        w[key] = val
        waits.append((key, val))

    def _deps(self, eng, R, W, waits):
        for b in R:
            self._need(eng, b.lw, waits)
        for b in W:
            self._need(eng, b.lw, waits)
            for k, v in b.rd.items():
                self._need(eng, (k, v), waits)

    def op(self, eng, fn, R=(), W=()):
        waits = []
        self._deps(eng, R, W, waits)
        self.n[eng] += 1
        key = ("e", eng)
        val = self.n[eng]
        for b in R:
            if b.rd.get(key, 0) < val:
                b.rd[key] = val
        for b in W:
            b.lw = (key, val)
            b.rd = {}
        self.streams[eng].append((waits, fn, None))

    def dma(self, q, fn, sb, R=(), W=()):
        waits = []
        self._deps(q, R, W, waits)
        if sb.cnt > 0:
            self._need(q, (("d", sb), 16 * sb.cnt), waits)
        if sb.sem is None:
            sb.sem = True
            self.dbufs.append(sb)
        sb.cnt += 1
        key = ("d", sb)
        val = 16 * sb.cnt
        for b in R:
            if b.rd.get(key, 0) < val:
                b.rd[key] = val
        for b in W:
            b.lw = (key, val)
            b.rd = {}
        self.streams[q].append((waits, fn, sb))

    def barrier(self):
        for e in self.ENG:
            waits = []
            for o in self.ENG:
                if o != e and self.n[o] > 0:
                    self._need(e, (("e", o), self.n[o]), waits)
            for b in self.dbufs:
                if b.cnt > 0:
                    self._need(e, (("d", b), 16 * b.cnt), waits)
            if e == "pe" and self.n["pe"] > 0:
                pass
            self.streams[e].append((waits, None, None))

    def final_wait(self, q, bufs):
        waits = []
        for b in bufs:
            self._need(q, b.lw, waits)
        self.streams[q].append((waits, None, None))

    def emit(self, nc, es):
        esem = {e: es.enter_context(nc.semaphore("S_" + e)) for e in self.ENG if self.n[e] > 0}
        for i, b in enumerate(self.dbufs):
            b.sem = es.enter_context(nc.semaphore("D%d" % i))

        def sem_of(key):
            return esem[key[1]] if key[0] == "e" else key[1].sem

        streams = self.streams

        def run(engname, eng):
            mysem = esem.get(engname)
            for waits, fn, sb in streams[engname]:
                for key, val in waits:
                    eng.wait_ge(sem_of(key), val)
                if fn is None:
                    continue
                ins = fn(eng)
                if sb is not None:
                    ins.then_inc(sb.sem, 16)
                else:
                    ins.then_inc(mysem, 1)

        with nc.Block() as block:
            if streams["pe"]:
                @block.tensor
                def _(e):
                    run("pe", e)
            if streams["act"]:
                @block.scalar
                def _(e):
                    run("act", e)
            if streams["dve"]:
                @block.vector
                def _(e):
                    run("dve", e)
            if streams["pool"]:
                @block.gpsimd
                def _(e):
                    run("pool", e)
            if streams["sp"]:
                @block.sync
                def _(e):
                    run("sp", e)


def _consts(S):
    ident = np.eye(128, dtype=np.float32)
    bands = np.zeros((128, 12, 128), np.float32)
    n = np.arange(128)
    for g, w in enumerate(POOLW):
        cur = ((n[:, None] <= n[None, :]) & (n[:, None] > n[None, :] - w)).astype(np.float32)
        bands[:, 0 * 4 + g, :] = cur / w - ident
        prv = ((n[:, None] - 128) > (n[None, :] - w)).astype(np.float32)
        bands[:, 1 * 4 + g, :] = prv / w
        cnt = np.minimum(w, n + 1).astype(np.float32)
        bands[:, 2 * 4 + g, :] = cur / cnt[None, :] - ident
    eps = np.broadcast_to(-(np.arange(S, dtype=np.float32)) * 1e-12, (128, S))
    iota = np.broadcast_to(np.arange(128, dtype=np.float32), (128, 128))
    return np.ascontiguousarray(
        np.concatenate([ident, bands.reshape(128, 12 * 128), eps, iota], axis=1).astype(np.float32))


def build(NB, S, NSEL, do_peer=True, dbg=None):
    NT = S // 128
    nc = bass.Bass("TRN2", target_bir_lowering=False)
    P = Prog()
    es = ExitStack()

    def din(name, shape, dt=F32):
        return nc.dram_tensor(name, list(shape), dt, kind="ExternalInput").ap()

    CW = 128 + 12 * 128 + S + 128
    x_d = din("x", [NB, S, D])
    crep_d = din("crep", [NB, 128, 1024])
    cst_d = din("cst", [128, CW])
    wada_d = din("w_ada", [D, 6 * D])
    bada_d = din("b_ada", [1, 6 * D])
    g1_d = din("g_norm1", [D])
    win_d = din("w_in", [D, INW])
    gq_d = din("g_q", [64])
    gk_d = din("g_k", [64])
    gik_d = din("g_ik", [64])
    wgrp_d = din("w_pool_grp", [4, 128, 128])
    spool_d = din("s_pool", [512])
    wua_d = din("w_up_attn", [512, D])
    wup_d = din("w_up_pool", [512, D])
    wout_d = din("w_out", [D, D])
    if do_peer:
        g2_d = din("g_norm2", [D])
        wpq_d = din("w_peer_q", [D, D])
        sk_d = din("peer_subkeys", [16, 128, 64])
        pu_d = din("peer_u", [NEXP, D])
        pv_d = din("peer_v", [NEXP, D])
    out_d = nc.dram_tensor("out", [NB, S, D], F32, kind="ExternalOutput").ap()
    dbg_d = None
    if dbg is not None:
        dbg_d = nc.dram_tensor("dbg", list(dbg), F32, kind="ExternalOutput").ap()

    def sb(name, shape, dt=F32):
        t = es.enter_context(nc.sbuf_tensor("s_" + name, list(shape), dt))
        return t, Buf(name)

    def ps(name):
        t = es.enter_context(nc.psum_tensor(name, [128, 512], F32))
        return t, Buf(name)

    OUTB = Buf("out_dram")

    cst, cstB = sb("cst_sb", [128, CW])
    ident_bf, identB = sb("ident_bf", [128, 128], BF16)
    irep, irepB = sb("irep", [128, 512], BF16)
    band, bandB = sb("band", [128, 12, 128], BF16)
    EPS0 = 128 + 12 * 128
    IOTA0 = EPS0 + S
    w_in, winB = sb("w_in", [128, 8, INW], BF16)
    w_out, woutB = sb("w_out", [128, 8, D], BF16)
    w_ua, wuaB = sb("w_ua", [128, 4, D], BF16)
    w_up, wupB = sb("w_up", [128, 4, D], BF16)
    w_grp, wgrpB = sb("w_grp", [128, 4, 128], BF16)
    g1bc, g1B = sb("g1bc", [128, D])
    gqbc, gqB = sb("gqbc", [128, 64])
    gkbc, gkB = sb("gkbc", [128, 64])
    gikbc, gikB = sb("gikbc", [128, 64])
    bbc, bbcB = sb("bbc", [128, 512])
    epst, epstB = sb("epst", [128, 1])
    stg = [sb("stg%d" % i, [128, 2048]) for i in range(2)]
    ada, adaB = sb("ada", [128, 3 * D])
    crep_bf, crepB = sb("crep_bf", [128, 8, 128], BF16)
    wada_bf, wadaB = sb("wada_bf", [128, 4, 512], BF16)
    kik, kikB = sb("kik", [128, S], BF16)
    kT, kTB = kik, kikB
    vaug, vaugB = sb("vaug", [128, NT, 65], BF16)
    xt = [sb("xt0", [128, D])] * 2
    junk, junkB = sb("junk", [128, D])
    tmpf, tmpfB = sb("tmpf", [128, D])
    tmpg, tmpgB = junk[:, 512:1024], junkB
    small, smallB = sb("small", [128, 64])
    h_bf, hbfB = sb("h_bf", [128, D], BF16)
    hT, hTB = sb("hT", [128, 8, 128], BF16)
    zq_f, zqB = sb("zq_f", [128, 512])
    qn_bf, qnB = sb("qn_bf", [128, 512], BF16)
    qT, qTB = sb("qT", [64, 1024], BF16)
    zb_f, zbB = sb("zb_f", [128, 452])
    knik, knikB = sb("knik", [128, 128], BF16)
    iq_pad, iqbB = sb("iq_pad", [128, 4, 128], BF16)
    iqT, iqTB = sb("iqT", [128, 4, 128], BF16)
    zp = [sb("zp%d" % i, [128, 512], BF16) for i in range(2)]
    gates, gatesB = sb("gates", [128, 2048], BF16)
    rbuf = [sb("rbuf%d" % i, [128, 512]) for i in range(2)] * 2
    sc, scB = stg[0]
    wk, wkB = stg[1]
    m8, m8B = sb("m8", [128, 8])
    Mb, MbB = sb("Mb", [128, S], BF16)
    pexp = [sb("pexp%d" % i, [128, 1024], BF16) for i in range(2)]
    attn, attnB = sb("attn", [128, 512], BF16)
    attnT, attnTB = sb("attnT", [128, 4, 128], BF16)
    mixT, mixTB = sb("mixT", [128, 4, 128], BF16)
    poolT, poolTB = sb("poolT", [128, 4, 128], BF16)
    m_bf, mbfB = sb("m_bf", [128, D], BF16)
    mT, mTB = sb("mT", [128, 8, 128], BF16)
    x1, x1B = tmpf, tmpfB

    bank = [ps("bank%d" % i) for i in range(8)]

    def bk(i):
        return bank[i][0]

    def bkB(i):
        return bank[i][1]

    def bkbf(i):
        return bank[i][0][:].bitcast(BF16)

    def dma(q, out_ap, in_ap, sbuf, R=(), W=()):
        P.dma(q, lambda e: e.dma_start(out=out_ap, in_=in_ap), sbuf, R=R, W=W)

    def mm(out_ap, lhsT, rhs, start, stop, R, W):
        P.op("pe", lambda e: e.matmul(out_ap, lhsT, rhs, start=start, stop=stop), R=R, W=W)

    def tr(out_ap, in_ap, R, W):
        P.op("pe", lambda e: e.transpose(out_ap, in_ap, ident_bf[:]), R=list(R) + [identB], W=W)

    def act(out_ap, in_ap, func, R, W, bias=None, scale=None, accum=None):
        kw = {}
        if bias is not None:
            kw["bias"] = bias
        if scale is not None:
            kw["scale"] = scale
        if accum is not None:
            kw["accum_out"] = accum
        P.op("act", lambda e: e.activation(out_ap, in_ap, func, **kw), R=R, W=W)

    def vcopy(eng, out_ap, in_ap, R, W):
        if eng == "act":
            P.op("act", lambda e: e.copy(out_ap, in_ap), R=R, W=W)
        else:
            P.op(eng, lambda e: e.tensor_copy(out_ap, in_ap), R=R, W=W)

    def tt(out_ap, a, b, op, R, W, eng="dve"):
        P.op(eng, lambda e: e.tensor_tensor(out_ap, a, b, op), R=R, W=W)

    def ts(out_ap, a, s1, s2, op0, op1, R, W, eng="dve", accum=None):
        if op1 is None:
            P.op(eng, lambda e: e.tensor_scalar(out_ap, a, s1, None, op0), R=R, W=W)
        elif accum is None:
            P.op(eng, lambda e: e.tensor_scalar(out_ap, a, s1, s2, op0, op1), R=R, W=W)
        else:
            P.op(eng, lambda e: e.tensor_scalar(out_ap, a, s1, s2, op0, op1, accum), R=R, W=W)

    def stt(out_ap, a, s, b, op0, op1, R, W, eng="dve"):
        P.op(eng, lambda e: e.scalar_tensor_tensor(out_ap, a, s, b, op0, op1), R=R, W=W)

    def rstd(out_ap, tmp_ap, ss_ap, n, RB, WB):
        act(tmp_ap, ss_ap, AF.Sqrt, [RB, epstB], [WB], bias=epst[:, 0:1], scale=1.0 / n)
        P.op("dve", lambda e: e.reciprocal(out_ap, tmp_ap), R=[WB], W=[WB])

    dma("sp", cst[:], cst_d, cstB, W=[cstB])
    vcopy("dve", ident_bf[:], cst[:, 0:128], [cstB], [identB])
    for i in range(4):
        vcopy("dve", irep[:, i * 128:(i + 1) * 128], cst[:, 0:128], [cstB], [irepB])
    vcopy("dve", band[:].rearrange("p a b -> p (a b)"), cst[:, 128:128 + 1536], [cstB], [bandB])
    P.op("dve", lambda e: e.memset(iq_pad[:], 0.0), W=[iqbB])
    P.op("dve", lambda e: e.memset(epst[:], 1e-6), W=[epstB])
    P.op("dve", lambda e: e.memset(vaug[:, :, 64:65], 1.0), W=[vaugB])
    dma("sp", g1bc[:], g1_d.partition_broadcast(128), g1B, W=[g1B])
    dma("sp", gqbc[:], gq_d.partition_broadcast(128), gqB, W=[gqB])
    dma("sp", gkbc[:], gk_d.partition_broadcast(128), gkB, W=[gkB])
    dma("sp", gikbc[:], gik_d.partition_broadcast(128), gikB, W=[gikB])

    nstg = [0]

    def load_cvt(dst_ap, src_ap, shape_free, dstB, eng=None):
        i = nstg[0] % 2
        nstg[0] += 1
        st, stB = stg[i]
        nel = int(np.prod(shape_free))
        view = st[:, 0:nel]
        if len(shape_free) == 2:
            view = view.rearrange("p (a b) -> p a b", a=shape_free[0])
        dma("sp", view, src_ap, stB, W=[stB])
        vcopy(eng or ("dve" if i == 0 else "act"), dst_ap, view, [stB], [dstB])

    win_v = win_d.rearrange("(kc p) n -> p kc n", p=128)
    for kc in range(8):
        load_cvt(w_in[:, kc, 0:2048], win_v[:, kc, 0:2048], [2048], winB)
        load_cvt(w_in[:, kc, 2048:INW], win_v[:, kc, 2048:INW], [INW - 2048], winB)
    wout_v = wout_d.rearrange("(kc p) n -> p kc n", p=128)
    for hh in range(4):
        load_cvt(w_out[:, hh * 2:(hh + 1) * 2, :], wout_v[:, hh * 2:(hh + 1) * 2, :], [2, D], woutB)
    wua_v = wua_d.rearrange("(kc p) n -> p kc n", p=128)
    wup_v = wup_d.rearrange("(kc p) n -> p kc n", p=128)
    for hh in range(2):
        load_cvt(w_ua[:, hh * 2:(hh + 1) * 2, :], wua_v[:, hh * 2:(hh + 1) * 2, :], [2, D], wuaB)
        load_cvt(w_up[:, hh * 2:(hh + 1) * 2, :], wup_v[:, hh * 2:(hh + 1) * 2, :], [2, D], wupB)
    st0, st0B = stg[0]
    st1, st1B = stg[1]
    dma("sp", st0[:, 0:512].rearrange("p (g d) -> p g d", g=4), wgrp_d.rearrange("g c d -> c g d"), st0B, W=[st0B])
    dma("sp", st1[:, 0:512], spool_d.partition_broadcast(128), st1B, W=[st1B])
    tt(w_grp[:].rearrange("p g d -> p (g d)"), st0[:, 0:512], st1[:, 0:512], ALU.mult, [st0B, st1B], [wgrpB])
    nstg[0] = 0

    def compute_ada(b, first_cg):
        st, stB = stg[0]
        dma("sp", st[:, 0:1024], crep_d[b], stB, W=[stB])
        vcopy("dve", crep_bf[:].rearrange("p a b -> p (a b)"), st[:, 0:1024], [stB], [crepB])
        nstg[0] = 1
        wada_v = wada_d.rearrange("(kc p) n -> p kc n", p=128)
        for ci in range(6):
            cg = first_cg + ci
            bi = ci % 2
            dma("sp", bbc[:], bada_d[0, cg * 512:(cg + 1) * 512].partition_broadcast(128), bbcB, W=[bbcB])
            for kh in range(2):
                load_cvt(wada_bf[:], wada_v[:, kh * 4:(kh + 1) * 4, cg * 512:(cg + 1) * 512], [4, 512], wadaB)
                for k4 in range(4):
                    kc = kh * 4 + k4
                    mm(bk(bi)[:], crep_bf[:, kc, :], wada_bf[:, k4, :], kc == 0, kc == 7,
                       [crepB, wadaB], [bkB(bi)])
            tt(ada[:, ci * 512:(ci + 1) * 512], bk(bi)[:], bbc[:], ALU.add, [bkB(bi), bbcB], [adaB])

    def ada_sl(i):
        return ada[:, i * D:(i + 1) * D]

    def rms_small(src_ap, width, gbc, gB, out_bf_ap, outB, srcB):
        tt(junk[:, 0:width], src_ap, src_ap, ALU.mult, [srcB], [junkB])
        P.op("dve", lambda e: e.tensor_reduce(small[:, 32:33], junk[:, 0:width], AX.X, ALU.add),
             R=[junkB], W=[smallB])
        rstd(small[:, 34:35], small[:, 33:34], small[:, 32:33], width, smallB, smallB)
        stt(out_bf_ap, src_ap, small[:, 34:35], gbc[:, 0:width], ALU.mult, ALU.mult,
            [srcB, smallB, gB], [outB])

    import os as _os
    _stage = int(_os.environ.get("KSTAGE", "99"))

    def token_tile(b, t):
        X, XB = xt[t % 2]
        dma("sp", X[:], x_d[b, t * 128:(t + 1) * 128, :], XB, W=[XB])
        act(junk[:], X[:], AF.Square, [XB], [junkB, smallB], accum=small[:, 0:1])
        rstd(small[:, 2:3], small[:, 1:2], small[:, 0:1], D, smallB, smallB)
        stt(tmpf[:], X[:], small[:, 2:3], ada_sl(1), ALU.mult, ALU.mult, [XB, smallB, adaB], [tmpfB])
        tt(h_bf[:], tmpf[:], ada_sl(0), ALU.add, [tmpfB, adaB], [hbfB])
        for kc in range(8):
            tr(bkbf(7)[:, kc * 128:(kc + 1) * 128], h_bf[:, kc * 128:(kc + 1) * 128], [hbfB], [bkB(7)])
        vcopy("act", hT[:].rearrange("p a b -> p (a b)"), bkbf(7)[:, 0:1024], [bkB(7)], [hTB])

        def zgroup(bi, c0, w):
            for kc in range(8):
                mm(bk(bi)[:, 0:w], hT[:, kc, :], w_in[:, kc, c0:c0 + w], kc == 0, kc == 7,
                   [hTB, winB], [bkB(bi)])

        if _stage < 3:
            return
        zgroup(0, C_Q, 512)
        vcopy("act", zq_f[:], bk(0)[:], [bkB(0)], [zqB])
        tt(junk[:, 0:512], zq_f[:], zq_f[:], ALU.mult, [zqB], [junkB])
        P.op("dve", lambda e: e.tensor_reduce(small[:, 8:16], junk[:, 0:512].rearrange("p (h d) -> p h d", h=8),
                                              AX.X, ALU.add), R=[junkB], W=[smallB])
        rstd(small[:, 24:32], small[:, 16:24], small[:, 8:16], 64, smallB, smallB)
        for h in range(8):
            stt(qn_bf[:, h * 64:(h + 1) * 64], zq_f[:, h * 64:(h + 1) * 64], small[:, 24 + h:25 + h],
                gqbc[:], ALU.mult, ALU.mult, [zqB, smallB, gqB], [qnB])
        for h in range(8):
            tr(bkbf(6)[0:64, h * 128:(h + 1) * 128], qn_bf[:, h * 64:(h + 1) * 64], [qnB], [bkB(6)])
        vcopy("act", qT[:], bkbf(6)[0:64, 0:1024], [bkB(6)], [qTB])
        if _stage < 4:
            return
        zgroup(1, C_K, 452)
        vcopy("act", zb_f[:], bk(1)[:, 0:452], [bkB(1)], [zbB])
        rms_small(zb_f[:, 0:64], 64, gkbc, gkB, knik[:, 0:64], knikB, zbB)
        rms_small(zb_f[:, 384:448], 64, gikbc, gikB, knik[:, 64:128], knikB, zbB)
        vcopy("dve", vaug[:, t, 0:64], zb_f[:, 64:128], [zbB], [vaugB])
        vcopy("dve", iq_pad[:, :, 64:128], zb_f[:, 128:384].rearrange("p (h d) -> p h d", h=4), [zbB], [iqbB])
        tr(bkbf(7)[:, 0:128], knik[:], [knikB], [bkB(7)])
        for h in range(4):
            tr(bkbf(7)[:, 128 + h * 128:128 + (h + 1) * 128], iq_pad[:, h, :], [iqbB], [bkB(7)])
        vcopy("act", kik[:, t * 128:(t + 1) * 128], bkbf(7)[:, 0:128], [bkB(7)], [kikB])
        vcopy("act", iqT[:].rearrange("p a b -> p (a b)"), bkbf(7)[:, 128:640], [bkB(7)], [iqTB])
        if _stage < 5:
            return
        ZP, ZPB = zp[t % 2]
        ZPp, ZPpB = zp[(t + 1) % 2]
        zgroup(2, C_P, 512)
        vcopy("act", ZP[:], bk(2)[:], [bkB(2)], [ZPB])
        for gi in range(4):
            bi = (3 + gi) % 4
            zgroup(bi, C_G + gi * 512, 512)
            act(gates[:, gi * 512:(gi + 1) * 512], bk(bi)[:], AF.Sigmoid, [bkB(bi)], [gatesB])

        if _stage < 6:
            return
        L = 128 * (t + 1)
        ngrp = (L + 511) // 512
        iw = zb_f[:, 448:452]
        for kg in range(ngrp):
            k0 = kg * 512
            w = min(512, L - k0)
            for h in range(4):
                mm(bk(h)[:, 0:w], iqT[64:128, h, :], kik[64:128, k0:k0 + w], True, True, [iqTB, kikB], [bkB(h)])
            for h in range(4):
                R_, RB_ = rbuf[h]
                act(R_[:, 0:w], bk(h)[:, 0:w], AF.Relu, [bkB(h)], [RB_])
                if h == 0:
                    stt(sc[:, k0:k0 + w], R_[:, 0:w], iw[:, 0:1], cst[:, EPS0 + k0:EPS0 + k0 + w],
                        ALU.mult, ALU.add, [RB_, zbB, cstB], [scB])
                else:
                    stt(sc[:, k0:k0 + w], R_[:, 0:w], iw[:, h:h + 1], sc[:, k0:k0 + w],
                        ALU.mult, ALU.add, [RB_, zbB, scB], [scB])
        if _stage < 7:
            return
        P.op("dve", lambda e: e.memset(sc[0:64, L - 64:L], NEG), W=[scB])
        if L - 64 >= NSEL:
            src = sc
            nr = NSEL // 8
            for r in range(nr):
                P.op("dve", (lambda s_: lambda e: e.max(m8[:], s_[:, 0:L]))(src), R=[scB, wkB], W=[m8B])
                if r < nr - 1:
                    P.op("dve", (lambda s_: lambda e: e.match_replace(wk[:, 0:L], m8[:], s_[:, 0:L], NEG))(src),
                         R=[scB, wkB, m8B], W=[wkB])
                    src = wk
            thr = m8[:, 7:8]
        else:
            P.op("dve", lambda e: e.memset(m8[:], -1.0e29), W=[m8B])
            thr = m8[:, 7:8]
        ts(Mb[:, 0:L], sc[:, 0:L], thr, -30000.0, ALU.is_lt, ALU.mult, [scB, m8B], [MbB])

        if _stage < 8:
            return
        def logits(j):
            lb = (0, 1) if j % 2 == 0 else (2, 3)
            for hf in range(2):
                bi = lb[hf]
                mm(bk(bi)[:], kik[0:64, j * 128:(j + 1) * 128], qT[:, hf * 512:(hf + 1) * 512], True, False,
                   [kTB, qTB], [bkB(bi)])
                mm(bk(bi)[:], Mb[:, j * 128:(j + 1) * 128], irep[:], False, True, [MbB, irepB], [bkB(bi)])

        logits(0)
        for j in range(t + 1):
            if j + 1 <= t:
                logits(j + 1)
            lb = (0, 1) if j % 2 == 0 else (2, 3)
            PX, PXB = pexp[j % 2]
            for hf in range(2):
                bi = lb[hf]
                act(PX[:, hf * 512:(hf + 1) * 512], bk(bi)[:], AF.Exp, [bkB(bi)], [PXB], scale=0.125)
            for h in range(8):
                ob = 4 + h // 4
                c0 = (h % 4) * 65
                mm(bk(ob)[:, c0:c0 + 65], PX[:, h * 128:(h + 1) * 128], vaug[:, j, :], (j == 0 and h % 4 == 0), (j == t and h % 4 == 3),
                   [PXB, vaugB], [bkB(ob)])
        if _stage < 9:
            return
        for hb in range(2):
            ov = bk(4 + hb)[:, 0:260].rearrange("p (h c) -> p h c", h=4)
            P.op("dve", (lambda o_, hb_: lambda e: e.reciprocal(small[:, 40 + 4 * hb_:44 + 4 * hb_].rearrange("p (h o) -> p h o", o=1),
                                                               o_[:, :, 64:65]))(ov, hb),
                 R=[bkB(4 + hb)], W=[smallB])
            tt(attn[:, hb * 256:(hb + 1) * 256].rearrange("p (h c) -> p h c", h=4), ov[:, :, 0:64],
               small[:, 40 + 4 * hb:44 + 4 * hb].rearrange("p (h o) -> p h o", o=1).to_broadcast([128, 4, 64]),
               ALU.mult, [bkB(4 + hb), smallB], [attnB])
        for c in range(4):
            tr(bkbf(6)[:, c * 128:(c + 1) * 128], attn[:, c * 128:(c + 1) * 128], [attnB], [bkB(6)])
        vcopy("act", attnT[:].rearrange("p a b -> p (a b)"), bkbf(6)[:, 0:512], [bkB(6)], [attnTB])
        for hf in range(2):
            for c in range(4):
                mm(bk(hf)[:], attnT[:, c, :], w_ua[:, c, hf * 512:(hf + 1) * 512], c == 0, c == 3,
                   [attnTB, wuaB], [bkB(hf)])
        if _stage < 10:
            return
        for g in range(4):
            kind = 2 if t == 0 else 0
            mm(bk(6)[:, g * 128:(g + 1) * 128], ZP[:, g * 128:(g + 1) * 128], band[:, kind * 4 + g, :], True, t == 0,
               [ZPB, bandB], [bkB(6)])
            if t > 0:
                mm(bk(6)[:, g * 128:(g + 1) * 128], ZPp[:, g * 128:(g + 1) * 128], band[:, 4 + g, :], False, True,
                   [ZPpB, bandB], [bkB(6)])
        vcopy("act", mixT[:].rearrange("p a b -> p (a b)"), bk(6)[:], [bkB(6)], [mixTB])
        for g in range(4):
            mm(bk(7)[:, g * 128:(g + 1) * 128], w_grp[:, g, :], mixT[:, g, :], True, True, [wgrpB, mixTB], [bkB(7)])
        vcopy("act", poolT[:].rearrange("p a b -> p (a b)"), bk(7)[:], [bkB(7)], [poolTB])
        for hf in range(2):
            for g in range(4):
                mm(bk(2 + hf)[:], poolT[:, g, :], w_up[:, g, hf * 512:(hf + 1) * 512], g == 0, g == 3,
                   [poolTB, wupB], [bkB(2 + hf)])
        if _stage < 11:
            return
        for hf in range(2):
            cs = slice(hf * 512, (hf + 1) * 512)
            tt(tmpf[:, cs], bk(hf)[:], gates[:, hf * 512:(hf + 1) * 512], ALU.mult, [bkB(hf), gatesB], [tmpfB])
            tt(tmpg[:], bk(2 + hf)[:], gates[:, 1024 + hf * 512:1024 + (hf + 1) * 512], ALU.mult,
               [bkB(2 + hf), gatesB], [tmpgB])
            tt(m_bf[:, cs], tmpf[:, cs], tmpg[:], ALU.add, [tmpfB, tmpgB], [mbfB])
        for kc in range(8):
            tr(bkbf(6)[:, kc * 128:(kc + 1) * 128], m_bf[:, kc * 128:(kc + 1) * 128], [mbfB], [bkB(6)])
        vcopy("act", mT[:].rearrange("p a b -> p (a b)"), bkbf(6)[:, 0:1024], [bkB(6)], [mTB])
        for hf in range(2):
            for kc in range(8):
                mm(bk(4 + hf)[:], mT[:, kc, :], w_out[:, kc, hf * 512:(hf + 1) * 512], kc == 0, kc == 7,
                   [mTB, woutB], [bkB(4 + hf)])
        for hf in range(2):
            cs = slice(hf * 512, (hf + 1) * 512)
            tt(tmpf[:, cs], bk(4 + hf)[:], ada[:, 2 * D + hf * 512:2 * D + (hf + 1) * 512], ALU.mult,
               [bkB(4 + hf), adaB], [tmpfB])
            tt(x1[:, cs], tmpf[:, cs], X[:, cs], ALU.add, [tmpfB, XB], [x1B])
        dma("sp", out_d[b, t * 128:(t + 1) * 128, :], x1[:], x1B, R=[x1B], W=[OUTB])

    for b in range(NB):
        if _stage < 1:
            break
        compute_ada(b, 0)
        if _stage < 2:
            break
        stt(ada_sl(1), ada_sl(1), 1.0, g1bc[:], ALU.add, ALU.mult, [adaB, g1B], [adaB])
        for t in range(NT):
            token_tile(b, t)

    P.final_wait("sp", [OUTB])
    P.emit(nc, es)
    es.close()
    if do_peer:
        nc.all_engine_barrier()
        _phase_p(nc, NB, S, dict(crep=crep_d, cst=cst_d, w_ada=wada_d, b_ada=bada_d, g2=g2_d, wpq=wpq_d,
                                 sk=sk_d, pu=pu_d, pv=pv_d, out=out_d, IOTA0=IOTA0))
    return nc


def _phase_p(nc, NB, S, dr):
    TG = 256
    NG = S // TG
    P = Prog()
    es = ExitStack()
    crep_d, cst_d, wada_d, bada_d, g2_d = dr["crep"], dr["cst"], dr["w_ada"], dr["b_ada"], dr["g2"]
    wpq_d, sk_d, pu_d, pv_d, out_d, IOTA0 = dr["wpq"], dr["sk"], dr["pu"], dr["pv"], dr["out"], dr["IOTA0"]
    uT_d = nc.dram_tensor("uT_scr", [128, 128, 1024], BF16, kind="Internal").ap()
    v_d = nc.dram_tensor("v_scr", [128, 128, 1024], BF16, kind="Internal").ap()
    UTD, VD, OUTB = Buf("uT_d"), Buf("v_d"), Buf("out2")

    def sb(name, shape, dt=F32):
        t = es.enter_context(nc.sbuf_tensor("p_" + name, list(shape), dt))
        return t, Buf(name)

    bank = []
    for i in range(8):
        t_ = es.enter_context(nc.psum_tensor("pbank%d" % i, [128, 512], F32))
        bank.append((t_, Buf("pbank%d" % i)))

    def bk(i):
        return bank[i][0]

    def bkB(i):
        return bank[i][1]

    def bkbf(i):
        return bank[i][0][:].bitcast(BF16)

    cst2, cst2B = sb("cst2", [128, 256])
    ident_bf, identB = sb("ident_bf", [128, 128], BF16)
    iota_bf, iotaB = sb("iota_bf", [128, 128], BF16)
    w_pq, wpqB = sb("w_pq", [128, 8, D], BF16)
    skf, skfB = sb("skf", [128, 8, 128], BF16)
    skbd, skbdB = sb("skbd", [128, 8, 256], BF16)
    g2bc, g2B = sb("g2bc", [128, D])
    ada, adaB = sb("ada", [128, 3 * D])
    epst, epstB = sb("epst", [128, 1])
    ssb, ssbB = sb("ssb", [128, 2048])
    wk2, wk2B = sb("wk2", [128, 2048])
    stg = [(ssb, ssbB), (wk2, wk2B)]
    Xa = sb("Xa", [128, D])
    Xe = sb("Xe", [128, D])
    tmpe, tmpeB = Xa
    OH_ENG = "pool"
    VQ = "pool"
    RATE = 1.3
    acc = [0.0]
    tmpf, tmpfB = sb("tmpf", [128, D])
    junk, junkB = tmpf, tmpfB
    small, smallB = sb("small", [128, 64])
    h2_bf, h2bB = sb("h2_bf", [128, D], BF16)
    h2T = [sb("h2T%d" % i, [128, 8, TG], BF16) for i in range(2)]
    qpT, qpTB = sb("qpT", [128, 8, 128], BF16)
    ts_, tsB = sb("ts", [128, 256])
    ti_, tiB = sb("ti", [128, 256], U32)
    tif, tifB = sb("tif", [128, 256])
    bs_, bsB = sb("bs", [128, 128])
    bp_, bpB = sb("bp", [128, 128], U32)
    bpf, bpfB = sb("bpf", [128, 3, 128])
    gx, gxB = sb("gx", [128, 2, 128])
    abg, abgB = sb("abg", [128, 3, 128], BF16)
    abgf, abgfB = sb("abgf", [128, 3, 128])
    abgT = [sb("abgT%d" % i, [128, 3, 128], BF16) for i in range(4)]
    Aoh = [sb("Aoh%d" % i, [128, 32, 128], BF16) for i in range(2)]
    Boh = [sb("Boh%d" % i, [128, 32, 128], BF16) for i in range(2)]
    _a0 = Aoh[0][0][:].rearrange("p n c -> p (n c)")
    crep_bf, crepB = _a0[:, 0:1024].rearrange("p (a b) -> p a b", a=8), Aoh[0][1]
    wada_bf, wadaB = _a0[:, 1024:3072].rearrange("p (a b) -> p a b", a=4), Aoh[0][1]
    bbc, bbcB = Boh[0][0][:].rearrange("p n c -> p (n c)").bitcast(F32)[:, 0:512], Boh[0][1]
    Gsb, GsbB = sb("Gsb", [128, 128, TG], BF16)
    NSL = 4
    uT_sb = [sb("uT_sb%d" % i, [128, D], BF16) for i in range(NSL)]
    v_sb = [sb("v_sb%d" % i, [128, D], BF16) for i in range(NSL)]
    ga = [sb("ga%d" % i, [128, TG]) for i in range(2)]
    GA = [sb("GA%d" % i, [128, TG], BF16) for i in range(2)]
    Gflat = Gsb[:].rearrange("p c n -> p (c n)")
    u_bf, ubfB = Gflat[:, 0:1024], Buf("u_bf")
    uT_st = [(Gflat[:, 1024 * (1 + i):1024 * (2 + i)], Buf("uT_st%d" % i)) for i in range(2)]
    v_st = [(Gflat[:, 1024 * (3 + i):1024 * (4 + i)], Buf("v_st%d" % i)) for i in range(2)]

    def dma(q, out_ap, in_ap, sbuf, R=(), W=()):
        P.dma(q, lambda e: e.dma_start(out=out_ap, in_=in_ap), sbuf, R=R, W=W)

    def mm(out_ap, lhsT, rhs, start, stop, R, W):
        P.op("pe", lambda e: e.matmul(out_ap, lhsT, rhs, start=start, stop=stop), R=R, W=W)

    def tr(out_ap, in_ap, R, W):
        P.op("pe", lambda e: e.transpose(out_ap, in_ap, ident_bf[:]), R=list(R) + [identB], W=W)

    def act(out_ap, in_ap, func, R, W, bias=None, scale=None, accum=None):
        kw = {}
        if bias is not None:
            kw["bias"] = bias
        if scale is not None:
            kw["scale"] = scale
        if accum is not None:
            kw["accum_out"] = accum
        P.op("act", lambda e: e.activation(out_ap, in_ap, func, **kw), R=R, W=W)

    def vcopy(eng, out_ap, in_ap, R, W):
        if eng == "act":
            P.op("act", lambda e: e.copy(out_ap, in_ap), R=R, W=W)
        else:
            P.op(eng, lambda e: e.tensor_copy(out_ap, in_ap), R=R, W=W)

    def tt(out_ap, a, b, op, R, W, eng="dve"):
        P.op(eng, lambda e: e.tensor_tensor(out_ap, a, b, op), R=R, W=W)

    def ts1(out_ap, a, s1, op0, R, W, eng="dve"):
        P.op(eng, lambda e: e.tensor_scalar(out_ap, a, s1, None, op0), R=R, W=W)

    def stt(out_ap, a, s_, b, op0, op1, R, W, eng="dve"):
        P.op(eng, lambda e: e.scalar_tensor_tensor(out_ap, a, s_, b, op0, op1), R=R, W=W)

    def rstd(out_ap, tmp_ap, ss_ap, n, RB, WB):
        act(tmp_ap, ss_ap, AF.Sqrt, [RB, epstB], [WB], bias=epst[:, 0:1], scale=1.0 / n)
        P.op("dve", lambda e: e.reciprocal(out_ap, tmp_ap), R=[WB], W=[WB])

    nstg = [0]

    def load_cvt(dst_ap, src_ap, shape_free, dstB, eng=None):
        i = nstg[0] % 2
        nstg[0] += 1
        st, stB = stg[i]
        nel = int(np.prod(shape_free))
        view = st[:, 0:nel]
        if len(shape_free) == 2:
            view = view.rearrange("p (a b) -> p a b", a=shape_free[0])
        dma("sp", view, src_ap, stB, W=[stB])
        vcopy(eng or ("dve" if i == 0 else "act"), dst_ap, view, [stB], [dstB])

    dma("sp", cst2[:, 0:128], cst_d[:, 0:128], cst2B, W=[cst2B])
    dma("sp", cst2[:, 128:256], cst_d[:, IOTA0:IOTA0 + 128], cst2B, W=[cst2B])
    vcopy("dve", ident_bf[:], cst2[:, 0:128], [cst2B], [identB])
    vcopy("dve", iota_bf[:], cst2[:, 128:256], [cst2B], [iotaB])
    P.op("dve", lambda e: e.memset(epst[:], 1e-6), W=[epstB])
    P.op("dve", lambda e: e.memset(skbd[:], 0.0), W=[skbdB])
    dma("sp", g2bc[:], g2_d.partition_broadcast(128), g2B, W=[g2B])
    wpq_v = wpq_d.rearrange("(kc p) n -> p kc n", p=128)
    for hh in range(4):
        load_cvt(w_pq[:, hh * 2:(hh + 1) * 2, :], wpq_v[:, hh * 2:(hh + 1) * 2, :], [2, D], wpqB)
    st0, st0B = stg[0]
    for p_ in range(2):
        dma("sp", st0[:, 0:1024].rearrange("k (h pd) -> k h pd", h=8)[:, :, p_ * 64:(p_ + 1) * 64],
            sk_d.rearrange("(h p) k d -> k h p d", p=2)[:, :, p_, :], st0B, W=[st0B])
    vcopy("dve", skf[:].rearrange("p a b -> p (a b)"), st0[:, 0:1024], [st0B], [skfB])
    for h in range(8):
        tr(bkbf(4)[:, h * 128:(h + 1) * 128], skf[:, h, :], [skfB], [bkB(4)])
    for h in range(8):
        vcopy("act", skbd[0:64, h, 0:128], bkbf(4)[0:64, h * 128:(h + 1) * 128], [bkB(4)], [skbdB])
        vcopy("act", skbd[64:128, h, 128:256], bkbf(4)[64:128, h * 128:(h + 1) * 128], [bkB(4)], [skbdB])

    for c in range(128):
        i = c % 2
        UT, UTB = uT_st[i]
        VS, VSB = v_st[i]
        dma("sp", ssb[:, 0:1024], pu_d[c * 128:(c + 1) * 128, :], ssbB, W=[ssbB])
        vcopy("dve", u_bf, ssb[:, 0:1024], [ssbB], [ubfB])
        for kc in range(8):
            tr(bkbf(4 + i)[:, kc * 128:(kc + 1) * 128], u_bf[:, kc * 128:(kc + 1) * 128], [ubfB], [bkB(4 + i)])
        vcopy("act", UT, bkbf(4 + i)[:, 0:1024], [bkB(4 + i)], [UTB])
        dma("sp", uT_d[c], UT, UTB, R=[UTB], W=[UTD])
        dma("sp", wk2[:, 0:1024], pv_d[c * 128:(c + 1) * 128, :], wk2B, W=[wk2B])
        vcopy("dve" if i == 0 else "act", VS, wk2[:, 0:1024], [wk2B], [VSB])
        dma("sp", v_d[c], VS, VSB, R=[VSB], W=[VD])
    P.barrier()

    def compute_ada(b, first_cg):
        st, stB = stg[0]
        dma("sp", st[:, 0:1024], crep_d[b], stB, W=[stB])
        vcopy("dve", crep_bf[:].rearrange("p a b -> p (a b)"), st[:, 0:1024], [stB], [crepB])
        nstg[0] = 1
        wada_v = wada_d.rearrange("(kc p) n -> p kc n", p=128)
        for ci in range(6):
            cg = first_cg + ci
            bi = 4 + ci % 2
            dma("sp", bbc[:], bada_d[0, cg * 512:(cg + 1) * 512].partition_broadcast(128), bbcB, W=[bbcB])
            for kh in range(2):
                load_cvt(wada_bf[:], wada_v[:, kh * 4:(kh + 1) * 4, cg * 512:(cg + 1) * 512], [4, 512], wadaB)
                for k4 in range(4):
                    kc = kh * 4 + k4
                    mm(bk(bi)[:], crep_bf[:, kc, :], wada_bf[:, k4, :], kc == 0, kc == 7,
                       [crepB, wadaB], [bkB(bi)])
            tt(ada[:, ci * 512:(ci + 1) * 512], bk(bi)[:], bbc[:], ALU.add, [bkB(bi), bbcB], [adaB])

    def bc_last(ap, shape):
        return ap.to_broadcast(list(shape))

    def peer_A1(b, t, tl, sl):
        X, XB = Xa
        H2T, H2TB = h2T[sl]
        ABT, ABTB = abgT[sl * 2 + tl]
        dma("sp", X[:], out_d[b, t * 128:(t + 1) * 128, :], XB, W=[XB])
        act(tmpf[:], X[:], AF.Square, [XB], [tmpfB, smallB], accum=small[:, 0:1])
        rstd(small[:, 2:3], small[:, 1:2], small[:, 0:1], D, smallB, smallB)
        stt(tmpf[:], X[:], small[:, 2:3], ada[:, D:2 * D], ALU.mult, ALU.mult, [XB, smallB, adaB], [tmpfB])
        tt(h2_bf[:], tmpf[:], ada[:, 0:D], ALU.add, [tmpfB, adaB], [h2bB])
        yield
        for kc in range(8):
            tr(bkbf(6)[:, kc * 128:(kc + 1) * 128], h2_bf[:, kc * 128:(kc + 1) * 128], [h2bB], [bkB(6)])
        vcopy("act", H2T[:, :, tl * 128:(tl + 1) * 128], bkbf(6)[:, 0:1024].rearrange("p (a b) -> p a b", a=8),
              [bkB(6)], [H2TB])
        yield
        for oc in range(8):
            bi = 6 + oc // 4
            for kc in range(8):
                mm(bk(bi)[:, (oc % 4) * 128:(oc % 4 + 1) * 128], w_pq[:, kc, oc * 128:(oc + 1) * 128],
                   H2T[:, kc, tl * 128:(tl + 1) * 128], kc == 0, kc == 7, [wpqB, H2TB], [bkB(bi)])
            yield
        for hh in range(2):
            vcopy("act", qpT[:, hh * 4:(hh + 1) * 4, :].rearrange("p a b -> p (a b)"), bk(6 + hh)[:],
                  [bkB(6 + hh)], [qpTB])
        yield
        for hq in range(2):
            for h4 in range(4):
                h = hq * 4 + h4
                mm(bk(6 + h4 // 2)[:, (h4 % 2) * 256:(h4 % 2 + 1) * 256], qpT[:, h, :], skbd[:, h, :], True, True,
                   [qpTB, skbdB], [bkB(6 + h4 // 2)])
            for q_ in range(2):
                vcopy("act", ssb[:, (hq * 2 + q_) * 512:(hq * 2 + q_ + 1) * 512], bk(6 + q_)[:], [bkB(6 + q_)], [ssbB])
            yield
        for hp in range(16):
            sl_ = slice(hp * 128, (hp + 1) * 128)
            o0 = slice(hp * 16, hp * 16 + 8)
            o1 = slice(hp * 16 + 8, hp * 16 + 16)
            P.op("dve", (lambda sl_=sl_, o0=o0: lambda e: e.max(ts_[:, o0], ssb[:, sl_]))(), R=[ssbB], W=[tsB])
            P.op("dve", (lambda sl_=sl_, o0=o0: lambda e: e.max_index(ti_[:, o0], ts_[:, o0], ssb[:, sl_]))(),
                 R=[ssbB, tsB], W=[tiB])
            P.op("dve", (lambda sl_=sl_, o0=o0: lambda e: e.match_replace(wk2[:, sl_], ts_[:, o0], ssb[:, sl_], NEG))(),
                 R=[ssbB, tsB], W=[wk2B])
            yield
            P.op("dve", (lambda sl_=sl_, o1=o1: lambda e: e.max(ts_[:, o1], wk2[:, sl_]))(), R=[wk2B], W=[tsB])
            P.op("dve", (lambda sl_=sl_, o1=o1: lambda e: e.max_index(ti_[:, o1], ts_[:, o1], wk2[:, sl_]))(),
                 R=[wk2B, tsB], W=[tiB])
            yield
        for h in range(8):
            a0 = ts_[:, (2 * h) * 16:(2 * h) * 16 + 16].rearrange("p (x o) -> p x o", o=1)
            a1 = ts_[:, (2 * h + 1) * 16:(2 * h + 1) * 16 + 16].rearrange("p (o y) -> p o y", o=1)
            tt(ssb[:, h * 256:(h + 1) * 256].rearrange("p (x y) -> p x y", x=16),
               bc_last(a0, [128, 16, 16]), bc_last(a1, [128, 16, 16]), ALU.add, [tsB], [ssbB])
            if h % 2:
                yield
        for h in range(8):
            sl_ = slice(h * 256, (h + 1) * 256)
            o0 = slice(h * 16, h * 16 + 8)
            o1 = slice(h * 16 + 8, h * 16 + 16)
            P.op("dve", (lambda sl_=sl_, o0=o0: lambda e: e.max(bs_[:, o0], ssb[:, sl_]))(), R=[ssbB], W=[bsB])
            P.op("dve", (lambda sl_=sl_, o0=o0: lambda e: e.max_index(bp_[:, o0], bs_[:, o0], ssb[:, sl_]))(),
                 R=[ssbB, bsB], W=[bpB])
            P.op("dve", (lambda sl_=sl_, o0=o0: lambda e: e.match_replace(wk2[:, sl_], bs_[:, o0], ssb[:, sl_], NEG))(),
                 R=[ssbB, bsB], W=[wk2B])
            yield
            P.op("dve", (lambda sl_=sl_, o1=o1: lambda e: e.max(bs_[:, o1], wk2[:, sl_]))(), R=[wk2B], W=[bsB])
            P.op("dve", (lambda sl_=sl_, o1=o1: lambda e: e.max_index(bp_[:, o1], bs_[:, o1], wk2[:, sl_]))(),
                 R=[wk2B, bsB], W=[bpB])
            yield
        bs3 = bs_[:].rearrange("p (h j) -> p h j", h=8)
        tt(gx[:, 1, :].rearrange("p (h j) -> p h j", h=8), bs3, bc_last(bs3[:, :, 0:1], [128, 8, 16]),
           ALU.subtract, [bsB], [gxB])
        act(gx[:, 0, :], gx[:, 1, :], AF.Exp, [gxB], [gxB])
        P.op("dve", lambda e: e.tensor_reduce(small[:, 8:16], gx[:, 0, :].rearrange("p (h j) -> p h j", h=8),
                                              AX.X, ALU.add), R=[gxB], W=[smallB])
        P.op("dve", lambda e: e.reciprocal(small[:, 16:24], small[:, 8:16]), R=[smallB], W=[smallB])
        tt(abgf[:, 2, :].rearrange("p (h j) -> p h j", h=8), gx[:, 0, :].rearrange("p (h j) -> p h j", h=8),
           bc_last(small[:, 16:24].rearrange("p (h o) -> p h o", o=1), [128, 8, 16]), ALU.mult,
           [gxB, smallB], [abgfB])
        yield
        vcopy("dve", bpf[:, 0, :], bp_[:], [bpB], [bpfB])
        vcopy("dve", tif[:], ti_[:], [tiB], [tifB])
        ts1(bpf[:, 2, :], bpf[:, 0, :], 16.0, ALU.is_ge, [bpfB], [bpfB])
        yield
        for k_ in range(2, 16):
            stt(bpf[:, 2, :], bpf[:, 0, :], 16.0 * k_, bpf[:, 2, :], ALU.is_ge, ALU.add, [bpfB], [bpfB])
            if k_ % 4 == 0:
                yield
        stt(bpf[:, 1, :], bpf[:, 2, :], -16.0, bpf[:, 0, :], ALU.mult, ALU.add, [bpfB], [bpfB])
        iota16 = cst2[:, 128:144]
        tif4 = tif[:].rearrange("p (h q x) -> p h q x", h=8, q=2)
        for which, (src_i, half) in enumerate(((2, 0), (1, 1))):
            for h in range(8):
                pv_ = bpf[:, src_i, h * 16:(h + 1) * 16].rearrange("p (j o) -> p j o", o=1)
                eqv = ssb[:, h * 256:(h + 1) * 256].rearrange("p (j x) -> p j x", j=16)
                tt(eqv, bc_last(pv_, [128, 16, 16]),
                   bc_last(iota16.rearrange("p (o x) -> p o x", o=1), [128, 16, 16]), ALU.is_equal,
                   [bpfB, cst2B], [ssbB])
                tt(wk2[:, h * 256:(h + 1) * 256].rearrange("p (j x) -> p j x", j=16), eqv,
                   bc_last(tif4[:, h, half, :].rearrange("p (o x) -> p o x", o=1), [128, 16, 16]), ALU.mult,
                   [ssbB, tifB], [wk2B])
                if h % 2:
                    yield
            P.op("dve", (lambda which=which: lambda e: e.tensor_reduce(
                abgf[:, which, :], wk2[:, 0:2048].rearrange("p (r x) -> p r x", x=16), AX.X, ALU.add))(),
                R=[wk2B], W=[abgfB])
            yield
        vcopy("dve", abg[:].rearrange("p a b -> p (a b)"), abgf[:].rearrange("p a b -> p (a b)"), [abgfB], [abgB])
        for i in range(3):
            tr(bkbf(7)[:, i * 128:(i + 1) * 128], abg[:, i, :], [abgB], [bkB(7)])
        vcopy("act", ABT[:].rearrange("p a b -> p (a b)"), bkbf(7)[:, 0:384], [bkB(7)], [ABTB])
        yield

    noh = [0]

    def peer_A2(sl):
        for tl in range(2):
            ABT, ABTB = abgT[sl * 2 + tl]
            for hs in range(4):
                n0 = hs * 32
                k = noh[0] % 2
                noh[0] += 1
                A_, AB_ = Aoh[k]
                B_, BB_ = Boh[k]
                io = bc_last(iota_bf[:].rearrange("p (o c) -> p o c", o=1), [128, 32, 128])
                aT = bc_last(ABT[:, 0, n0:n0 + 32].rearrange("p (n o) -> p n o", o=1), [128, 32, 128])
                bT = bc_last(ABT[:, 1, n0:n0 + 32].rearrange("p (n o) -> p n o", o=1), [128, 32, 128])
                gT = bc_last(ABT[:, 2, n0:n0 + 32].rearrange("p (n o) -> p n o", o=1), [128, 32, 128])
                tt(A_[:], io, aT, ALU.is_equal, [iotaB, ABTB], [AB_])
                tt(A_[:], A_[:], gT, ALU.mult, [AB_, ABTB], [AB_], eng=OH_ENG)
                tt(B_[:], io, bT, ALU.is_equal, [iotaB, ABTB], [BB_])
                for q4 in range(8):
                    bi = 6 + q4 % 2
                    for i in range(4):
                        n = q4 * 4 + i
                        mm(bk(bi)[:, i * 128:(i + 1) * 128], B_[:, n, :], A_[:, n, :], True, True,
                           [BB_, AB_], [bkB(bi)])
                    tok0 = tl * 128 + n0 + q4 * 4
                    vcopy("act", Gsb[:, :, tok0:tok0 + 4].rearrange("p c n -> p n c"),
                          bk(bi)[:].rearrange("p (n c) -> p n c", n=4), [bkB(bi)], [GsbB])

    cnt = [0]

    def peer_B(b, gi, sl, nxt):
        H2T, H2TB = h2T[sl]
        base = cnt[0]
        cnt[0] += 128

        def loads(c):
            UT, UTB = uT_sb[(base + c) % NSL]
            VS, VSB = v_sb[(base + c) % NSL]
            dma("sp", UT[:], uT_d[c], UTB, R=[UTD], W=[UTB])
            dma(VQ, VS[:], v_d[c], VSB, R=[VD], W=[VSB])

        def actmm(c):
            UT, UTB = uT_sb[(base + c) % NSL]
            bi = 4 + c % 2
            for kc in range(8):
                mm(bk(bi)[:, 0:TG], UT[:, kc * 128:(kc + 1) * 128], H2T[:, kc, :], kc == 0, kc == 7,
                   [UTB, H2TB], [bkB(bi)])

        loads(0)
        loads(1)
        loads(2)
        actmm(0)
        for c in range(128):
            if c + 3 < 128:
                loads(c + 3)
            if c + 1 < 128:
                actmm(c + 1)
            VS, VSB = v_sb[(base + c) % NSL]
            bi = 4 + c % 2
            g_, gB_ = ga[c % 2]
            G_, GB_ = GA[c % 2]
            act(g_[:], bk(bi)[:, 0:TG], AF.Gelu, [bkB(bi)], [gB_])
            tt(G_[:], g_[:], Gsb[:, c, :], ALU.mult, [gB_, GsbB], [GB_])
            for tl in range(2):
                for hf in range(2):
                    yb = tl * 2 + hf
                    mm(bk(yb)[:], G_[:, tl * 128:(tl + 1) * 128], VS[:, hf * 512:(hf + 1) * 512], c == 0, c == 127,
                       [GB_, VSB], [bkB(yb)])
            if nxt is not None:
                acc[0] += RATE
                while nxt is not None and acc[0] >= 1.0:
                    acc[0] -= 1.0
                    if next(nxt, "end") == "end":
                        nxt = None
        if nxt is not None:
            for _ in nxt:
                pass
        X, XB = Xe
        for tl in range(2):
            t = gi * 2 + tl
            dma("sp", X[:], out_d[b, t * 128:(t + 1) * 128, :], XB, W=[XB])
            for hf in range(2):
                cs = slice(hf * 512, (hf + 1) * 512)
                yb = tl * 2 + hf
                tt(tmpe[:, cs], bk(yb)[:], ada[:, 2 * D + hf * 512:2 * D + (hf + 1) * 512], ALU.mult,
                   [bkB(yb), adaB], [tmpeB])
                tt(X[:, cs], tmpe[:, cs], X[:, cs], ALU.add, [tmpeB, XB], [XB])
            dma("sp", out_d[b, t * 128:(t + 1) * 128, :], X[:], XB, R=[XB], W=[OUTB])

    def a1_group(b, gi, sl):
        for tl in range(2):
            yield from peer_A1(b, gi * 2 + tl, tl, sl)

    for b in range(NB):
        compute_ada(b, 6)
        stt(ada[:, D:2 * D], ada[:, D:2 * D], 1.0, g2bc[:], ALU.add, ALU.mult, [adaB, g2B], [adaB])
        for _ in a1_group(b, 0, 0):
            pass
        for gi in range(NG):
            sl = gi % 2
            peer_A2(sl)
            nxt = a1_group(b, gi + 1, 1 - sl) if gi + 1 < NG else None
            peer_B(b, gi, sl, nxt)
    P.barrier()
    P.emit(nc, es)
    es.close()


def _prep_inputs(inp, NB, S, core, ncores_total):
    f = lambda a: np.ascontiguousarray(np.asarray(a, dtype=np.float32))
    b0 = core * NB
    c = f(inp["c"])[b0:b0 + NB]
    crep = np.ascontiguousarray(
        np.broadcast_to(c.reshape(NB, 8, 128).transpose(0, 2, 1)[:, :, :, None], (NB, 128, 8, 128)).reshape(NB, 128, 1024))
    m = {
        "x": f(inp["x"])[b0:b0 + NB],
        "crep": crep,
        "cst": _consts(S),
        "w_ada": f(inp["w_ada"])[0],
        "b_ada": f(inp["b_ada"])[0].reshape(1, 6 * D),
        "g_norm1": f(inp["g_norm1"])[0],
        "w_in": f(inp["w_in"])[0],
        "g_q": f(inp["g_q"])[0],
        "g_k": f(inp["g_k"])[0],
        "g_ik": f(inp["g_ik"])[0],
        "w_pool_grp": f(inp["w_pool_grp"])[0],
        "s_pool": f(inp["s_pool"])[0].reshape(512),
        "w_up_attn": f(inp["w_up_attn"])[0],
        "w_up_pool": f(inp["w_up_pool"])[0],
        "w_out": f(inp["w_out"])[0],
        "g_norm2": f(inp["g_norm2"])[0],
        "w_peer_q": f(inp["w_peer_q"])[0],
        "peer_subkeys": f(inp["peer_subkeys"])[0].reshape(16, 128, 64),
        "peer_u": f(inp["peer_u"])[0],
        "peer_v": f(inp["peer_v"])[0],
    }
    return m


def kernel(**inputs):
    x = np.asarray(inputs["x"])
    B, S, _ = x.shape
    NB = B // NCORES
    NSEL = min(256, S // 4)
    nc = build(NB, S, NSEL)
    in_maps = [_prep_inputs(inputs, NB, S, i, NCORES) for i in range(NCORES)]
    res = run_bass_kernel_spmd(nc, in_maps, core_ids=list(range(NCORES)))
    out = np.concatenate([np.asarray(r["out"]) for r in res.results], axis=0)
    return out.astype(np.float32)
```
